# Optimizing a Trainium2 kernel written in Bass

```python
import math
import jax, jax.numpy as jnp
from jax import lax
import numpy as np

D_MODEL = 1024
BATCH = 8
SEQ = 4096
DEPTH = 1

GRID_W = 64
CTX_LEN = 256
EPS = 1e-6
S5_WIDTH = 512
S5_GROUP = 16
S5_GROUPS = S5_WIDTH // S5_GROUP
S5_STATE = 64
S5_DT_MIN = 1e-3
S5_DT_MAX = 1e-1
S5_MAX_RE = -1e-4
DA_HEADS = 4
DA_HEAD_DIM = 64
DA_V_DIM = 2 * DA_HEAD_DIM
DA_QK_WIDTH = DA_HEADS * 2 * DA_HEAD_DIM
DA_WIDTH = DA_HEADS * DA_V_DIM
Q_BLOCK = 128
ROPE_THETA = 10000.0
ROPE_PAIRS = DA_HEAD_DIM // 4
IN_SPLITS = (S5_WIDTH, S5_WIDTH + DA_QK_WIDTH, S5_WIDTH + 2 * DA_QK_WIDTH,
             S5_WIDTH + 2 * DA_QK_WIDTH + DA_WIDTH,
             S5_WIDTH + 2 * DA_QK_WIDTH + DA_WIDTH + D_MODEL)
IN_COLS = S5_WIDTH + 2 * DA_QK_WIDTH + DA_WIDTH + 2 * D_MODEL
N_EXPERTS = 16
EC_CAPACITY = 2
D_EXPERT = 2048

kernel_name = 'hybrid_s5_diffattn_ecmoe_dit_layer'


def rms_norm(x, w):
    xf = x.astype(jnp.float32)
    y = xf * lax.rsqrt(jnp.mean(xf * xf, axis=-1, keepdims=True) + EPS)
    return (y * w.astype(jnp.float32)).astype(x.dtype)


def modulate(h, shift, scale):
    return h * (1.0 + scale) + shift


def split_in(z):
    b, l = z.shape[:2]
    u, q, k, v, ga, gb = jnp.split(z, IN_SPLITS, axis=-1)
    q = q.reshape(b, l, DA_HEADS, 2, DA_HEAD_DIM)
    k = k.reshape(b, l, DA_HEADS, 2, DA_HEAD_DIM)
    v = v.reshape(b, l, DA_HEADS, DA_V_DIM)
    return u, q, k, v, ga, gb


def rope_1d(x, ang):
    x1, x2 = jnp.split(x, 2, axis=-1)
    cos = jnp.cos(ang)[:, None, None, :].astype(x.dtype)
    sin = jnp.sin(ang)[:, None, None, :].astype(x.dtype)
    return jnp.concatenate([x1 * cos - x2 * sin, x2 * cos + x1 * sin], axis=-1)


def rope_2d(x, row_ang, col_ang):
    half = x.shape[-1] // 2
    return jnp.concatenate([rope_1d(x[..., :half], row_ang), rope_1d(x[..., half:], col_ang)], axis=-1)


def _scan_combine(e1, e2):
    a1r, a1i, b1r, b1i = e1
    a2r, a2i, b2r, b2i = e2
    return (a2r * a1r - a2i * a1i, a2r * a1i + a2i * a1r,
            a2r * b1r - a2i * b1i + b2r, a2r * b1i + a2i * b1r + b2i)


def s5_discretise(lam_re, lam_im, log_dt, b_re, b_im):
    lam_re = jnp.minimum(lam_re.astype(jnp.float32), S5_MAX_RE)
    lam_im = lam_im.astype(jnp.float32)
    dt = jnp.exp(log_dt.astype(jnp.float32))[:, None]
    mag = jnp.exp(lam_re * dt)
    lb_re = mag * jnp.cos(lam_im * dt)
    lb_im = mag * jnp.sin(lam_im * dt)
    den = lam_re * lam_re + lam_im * lam_im
    num_re = lb_re - 1.0
    co_re = (num_re * lam_re + lb_im * lam_im) / den
    co_im = (lb_im * lam_re - num_re * lam_im) / den
    br = b_re.astype(jnp.float32)
    bi = b_im.astype(jnp.float32)
    bb_re = co_re[..., None] * br - co_im[..., None] * bi
    bb_im = co_re[..., None] * bi + co_im[..., None] * br
    return lb_re, lb_im, bb_re, bb_im


def s5_scan(u, lb_re, lb_im, bb_re, bb_im, h0, reverse):
    bu_re = jnp.einsum('blgh,gph->blgp', u, bb_re)
    bu_im = jnp.einsum('blgh,gph->blgp', u, bb_im)
    if h0 is not None:
        h0_re, h0_im = h0
        edge = -1 if reverse else 0
        bu_re = bu_re.at[:, edge].add(lb_re * h0_re - lb_im * h0_im)
        bu_im = bu_im.at[:, edge].add(lb_re * h0_im + lb_im * h0_re)
    a_re = jnp.broadcast_to(lb_re, bu_re.shape)
    a_im = jnp.broadcast_to(lb_im, bu_im.shape)
    _, _, h_re, h_im = lax.associative_scan(_scan_combine, (a_re, a_im, bu_re, bu_im),
                                            reverse=reverse, axis=1)
    return h_re, h_im


def s5_readout(h_re, h_im, c_re, c_im):
    return (jnp.einsum('blgp,ghp->blgh', h_re, c_re.astype(jnp.float32))
            - jnp.einsum('blgp,ghp->blgh', h_im, c_im.astype(jnp.float32)))


def s5_output(y, u, d_skip, w_glu, dtype):
    b, l = y.shape[:2]
    y = y + d_skip.astype(jnp.float32).reshape(S5_GROUPS, S5_GROUP) * u
    y = jax.nn.gelu(y.reshape(b, l, S5_WIDTH).astype(dtype))
    return y * jax.nn.sigmoid(y @ w_glu)


def s5_mixer(u_ctx, u_lat, p, need_ctx_out):
    b = u_lat.shape[0]
    uc = u_ctx.astype(jnp.float32).reshape(b, u_ctx.shape[1], S5_GROUPS, S5_GROUP)
    ul = u_lat.astype(jnp.float32).reshape(b, u_lat.shape[1], S5_GROUPS, S5_GROUP)
    y_lat = jnp.zeros_like(ul)
    y_ctx = jnp.zeros_like(uc)
    for d, reverse in ((0, False), (1, True)):
        disc = s5_discretise(p['s5_lam_re'][d], p['s5_lam_im'][d], p['s5_log_dt'][d],
                             p['s5_b_re'][d], p['s5_b_im'][d])
        hc_re, hc_im = s5_scan(uc, *disc, None, reverse)
        edge = 0 if reverse else -1
        hl_re, hl_im = s5_scan(ul, *disc, (hc_re[:, edge], hc_im[:, edge]), reverse)
        y_lat = y_lat + s5_readout(hl_re, hl_im, p['s5_c_re'][d], p['s5_c_im'][d])
        if need_ctx_out:
            y_ctx = y_ctx + s5_readout(hc_re, hc_im, p['s5_c_re'][d], p['s5_c_im'][d])
    out_lat = s5_output(y_lat, ul, p['s5_d'], p['w_glu'], u_lat.dtype)
    out_ctx = s5_output(y_ctx, uc, p['s5_d'], p['w_glu'], u_ctx.dtype) if need_ctx_out else None
    return out_lat, out_ctx


def diff_attention(q, k, v, lam, subln_w, lam_init):
    b, lq = q.shape[:2]
    nb = lq // Q_BLOCK
    qb = jnp.moveaxis(q.reshape(b, nb, Q_BLOCK, DA_HEADS, 2, DA_HEAD_DIM), 1, 0)
    scale = DA_HEAD_DIM ** -0.5

    def one_block(qi):
        s = jnp.einsum('bqhcd,bkhcd->bchqk', qi, k).astype(jnp.float32) * scale
        pr = jax.nn.softmax(s, axis=-1)
        pd = pr[:, 0] - lam * pr[:, 1]
        return jnp.einsum('bhqk,bkhd->bqhd', pd.astype(v.dtype), v)

    o = lax.map(one_block, qb)
    o = jnp.moveaxis(o, 0, 1).reshape(b, lq, DA_HEADS, DA_V_DIM)
    o = rms_norm(o, subln_w) * (1.0 - lam_init)
    return o.reshape(b, lq, DA_WIDTH)


def merge_branches(ya, yb, ga, gb, p):
    m = jax.nn.sigmoid(ga) * (ya @ p['w_proj_a']) + jax.nn.sigmoid(gb) * (yb @ p['w_proj_b'])
    return m @ p['w_out']


def ec_moe(h, w_router, w_gate, w_up, w_down):
    b, l, d = h.shape
    cap = EC_CAPACITY * l // N_EXPERTS
    aff = jax.nn.softmax((h @ w_router).astype(jnp.float32), axis=-1)
    gates, idx = lax.top_k(jnp.swapaxes(aff, 1, 2), cap)
    xs = jax.vmap(lambda hb, ib: hb[ib])(h, idx)
    hid = jax.nn.silu(jnp.einsum('becd,edf->becf', xs, w_gate)) * jnp.einsum('becd,edf->becf', xs, w_up)
    ys = jnp.einsum('becf,efd->becd', hid, w_down) * gates[..., None].astype(h.dtype)
    return jax.vmap(lambda yb, ib: jnp.zeros((l, d), yb.dtype).at[ib.reshape(-1)].add(yb.reshape(-1, d)))(ys, idx)


def hybrid_layer(x, xc, mod_lat, mod_ctx, p, row_ang, col_ang, lam_init, need_ctx_out):
    sh_m, sc_m, g_m, sh_f, sc_f, g_f = jnp.split(mod_lat, 6, axis=-1)
    csh_m, csc_m, cg_m, csh_f, csc_f, cg_f = jnp.split(mod_ctx, 6, axis=-1)
    h = modulate(rms_norm(x, p['norm_pre_mix']), sh_m, sc_m)
    hc = modulate(rms_norm(xc, p['norm_pre_mix']), csh_m, csc_m)
    u, q, k, v, ga, gb = split_in(h @ p['w_in'])
    uc, qc, kc, vc, gac, gbc = split_in(hc @ p['w_in'])
    ya, yac = s5_mixer(uc, u, p, need_ctx_out)
    q = rope_2d(q, row_ang, col_ang)
    k = rope_2d(k, row_ang, col_ang)
    lq1, lk1, lq2, lk2 = p['da_lambda'].astype(jnp.float32)
    lam = jnp.exp(jnp.sum(lq1 * lk1)) - jnp.exp(jnp.sum(lq2 * lk2)) + lam_init
    yb = diff_attention(q, jnp.concatenate([kc, k], axis=1), jnp.concatenate([vc, v], axis=1),
                        lam, p['da_subln'], lam_init)
    x = x + g_m * rms_norm(merge_branches(ya, yb, ga, gb, p), p['norm_post_mix'])
    h = modulate(rms_norm(x, p['norm_pre_ffn']), sh_f, sc_f)
    f = ec_moe(h, p['w_router'], p['w_exp_gate'], p['w_exp_up'], p['w_exp_down'])
    x = x + g_f * rms_norm(f, p['norm_post_ffn'])
    if need_ctx_out:
        ybc = diff_attention(qc, kc, vc, lam, p['da_subln'], lam_init)
        xc = xc + cg_m * rms_norm(merge_branches(yac, ybc, gac, gbc, p), p['norm_post_mix'])
        hc = modulate(rms_norm(xc, p['norm_pre_ffn']), csh_f, csc_f)
        fc = ec_moe(hc, p['w_router'], p['w_exp_gate'], p['w_exp_up'], p['w_exp_down'])
        xc = xc + cg_f * rms_norm(fc, p['norm_post_ffn'])
    return x, xc


def setup_inputs(seed: int = 0) -> dict:
    key = jax.random.key(seed)
    ks = jax.random.split(key, 32)

    def nrm(k, shape, scale):
        return jax.random.normal(k, shape, jnp.float32) * scale

    G, P, H = S5_GROUPS, S5_STATE, S5_GROUP
    lam_im0 = jnp.pi * jnp.arange(P, dtype=jnp.float32)
    return {
        'x': nrm(ks[0], (BATCH, SEQ, D_MODEL), 1.0),
        'c': nrm(ks[1], (BATCH, D_MODEL), 1.0),
        'ctx': nrm(ks[2], (BATCH, CTX_LEN, D_MODEL), 1.0),
        'c_ctx': nrm(ks[3], (D_MODEL,), 1.0),
        'w_ada': nrm(ks[4], (DEPTH, D_MODEL, 6 * D_MODEL), 0.5 * D_MODEL ** -0.5),
        'b_ada': nrm(ks[5], (DEPTH, 6 * D_MODEL), 0.01),
        'norm_pre_mix': 1.0 + nrm(ks[6], (DEPTH, D_MODEL), 0.05),
        'norm_post_mix': 1.0 + nrm(ks[7], (DEPTH, D_MODEL), 0.05),
        'norm_pre_ffn': 1.0 + nrm(ks[8], (DEPTH, D_MODEL), 0.05),
        'norm_post_ffn': 1.0 + nrm(ks[9], (DEPTH, D_MODEL), 0.05),
        'w_in': nrm(ks[10], (DEPTH, D_MODEL, IN_COLS), D_MODEL ** -0.5),
        's5_lam_re': -0.5 + nrm(ks[11], (DEPTH, 2, G, P), 0.01),
        's5_lam_im': lam_im0 + nrm(ks[12], (DEPTH, 2, G, P), 0.01),
        's5_log_dt': jax.random.uniform(ks[13], (DEPTH, 2, G), jnp.float32,
                                        math.log(S5_DT_MIN), math.log(S5_DT_MAX)),
        's5_b_re': nrm(ks[14], (DEPTH, 2, G, P, H), (2 * H) ** -0.5),
        's5_b_im': nrm(ks[15], (DEPTH, 2, G, P, H), (2 * H) ** -0.5),
        's5_c_re': nrm(ks[16], (DEPTH, 2, G, H, P), P ** -0.5),
        's5_c_im': nrm(ks[17], (DEPTH, 2, G, H, P), P ** -0.5),
        's5_d': nrm(ks[18], (DEPTH, S5_WIDTH), 1.0),
        'w_glu': nrm(ks[19], (DEPTH, S5_WIDTH, S5_WIDTH), S5_WIDTH ** -0.5),
        'da_lambda': nrm(ks[20], (DEPTH, 4, DA_HEAD_DIM), 0.1),
        'da_subln': 1.0 + nrm(ks[21], (DEPTH, DA_V_DIM), 0.05),
        'w_proj_a': nrm(ks[22], (DEPTH, S5_WIDTH, D_MODEL), S5_WIDTH ** -0.5),
        'w_proj_b': nrm(ks[23], (DEPTH, DA_WIDTH, D_MODEL), DA_WIDTH ** -0.5),
        'w_out': nrm(ks[24], (DEPTH, D_MODEL, D_MODEL), D_MODEL ** -0.5),
        'w_router': nrm(ks[25], (DEPTH, D_MODEL, N_EXPERTS), D_MODEL ** -0.5),
        'w_exp_gate': nrm(ks[26], (DEPTH, N_EXPERTS, D_MODEL, D_EXPERT), D_MODEL ** -0.5),
        'w_exp_up': nrm(ks[27], (DEPTH, N_EXPERTS, D_MODEL, D_EXPERT), D_MODEL ** -0.5),
        'w_exp_down': nrm(ks[28], (DEPTH, N_EXPERTS, D_EXPERT, D_MODEL), D_EXPERT ** -0.5),
    }


def reference(x, c, ctx, c_ctx, w_ada, b_ada, norm_pre_mix, norm_post_mix, norm_pre_ffn,
              norm_post_ffn, w_in, s5_lam_re, s5_lam_im, s5_log_dt, s5_b_re, s5_b_im,
              s5_c_re, s5_c_im, s5_d, w_glu, da_lambda, da_subln, w_proj_a, w_proj_b,
              w_out, w_router, w_exp_gate, w_exp_up, w_exp_down):
    seq_len = x.shape[1]
    rows = seq_len // GRID_W
    row = jnp.repeat(jnp.arange(rows), GRID_W).astype(jnp.float32)
    col = jnp.tile(jnp.arange(GRID_W), rows).astype(jnp.float32)
    inv_freq = ROPE_THETA ** (-jnp.arange(ROPE_PAIRS, dtype=jnp.float32) / ROPE_PAIRS)
    row_ang = row[:, None] * inv_freq[None, :]
    col_ang = col[:, None] * inv_freq[None, :]
    xc = ctx
    for i in range(DEPTH):
        p = {
            'norm_pre_mix': norm_pre_mix[i], 'norm_post_mix': norm_post_mix[i],
            'norm_pre_ffn': norm_pre_ffn[i], 'norm_post_ffn': norm_post_ffn[i],
            'w_in': w_in[i], 's5_lam_re': s5_lam_re[i], 's5_lam_im': s5_lam_im[i],
            's5_log_dt': s5_log_dt[i], 's5_b_re': s5_b_re[i], 's5_b_im': s5_b_im[i],
            's5_c_re': s5_c_re[i], 's5_c_im': s5_c_im[i], 's5_d': s5_d[i], 'w_glu': w_glu[i],
            'da_lambda': da_lambda[i], 'da_subln': da_subln[i], 'w_proj_a': w_proj_a[i],
            'w_proj_b': w_proj_b[i], 'w_out': w_out[i], 'w_router': w_router[i],
            'w_exp_gate': w_exp_gate[i], 'w_exp_up': w_exp_up[i], 'w_exp_down': w_exp_down[i],
        }
        mod_lat = (jax.nn.silu(c) @ w_ada[i] + b_ada[i])[:, None, :]
        mod_ctx = jax.nn.silu(c_ctx) @ w_ada[i] + b_ada[i]
        lam_init = 0.8 - 0.6 * math.exp(-0.3 * i)
        x, xc = hybrid_layer(x, xc, mod_lat, mod_ctx, p, row_ang, col_ang, lam_init,
                             need_ctx_out=(i < DEPTH - 1))
    return x
```

```python
import math
from contextlib import ExitStack
import numpy as np
import concourse.bass as bass
import concourse.mybir as mybir
from concourse.bass_utils import run_bass_kernel_spmd

F32 = mybir.dt.float32
BF16 = mybir.dt.bfloat16
I32 = mybir.dt.int32
AF = mybir.ActivationFunctionType
ALU = mybir.AluOpType
AX = mybir.AxisListType

D = 1024
L = 4096
LC = 256
NT = (L + LC) // 128
EPS = 1e-6
MAGIC = 12582912.0
TWO_PI = 2.0 * math.pi


class Buf:
    _n = 0

    def __init__(self, name, t=None):
        Buf._n += 1
        self.key = "b%d" % Buf._n
        self.name = name
        self.t = t
        self.w = None
        self.r = {}
        self.dsem = None
        self.dcnt = 0
        self.dw = {}
        self.dr = {}

    def __getitem__(self, idx):
        return self.t[idx]


class Sched:
    ENG = ("pe", "act", "dve", "pool", "sp")

    def __init__(self, nc, stack):
        self.nc = nc
        self.stack = stack
        self.eng = {"pe": nc.tensor, "act": nc.scalar, "dve": nc.vector, "pool": nc.gpsimd, "sp": nc.sync}
        self.sem = {}
        self.tick = {e: 0 for e in self.ENG}
        for e in self.ENG:
            self.sem[e] = stack.enter_context(nc.semaphore("sem_" + e))
        self.seen = {e: {} for e in self.ENG}
        self.n_dsem = 0
        self.ninst = {e: 0 for e in self.ENG}
        self.nwait = 0
        self.uid = 0
        self.live = []
        self.free_sems = []

    def sb(self, name, shape, dt, stack=None):
        self.uid += 1
        t = (stack or self.stack).enter_context(self.nc.sbuf_tensor("%s_%d" % (name, self.uid), list(shape), dt))
        b = Buf(name, t)
        self.live.append(b)
        return b

    def mark(self):
        return len(self.live)

    def release(self, mark):
        bufs = self.live[mark:]
        del self.live[mark:]
        for b in bufs:
            if b.dsem is not None:
                self._wait("sp", b.key, b.dsem, b.dcnt)
        self.barrier()
        for b in bufs:
            if b.dsem is not None:
                self.free_sems.append((b.dsem, b.dcnt))
                b.dsem = None

    def _dsem(self, b):
        if b.dsem is None:
            if self.free_sems:
                b.dsem, b.dcnt = self.free_sems.pop()
            else:
                b.dsem = self.stack.enter_context(self.nc.semaphore("ds%d" % self.n_dsem))
                self.n_dsem += 1
        return b.dsem

    def _wait(self, e, semkey, semh, val):
        if val <= 0:
            return
        if self.seen[e].get(semkey, 0) >= val:
            return
        self.eng[e].wait_ge(semh, val)
        self.seen[e][semkey] = val
        self.nwait += 1

    def _wait_eng(self, e, other, tick):
        if other == e and e == "pe":
            return
        self._wait(e, other, self.sem[other], tick)

    def _deps(self, e, reads, writes):
        for b in reads:
            if b.w is not None:
                self._wait_eng(e, b.w[0], b.w[1])
            for k, (sh, v) in b.dw.items():
                self._wait(e, k, sh, v)
        for b in writes:
            if b.w is not None:
                self._wait_eng(e, b.w[0], b.w[1])
            for oe, tk in b.r.items():
                self._wait_eng(e, oe, tk)
            for k, (sh, v) in b.dw.items():
                self._wait(e, k, sh, v)
            for k, (sh, v) in b.dr.items():
                self._wait(e, k, sh, v)

    def op(self, e, fn, reads=(), writes=()):
        self._deps(e, reads, writes)
        inst = fn(self.eng[e])
        self.tick[e] += 1
        inst.then_inc(self.sem[e], 1)
        tk = self.tick[e]
        for b in reads:
            b.r[e] = tk
        for b in writes:
            b.w = (e, tk)
            b.r = {}
            b.dw = {}
            b.dr = {}
        self.ninst[e] += 1
        return inst

    def dma(self, q, fn, reads, writes, semb, concurrent=False):
        if concurrent:
            self._deps(q, reads, [])
            for b in writes:
                if b.w is not None:
                    self._wait_eng(q, b.w[0], b.w[1])
                for oe, tk in b.r.items():
                    self._wait_eng(q, oe, tk)
                for k, (sh, v) in b.dr.items():
                    self._wait(q, k, sh, v)
        else:
            self._deps(q, reads, writes)
        inst = fn(self.eng[q])
        sem = self._dsem(semb)
        semb.dcnt += 16
        inst.then_inc(sem, 16)
        ev = (sem, semb.dcnt)
        k = semb.key
        for b in reads:
            b.dr[k] = ev
        for b in writes:
            if concurrent:
                b.dw[k] = ev
            else:
                b.w = None
                b.r = {}
                b.dr = {}
                b.dw = {k: ev}
        self.ninst[q] += 1
        return inst

    def wait_all(self, e, bufs):
        self._deps(e, [], bufs)

    def barrier(self):
        for e in self.ENG:
            for o in self.ENG:
                if o != e:
                    self._wait_eng(e, o, self.tick[o])


class Ring:
    def __init__(self, S, name, shape, dt, n, stack=None):
        self.bufs = [S.sb("%s%d" % (name, i), shape, dt, stack) for i in range(n)]
        self.i = 0

    def next(self):
        b = self.bufs[self.i % len(self.bufs)]
        self.i += 1
        return b


def build(stage=99, dbg=False):
    nc = bass.Bass("TRN2", target_bir_lowering=False)

    def din(name, shape, dt=F32):
        return nc.dram_tensor(name, list(shape), dt, kind="ExternalInput").ap()

    def dscr(name, shape, dt):
        return nc.dram_tensor(name, list(shape), dt, kind="Internal").ap()

    A = {}
    A["x"] = din("x", [L, D]); A["ctx"] = din("ctx", [LC, D])
    A["c"] = din("c", [1, D]); A["c_ctx"] = din("c_ctx", [1, D])
    A["w_ada"] = din("w_ada", [D, 6 * D]); A["b_ada"] = din("b_ada", [1, 6 * D])
    for n_ in ("norm_pre_mix", "norm_post_mix", "norm_pre_ffn", "norm_post_ffn"):
        A[n_] = din(n_, [1, D])
    A["w_in"] = din("w_in", [D, 4096])
    A["s5_lam_re"] = din("s5_lam_re", [64, 64]); A["s5_lam_im"] = din("s5_lam_im", [64, 64])
    A["s5_log_dt"] = din("s5_log_dt", [1, 64])
    A["s5_b_re"] = din("s5_b_re", [64, 64, 16]); A["s5_b_im"] = din("s5_b_im", [64, 64, 16])
    A["s5_c_re"] = din("s5_c_re", [2, 512, 64]); A["s5_c_im"] = din("s5_c_im", [2, 512, 64])
    A["s5_d"] = din("s5_d", [1, 512]); A["w_glu"] = din("w_glu", [512, 512])
    A["da_lambda"] = din("da_lambda", [1, 256]); A["da_subln"] = din("da_subln", [1, 128])
    A["w_proj_a"] = din("w_proj_a", [512, D]); A["w_proj_b"] = din("w_proj_b", [512, D])
    A["w_out"] = din("w_out", [D, D]); A["w_router"] = din("w_router", [D, 16])
    A["w_exp_gate"] = din("w_exp_gate", [16, D, 2048]); A["w_exp_up"] = din("w_exp_up", [16, D, 2048])
    A["w_exp_down"] = din("w_exp_down", [16, 2048, D])
    out_ap = nc.dram_tensor("out", [L, D], F32, kind="ExternalOutput").ap()

    U_d = dscr("U_d", [5, 128, 4096], BF16)
    QT_d = dscr("QT_d", [512, L], BF16)
    KT_d = dscr("KT_d", [512, LC + L], BF16)
    V_d = dscr("V_d", [LC + L, 512], BF16)
    SG_d = dscr("SG_d", [2048, L], BF16)
    YA_d = dscr("YA_d", [512, L], BF16)
    YB_d = dscr("YB_d", [512, L], BF16)
    X1_d = dscr("X1_d", [L, D], F32)
    H2_d = dscr("H2_d", [L, D], BF16)
    F_d = dscr("F_d", [L, D], F32)
    DB = {}
    for k_ in ("x", "ctx", "c", "c_ctx", "w_ada", "b_ada", "norm_pre_mix", "norm_post_mix", "norm_pre_ffn",
               "norm_post_ffn", "w_in", "s5p", "w_glu", "da", "w_proj", "w_out", "w_router", "w_exp",
               "U", "QT", "KT", "V", "SG", "YA", "YB", "X1", "H2", "F", "out", "dbg"):
        DB[k_] = Buf("D_" + k_)

    dbg_out = {}

    def dbg_tensor(name, shape, dt=F32):
        ap = nc.dram_tensor("dbg_" + name, list(shape), dt, kind="ExternalOutput").ap()
        dbg_out[name] = ap
        return ap

    with ExitStack() as top:
        S = Sched(nc, top)
        psum = top.enter_context(nc.psum_tensor("psum", [128, 4096], F32))
        PB = [Buf("bank%d" % i, psum[:, i * 512:(i + 1) * 512]) for i in range(8)]

        ident_bf = S.sb("ident_bf", [128, 128], BF16)
        ident_f = S.sb("ident_f", [128, 128], F32)
        iota_f = S.sb("iota_f", [128, 512], F32)
        pidx = S.sb("pidx", [128, 1], F32)
        ones_f = S.sb("ones_f", [128, 128], F32)
        ones_bf = S.sb("ones_bf", [128, 128], BF16)
        S.op("pool", lambda e: e.iota(iota_f[:], pattern=[[1, 512]], base=0, channel_multiplier=0,
                                      allow_small_or_imprecise_dtypes=True), [], [iota_f])
        S.op("pool", lambda e: e.iota(pidx[:], pattern=[[0, 1]], base=0, channel_multiplier=1,
                                      allow_small_or_imprecise_dtypes=True), [], [pidx])
        S.op("dve", lambda e: e.tensor_scalar(out=ident_bf[:], in0=iota_f[:, 0:128], scalar1=pidx[:, 0:1], scalar2=None,
                                              op0=ALU.is_equal), [iota_f, pidx], [ident_bf])
        S.op("dve", lambda e: e.tensor_scalar(out=ident_f[:], in0=iota_f[:, 0:128], scalar1=pidx[:, 0:1], scalar2=None,
                                              op0=ALU.is_equal), [iota_f, pidx], [ident_f])
        S.op("dve", lambda e: e.memset(ones_f[:], 1.0), [], [ones_f])
        S.op("dve", lambda e: e.memset(ones_bf[:], 1.0), [], [ones_bf])

        modc = S.sb("modc", [128, 4, 8], F32)
        ROWS_d = dscr("ROWS_d", [128, 4 * D], F32)
        DB["ROWS"] = Buf("D_ROWS")
        aff = S.sb("aff", [128, 32, 16], F32)

        mk0 = S.mark()
        with ExitStack() as ph:
            cc = S.sb("cc", [128, 8, 2], F32, ph)
            sc = S.sb("sc", [128, 8, 2], F32, ph)
            bcol = S.sb("bcol", [128, 2, 8], F32, ph)
            ncol = S.sb("ncol", [128, 8], F32, ph)
            brow = S.sb("brow", [1, 6 * D], F32, ph)
            nrow = S.sb("nrow", [1, 3, D], F32, ph)
            mrow = S.sb("mrow", [1, 4, D], F32, ph)
            wring = Ring(S, "wada", [128, 8, 512], F32, 2, ph)
            tmpc = S.sb("tmpc", [128, 4, 8], F32, ph)
            rows = S.sb("rows", [128, 4, D], F32, ph)
            S.dma("sp", lambda e: e.dma_start(out=cc[:, :, 0], in_=A["c"].rearrange("o (k p) -> p (o k)", p=128),
                                              allow_slow_non_contiguous=True), [DB["c"]], [cc], cc)
            S.dma("sp", lambda e: e.dma_start(out=cc[:, :, 1], in_=A["c_ctx"].rearrange("o (k p) -> p (o k)", p=128),
                                              allow_slow_non_contiguous=True), [DB["c_ctx"]], [cc], cc, concurrent=True)
            S.dma("sp", lambda e: e.dma_start(out=bcol[:, :, :], in_=A["b_ada"][:, 0:2048].rearrange("o (j k p) -> p (o j) k", p=128, k=8),
                                              allow_slow_non_contiguous=True), [DB["b_ada"]], [bcol], bcol)
            S.dma("sp", lambda e: e.dma_start(out=ncol[:, :], in_=A["norm_pre_mix"].rearrange("o (k p) -> p (o k)", p=128),
                                              allow_slow_non_contiguous=True), [DB["norm_pre_mix"]], [ncol], ncol)
            S.dma("sp", lambda e: e.dma_start(out=brow[:, :], in_=A["b_ada"]), [DB["b_ada"]], [brow], brow)
            for i_, n_ in enumerate(("norm_post_mix", "norm_pre_ffn", "norm_post_ffn")):
                S.dma("sp", lambda e, i_=i_, n_=n_: e.dma_start(out=nrow[:, i_, :], in_=A[n_]), [DB[n_]], [nrow], nrow,
                      concurrent=(i_ > 0))
            S.op("act", lambda e: e.activation(out=sc[:], in_=cc[:], func=AF.Silu), [cc], [sc])
            wv = A["w_ada"].rearrange("(k p) n -> p k n", p=128)
            pcol = PB[0]
            for blk in range(4):
                wt = wring.next()
                S.dma("sp", lambda e, wt=wt, blk=blk: e.dma_start(out=wt[:], in_=wv[:, :, blk * 512:(blk + 1) * 512]),
                      [DB["w_ada"]], [wt], wt)
                for ct in range(4):
                    col = blk * 4 + ct
                    for kt in range(8):
                        S.op("pe", lambda e, wt=wt, ct=ct, kt=kt, col=col: e.matmul(
                            pcol[:, col * 2:col * 2 + 2], lhsT=wt[:, kt, ct * 128:(ct + 1) * 128], rhs=sc[:, kt, :],
                            start=(kt == 0), stop=(kt == 7)), [wt, sc], [pcol])
            pv = pcol[:, 0:32].rearrange("p (j k t) -> p j k t", j=2, k=8)
            for t_ in range(2):
                S.op("dve", lambda e, t_=t_: e.tensor_tensor(out=modc[:, 2 * t_ + 1, :], in0=pv[:, 0, :, t_], in1=bcol[:, 0, :], op=ALU.add),
                     [pcol, bcol], [modc])
                S.op("dve", lambda e, t_=t_: e.scalar_tensor_tensor(out=tmpc[:, t_, :], in0=pv[:, 1, :, t_], scalar=1.0, in1=bcol[:, 1, :],
                                                                   op0=ALU.add, op1=ALU.add), [pcol, bcol], [tmpc])
                S.op("dve", lambda e, t_=t_: e.tensor_tensor(out=modc[:, 2 * t_, :], in0=tmpc[:, t_, :], in1=ncol[:, :], op=ALU.mult),
                     [tmpc, ncol], [modc])
            for ch in range(4):
                for half in range(2):
                    blk = (2 + ch) * 2 + half
                    wt = wring.next()
                    S.dma("sp", lambda e, wt=wt, blk=blk: e.dma_start(out=wt[:], in_=wv[:, :, blk * 512:(blk + 1) * 512]),
                          [DB["w_ada"]], [wt], wt)
                    pr = PB[1 + (blk % 2)]
                    for kt in range(8):
                        S.op("pe", lambda e, wt=wt, kt=kt, pr=pr: e.matmul(pr[0:1, :], lhsT=sc[:, kt, 0:1], rhs=wt[:, kt, :],
                                                                          start=(kt == 0), stop=(kt == 7)), [wt, sc], [pr])
                    S.op("dve", lambda e, pr=pr, ch=ch, half=half, blk=blk: e.tensor_tensor(
                        out=mrow[0:1, ch, half * 512:(half + 1) * 512], in0=pr[0:1, :], in1=brow[0:1, blk * 512:(blk + 1) * 512], op=ALU.add),
                        [pr, brow], [mrow])
            S.op("dve", lambda e: e.tensor_tensor(out=mrow[0:1, 0, :], in0=mrow[0:1, 0, :], in1=nrow[0:1, 0, :], op=ALU.mult), [mrow, nrow], [mrow])
            S.op("dve", lambda e: e.scalar_tensor_tensor(out=mrow[0:1, 2, :], in0=mrow[0:1, 2, :], scalar=1.0, in1=nrow[0:1, 1, :],
                                                         op0=ALU.add, op1=ALU.mult), [mrow, nrow], [mrow])
            S.op("dve", lambda e: e.tensor_tensor(out=mrow[0:1, 3, :], in0=mrow[0:1, 3, :], in1=nrow[0:1, 2, :], op=ALU.mult), [mrow, nrow], [mrow])
            for ri, mi in enumerate((0, 2, 1, 3)):
                for half in range(2):
                    pr = PB[3 + ((ri * 2 + half) % 2)]
                    S.op("pe", lambda e, pr=pr, mi=mi, half=half: e.matmul(pr[:, :], lhsT=ones_f[0:1, :], rhs=mrow[0:1, mi, half * 512:(half + 1) * 512],
                                                                           start=True, stop=True), [ones_f, mrow], [pr])
                    S.op("act", lambda e, pr=pr, ri=ri, half=half: e.activation(out=rows[:, ri, half * 512:(half + 1) * 512], in_=pr[:, :], func=AF.Copy),
                         [pr], [rows])
            S.dma("sp", lambda e: e.dma_start(out=ROWS_d, in_=rows[:].rearrange("p a b -> p (a b)")), [rows], [DB["ROWS"]], rows)
            if dbg and stage <= 1:
                d1 = dbg_tensor("modc", [128, 32]); d2 = dbg_tensor("rows", [128, 4 * D])
                S.dma("sp", lambda e: e.dma_start(out=d1, in_=modc[:].rearrange("p a b -> p (a b)")), [modc], [DB["dbg"]], modc, concurrent=True)
                S.dma("sp", lambda e: e.dma_start(out=d2, in_=rows[:].rearrange("p a b -> p (a b)")), [rows], [DB["dbg"]], rows, concurrent=True)
            S.barrier()
        S.release(mk0)
        if stage == 0:
            return finish(nc, S, DB, dbg_out)

        PHASES(nc, S, A, DB, PB, top, dict(ident_bf=ident_bf, ident_f=ident_f, iota_f=iota_f, pidx=pidx, ones_f=ones_f, ones_bf=ones_bf,
                                           modc=modc, ROWS_d=ROWS_d, aff=aff, out=out_ap, U_d=U_d, QT_d=QT_d, KT_d=KT_d, V_d=V_d, SG_d=SG_d,
                                           YA_d=YA_d, YB_d=YB_d, X1_d=X1_d, H2_d=H2_d, F_d=F_d), stage, dbg, dbg_tensor)
        return finish(nc, S, DB, dbg_out)


def finish(nc, S, DB, dbg_out):
    S.wait_all("sp", [DB["out"], DB["dbg"]])
    S.barrier()
    nc._dbg_out = dbg_out
    nc._stats = (dict(S.ninst), S.nwait, S.n_dsem)
    return nc


def PHASES(nc, S, A, DB, PB, top, K, stage, dbg, dbg_tensor):
    def run(fn):
        mk_ = S.mark()
        fn(nc, S, A, DB, PB, K, stage, dbg, dbg_tensor)
        S.release(mk_)
    if stage != 30:
        run(phase12)
    if stage <= 2:
        return
    if stage != 4:
        run(phase3)
    if stage in (3, 30):
        return
    run(phase4)
    if stage == 4:
        return
    run(phase5)
    if stage == 5:
        return
    run(phase678)
    if stage == 6:
        return


def phase12(nc, S, A, DB, PB, K, stage, dbg, dbg_tensor):
    ident_bf = K["ident_bf"]; modc = K["modc"]; iota_f = K["iota_f"]; pidx = K["pidx"]
    with ExitStack() as ph:
        hT = S.sb("hT", [128, 8, NT * 128], BF16, ph)
        cosT = S.sb("cosT", [128, L], BF16, ph)
        sinT = S.sb("sinT", [128, L], BF16, ph)
        with ExitStack() as ph0:
            fi = S.sb("fi", [128, 8], F32, ph0)
            wcol = S.sb("wcol", [128, 4], F32, ph0)
            rc = S.sb("rc", [128, 2, 64], F32, ph0)
            ang = S.sb("ang", [128, 64, 64], F32, ph0)
            t1 = S.sb("t1", [128, L], F32, ph0)
            t2 = S.sb("t2", [128, L], F32, ph0)

            def pfloor(dst, div, off):
                S.op("dve", lambda e: e.tensor_scalar(out=dst, in0=pidx[:, 0:1], scalar1=1.0 / div, scalar2=-off, op0=ALU.mult, op1=ALU.add), [pidx], [fi])
                S.op("dve", lambda e: e.tensor_scalar(out=dst, in0=dst, scalar1=MAGIC, scalar2=None, op0=ALU.add), [fi], [fi])
                S.op("dve", lambda e: e.tensor_scalar(out=dst, in0=dst, scalar1=-MAGIC, scalar2=None, op0=ALU.add), [fi], [fi])
            pfloor(fi[:, 2:3], 16.0, 0.46875)
            pfloor(fi[:, 3:4], 32.0, 0.484375)
            pfloor(fi[:, 4:5], 64.0, 0.4921875)
            S.op("dve", lambda e: e.scalar_tensor_tensor(out=fi[:, 0:1], in0=fi[:, 2:3], scalar=-16.0, in1=pidx[:, 0:1], op0=ALU.mult, op1=ALU.add), [fi, pidx], [fi])
            S.op("dve", lambda e: e.scalar_tensor_tensor(out=fi[:, 1:2], in0=fi[:, 4:5], scalar=-2.0, in1=fi[:, 3:4], op0=ALU.mult, op1=ALU.add), [fi], [fi])
            S.op("act", lambda e: e.activation(out=wcol[:, 0:1], in_=fi[:, 0:1], func=AF.Exp, scale=-math.log(10000.0) / 16.0), [fi], [wcol])
            S.op("dve", lambda e: e.tensor_tensor(out=wcol[:, 2:3], in0=wcol[:, 0:1], in1=fi[:, 1:2], op=ALU.mult), [wcol, fi], [wcol])
            S.op("dve", lambda e: e.tensor_tensor(out=wcol[:, 1:2], in0=wcol[:, 0:1], in1=wcol[:, 2:3], op=ALU.subtract), [wcol], [wcol])
            S.op("dve", lambda e: e.tensor_scalar(out=rc[:, 0, :], in0=iota_f[:, 0:64], scalar1=wcol[:, 1:2], scalar2=1.0 / TWO_PI,
                                                  op0=ALU.mult, op1=ALU.mult), [iota_f, wcol], [rc])
            S.op("dve", lambda e: e.tensor_scalar(out=rc[:, 1, :], in0=iota_f[:, 0:64], scalar1=wcol[:, 2:3], scalar2=1.0 / TWO_PI,
                                                  op0=ALU.mult, op1=ALU.mult), [iota_f, wcol], [rc])
            S.op("dve", lambda e: e.tensor_tensor(out=ang[:], in0=rc[:, 0, :].unsqueeze(2).to_broadcast([128, 64, 64]),
                                                  in1=rc[:, 1, :].unsqueeze(1).to_broadcast([128, 64, 64]), op=ALU.add), [rc], [ang])
            angf = ang[:].rearrange("p a b -> p (a b)")
            for tab, off in ((sinT, 0.0), (cosT, 0.25)):
                S.op("dve", lambda e, off=off: e.tensor_scalar(out=t1[:], in0=angf, scalar1=off, scalar2=MAGIC, op0=ALU.add, op1=ALU.add), [ang], [t1])
                S.op("dve", lambda e: e.tensor_scalar(out=t1[:], in0=t1[:], scalar1=-MAGIC, scalar2=None, op0=ALU.add), [t1], [t1])
                S.op("dve", lambda e, off=off: e.scalar_tensor_tensor(out=t2[:], in0=angf, scalar=off, in1=t1[:], op0=ALU.add, op1=ALU.subtract),
                     [ang, t1], [t2])
                S.op("act", lambda e, tab=tab: e.activation(out=tab[:], in_=t2[:], func=AF.Sin, scale=TWO_PI * 0.999999), [t2], [tab])
            S.barrier()

        xring = Ring(S, "xt", [128, D], F32, 3, ph)
        xnring = Ring(S, "xn", [128, D], BF16, 2, ph)
        junk = S.sb("junk", [128, D], BF16, ph)
        stat = S.sb("stat", [128, NT, 4], F32, ph)
        for i in range(NT):
            xt = xring.next(); xn = xnring.next()
            src = A["ctx"][i * 128:(i + 1) * 128, :] if i < 2 else A["x"][(i - 2) * 128:(i - 1) * 128, :]
            srcb = DB["ctx"] if i < 2 else DB["x"]
            S.dma("sp", lambda e, xt=xt, src=src: e.dma_start(out=xt[:], in_=src), [srcb], [xt], xt)
            S.op("act", lambda e, xt=xt, i=i: e.activation(out=junk[:], in_=xt[:], func=AF.Square, accum_out=stat[:, i, 0:1]), [xt], [junk, stat])
            S.op("act", lambda e, i=i: e.activation(out=stat[:, i, 1:2], in_=stat[:, i, 0:1], func=AF.Sqrt, scale=1.0 / D, bias=EPS), [stat], [stat])
            S.op("dve", lambda e, i=i: e.reciprocal(out=stat[:, i, 2:3], in_=stat[:, i, 1:2]), [stat], [stat])
            S.op("dve", lambda e, xt=xt, xn=xn, i=i: e.tensor_scalar(out=xn[:], in0=xt[:], scalar1=stat[:, i, 2:3], scalar2=None, op0=ALU.mult),
                 [xt, stat], [xn])
            pt = PB[i % 2]
            ptb = pt.t.bitcast(BF16)
            for dt_ in range(8):
                S.op("pe", lambda e, ptb=ptb, xn=xn, dt_=dt_: e.transpose(ptb[:, dt_ * 128:(dt_ + 1) * 128], xn[:, dt_ * 128:(dt_ + 1) * 128], ident_bf[:]),
                     [xn, ident_bf], [pt])
            mi = 2 if i < 2 else 0
            pv = ptb.rearrange("p (a b) -> p a b", a=8)
            hv = hT[:, :, i * 128:(i + 1) * 128]
            S.op("dve", lambda e, pv=pv, hv=hv, mi=mi: e.tensor_tensor(out=hv, in0=pv, in1=modc[:, mi, :].unsqueeze(2).to_broadcast([128, 8, 128]), op=ALU.mult),
                 [pt, modc], [hT])
            S.op("dve", lambda e, hv=hv, mi=mi: e.tensor_tensor(out=hv, in0=hv, in1=modc[:, mi + 1, :].unsqueeze(2).to_broadcast([128, 8, 128]), op=ALU.add),
                 [hT, modc], [hT])
        if dbg and stage == 1:
            d1 = dbg_tensor("hT", [128, 8 * NT * 128], BF16)
            S.dma("sp", lambda e: e.dma_start(out=d1, in_=hT[:].rearrange("p a b -> p (a b)")), [hT], [DB["dbg"]], hT, concurrent=True)
            S.barrier()
            return
        S.barrier()

        U_d = K["U_d"]; QT_d = K["QT_d"]; KT_d = K["KT_d"]; V_d = K["V_d"]; SG_d = K["SG_d"]
        wv = A["w_in"].rearrange("(k p) n -> p k n", p=128)
        wring = Ring(S, "win", [128, 8, 512], BF16, 2, ph)
        wsw = S.sb("wsw", [128, 8, 512], BF16, ph)
        oring = Ring(S, "o2", [128, 512], BF16, 4, ph)
        tring = Ring(S, "t32", [128, 512], F32, 4, ph)
        bank_i = [0]

        def pbank():
            b = PB[2 + (bank_i[0] % 6)]
            bank_i[0] += 1
            return b

        def load_w(c0):
            wt = wring.next()
            S.dma("pool", lambda e: e.dma_start(out=wt[:], in_=wv[:, :, c0:c0 + 512]), [DB["w_in"]], [wt], wt)
            return wt

        def tok_major(c0, dst_rows):
            wt = load_w(c0)
            for i in range(NT):
                pb = pbank()
                for kt in range(8):
                    S.op("pe", lambda e, pb=pb, kt=kt, i=i: e.matmul(pb[:, :], lhsT=hT[:, kt, i * 128:(i + 1) * 128], rhs=wt[:, kt, :],
                                                                    start=(kt == 0), stop=(kt == 7)), [hT, wt], [pb])
                ob = oring.next()
                S.op("act", lambda e, pb=pb, ob=ob: e.activation(out=ob[:], in_=pb[:, :], func=AF.Copy), [pb], [ob])
                for (dap, dbuf) in dst_rows(i):
                    S.dma("sp", lambda e, dap=dap, ob=ob: e.dma_start(out=dap, in_=ob[:]), [ob], [dbuf], ob, concurrent=True)

        wt_u = load_w(0)
        ucm_ring = Ring(S, "ucm", [128, 32, 8, 16], BF16, 2, ph)
        for ct in range(5):
            nchunk = 32 if ct == 0 else 128
            t0_ = 0 if ct == 0 else LC + (ct - 1) * 1024
            ucm = ucm_ring.next()
            for j in range(8):
                pb = pbank()
                for kt in range(8):
                    lh = hT[:, kt, t0_:t0_ + nchunk * 8].rearrange("p (c j) -> p c j", j=8)[:, :, j]
                    S.op("pe", lambda e, pb=pb, kt=kt, lh=lh, nchunk=nchunk: e.matmul(pb[0:nchunk, :], lhsT=lh, rhs=wt_u[:, kt, :], start=(kt == 0), stop=(kt == 7)),
                         [hT, wt_u], [pb])
                if j % 2 == 0:
                    S.op("act", lambda e, pb=pb, ucm=ucm, j=j, nchunk=nchunk: e.activation(out=ucm[0:nchunk, :, j, :], in_=pb[0:nchunk, :].rearrange("p (g h) -> p g h", h=16), func=AF.Copy),
                         [pb], [ucm])
                else:
                    S.op("dve", lambda e, pb=pb, ucm=ucm, j=j, nchunk=nchunk: e.tensor_copy(out=ucm[0:nchunk, :, j, :], in_=pb[0:nchunk, :].rearrange("p (g h) -> p g h", h=16)),
                         [pb], [ucm])
            S.dma("sp", lambda e, ucm=ucm, ct=ct, nchunk=nchunk: e.dma_start(out=U_d[ct, 0:nchunk, :], in_=ucm[0:nchunk, :, :, :].rearrange("p g j h -> p (g j h)")),
                  [ucm], [DB["U"]], ucm, concurrent=True)
        tok_major(1536, lambda i: [(V_d[i * 128:(i + 1) * 128, :], DB["V"])])

        def rope_block(c0, dstT, dbuf, with_ctx):
            wt = load_w(c0)
            wtv = wt[:].rearrange("p k (a two h) -> p k a two h", two=2, h=16)
            wsv = wsw[:].rearrange("p k (a two h) -> p k a two h", two=2, h=16)
            S.op("pool", lambda e: e.tensor_scalar(out=wsv[:, :, :, 0, :], in0=wtv[:, :, :, 1, :], scalar1=-1.0, scalar2=None, op0=ALU.mult), [wt], [wsw])
            S.op("pool", lambda e: e.tensor_copy(out=wsv[:, :, :, 1, :], in_=wtv[:, :, :, 0, :]), [wt], [wsw])
            for ft in range(4):
                if with_ctx:
                    pb = pbank()
                    for kt in range(8):
                        S.op("pe", lambda e, pb=pb, kt=kt, ft=ft: e.matmul(pb[:, 0:LC], lhsT=wt[:, kt, ft * 128:(ft + 1) * 128], rhs=hT[:, kt, 0:LC],
                                                                         start=(kt == 0), stop=(kt == 7)), [hT, wt], [pb])
                    ob = oring.next()
                    S.op("act", lambda e, pb=pb, ob=ob: e.activation(out=ob[:, 0:LC], in_=pb[:, 0:LC], func=AF.Copy), [pb], [ob])
                    S.dma("sp", lambda e, ob=ob, ft=ft: e.dma_start(out=dstT[ft * 128:(ft + 1) * 128, 0:LC], in_=ob[:, 0:LC]), [ob], [dbuf], ob, concurrent=True)
                coff = LC if with_ctx else 0
                for tb in range(8):
                    p1 = pbank(); p2 = pbank()
                    t0_ = LC + tb * 512
                    for (pp, ww) in ((p1, wt), (p2, wsw)):
                        for kt in range(8):
                            S.op("pe", lambda e, pp=pp, ww=ww, kt=kt, ft=ft, t0_=t0_: e.matmul(pp[:, :], lhsT=ww[:, kt, ft * 128:(ft + 1) * 128],
                                                                                              rhs=hT[:, kt, t0_:t0_ + 512], start=(kt == 0), stop=(kt == 7)),
                                 [hT, ww], [pp])
                    ta = tring.next(); tb_ = tring.next(); ob = oring.next()
                    S.op("dve", lambda e, p1=p1, ta=ta, tb=tb: e.tensor_tensor(out=ta[:], in0=p1[:, :], in1=cosT[:, tb * 512:(tb + 1) * 512], op=ALU.mult),
                         [p1, cosT], [ta])
                    S.op("dve", lambda e, p2=p2, tb_=tb_, tb=tb: e.tensor_tensor(out=tb_[:], in0=p2[:, :], in1=sinT[:, tb * 512:(tb + 1) * 512], op=ALU.mult),
                         [p2, sinT], [tb_])
                    S.op("pool", lambda e, ta=ta, tb_=tb_, ob=ob: e.tensor_tensor(out=ob[:], in0=ta[:], in1=tb_[:], op=ALU.add), [ta, tb_], [ob])
                    S.dma("sp", lambda e, ob=ob, ft=ft, tb=tb, coff=coff: e.dma_start(out=dstT[ft * 128:(ft + 1) * 128, coff + tb * 512:coff + (tb + 1) * 512], in_=ob[:]),
                          [ob], [dbuf], ob, concurrent=True)

        rope_block(512, QT_d, DB["QT"], False)
        rope_block(1024, KT_d, DB["KT"], True)

        for j in range(4):
            wt = load_w(2048 + j * 512)
            for ft in range(4):
                for tb in range(8):
                    pb = pbank()
                    t0_ = LC + tb * 512
                    for kt in range(8):
                        S.op("pe", lambda e, pb=pb, kt=kt, ft=ft, t0_=t0_, wt=wt: e.matmul(pb[:, :], lhsT=wt[:, kt, ft * 128:(ft + 1) * 128],
                                                                                          rhs=hT[:, kt, t0_:t0_ + 512], start=(kt == 0), stop=(kt == 7)),
                             [hT, wt], [pb])
                    ob = oring.next()
                    S.op("act", lambda e, pb=pb, ob=ob: e.activation(out=ob[:], in_=pb[:, :], func=AF.Sigmoid), [pb], [ob])
                    r0 = (j * 4 + ft) * 128
                    S.dma("sp", lambda e, ob=ob, r0=r0, tb=tb: e.dma_start(out=SG_d[r0:r0 + 128, tb * 512:(tb + 1) * 512], in_=ob[:]), [ob], [DB["SG"]], ob,
                          concurrent=True)
        S.barrier()
        if dbg and stage == 2:
            stg = Ring(S, "dstg", [128, 2176], BF16, 2, ph)
            for (nm, src, dbk, rows_, cols_) in (("V", V_d, "V", LC + L, 512), ("QT", QT_d, "QT", 512, L),
                                               ("KT", KT_d, "KT", 512, LC + L), ("SG", SG_d, "SG", 2048, L)):
                dd = dbg_tensor(nm, [rows_, cols_], BF16)
                for r0 in range(0, rows_, 128):
                    for c0 in range(0, cols_, 2176):
                        cw = min(2176, cols_ - c0)
                        sg = stg.next()
                        S.dma("sp", lambda e, sg=sg, src=src, r0=r0, c0=c0, cw=cw: e.dma_start(out=sg[:, 0:cw], in_=src[r0:r0 + 128, c0:c0 + cw]), [DB[dbk]], [sg], sg)
                        S.dma("sp", lambda e, sg=sg, dd=dd, r0=r0, c0=c0, cw=cw: e.dma_start(out=dd[r0:r0 + 128, c0:c0 + cw], in_=sg[:, 0:cw]), [sg], [DB["dbg"]], sg,
                              concurrent=True)
            S.barrier()


def phase3(nc, S, A, DB, PB, K, stage, dbg, dbg_tensor):
    ident_bf = K["ident_bf"]; ident_f = K["ident_f"]; iota_f = K["iota_f"]
    U_d = K["U_d"]; YA_d = K["YA_d"]
    YT_d = nc.dram_tensor("YT_d", [512, L], BF16, kind="Internal").ap()
    DB_YT = Buf("D_YT")
    NCH = 544
    with ExitStack() as ph:
        M_bf = S.sb("M_bf", [128, 32, 128], BF16, ph)
        W1_bf = S.sb("W1_bf", [128, 64, 2, 64], BF16, ph)
        W3_bf = S.sb("W3_bf", [64, 64, 2, 128], BF16, ph)
        PW = S.sb("PW", [64, 64, 2, 29], F32, ph)
        PWr = S.sb("PWr", [64, 32, 2, 16], F32, ph)
        TAUS = list(range(9)) + [8 * k for k in range(2, 17)]
        with ExitStack() as p0:
            nat = S.sb("nat", [64, 2, 64], F32, p0)
            lamT = S.sb("lamT", [64, 2, 64], F32, p0)
            dtb = S.sb("dtb", [64, 64], F32, p0)
            sm = S.sb("sm", [64, 8, 64], F32, p0)
            Bb = S.sb("Bb", [64, 2, 64, 16], F32, p0)
            XC = S.sb("XC", [64, 2, 64, 9, 16], BF16, p0)
            Dcol = S.sb("Dcol", [128, 32], F32, p0)
            pA = ExitStack()
            xr = S.sb("xr", [64, 64], F32, pA)
            an = S.sb("an", [64, 64], F32, pA)
            tau = S.sb("tau", [64, 29], F32, pA)
            PH = S.sb("PH", [64, 64, 29], F32, pA)
            Y1 = S.sb("Y1", [64, 64, 29], F32, pA)
            Y2 = S.sb("Y2", [64, 64, 29], F32, pA)
            MG = S.sb("MG", [64, 64, 29], F32, pA)
            S.dma("sp", lambda e: e.dma_start(out=nat[:, 0, :], in_=A["s5_lam_re"]), [DB["s5p"]], [nat], nat)
            S.dma("sp", lambda e: e.dma_start(out=nat[:, 1, :], in_=A["s5_lam_im"]), [DB["s5p"]], [nat], nat, concurrent=True)
            S.dma("sp", lambda e: e.dma_start(out=dtb[:], in_=A["s5_log_dt"].partition_broadcast(64)), [DB["s5p"]], [dtb], dtb)
            for s_ in range(8):
                S.dma("sp", lambda e, s_=s_: e.dma_start(out=Dcol[16 * s_:16 * s_ + 16, :], in_=A["s5_d"][0, :].rearrange("(g h) -> h g", h=16),
                                                         allow_slow_non_contiguous=True), [DB["s5p"]], [Dcol], Dcol, concurrent=(s_ > 0))
            pb = PB[0]
            for ri in range(2):
                S.op("pe", lambda e, ri=ri: e.transpose(pb[0:64, ri * 64:(ri + 1) * 64], nat[:, ri, :], ident_f[0:64, 0:64]), [nat, ident_f], [pb])
            S.op("dve", lambda e: e.tensor_copy(out=lamT[:].rearrange("p a b -> p (a b)"), in_=pb[0:64, 0:128]), [pb], [lamT])
            S.op("dve", lambda e: e.tensor_scalar(out=lamT[:, 0, :], in0=lamT[:, 0, :], scalar1=-1e-4, scalar2=None, op0=ALU.min), [lamT], [lamT])
            S.op("act", lambda e: e.activation(out=dtb[:], in_=dtb[:], func=AF.Exp), [dtb], [dtb])
            S.op("dve", lambda e: e.tensor_tensor(out=xr[:], in0=lamT[:, 0, :], in1=dtb[:], op=ALU.mult), [lamT, dtb], [xr])
            S.op("dve", lambda e: e.tensor_scalar(out=an[:], in0=lamT[:, 1, :], scalar1=dtb[:, 0:1] if False else 1.0 / TWO_PI, scalar2=None, op0=ALU.mult), [lamT], [an])
            S.op("dve", lambda e: e.tensor_tensor(out=an[:], in0=an[:], in1=dtb[:], op=ALU.mult), [an, dtb], [an])
            S.op("dve", lambda e: e.tensor_copy(out=tau[:, 0:9], in_=iota_f[0:64, 0:9]), [iota_f], [tau])
            S.op("dve", lambda e: e.tensor_scalar(out=tau[:, 9:24], in0=iota_f[0:64, 2:17], scalar1=8.0, scalar2=None, op0=ALU.mult), [iota_f], [tau])
            for j_ in range(5):
                S.op("dve", lambda e, j_=j_: e.memset(tau[:, 24 + j_:25 + j_], float(256 * (2 ** j_))), [], [tau])
            bc_dg = lambda t: t[:].unsqueeze(2).to_broadcast([64, 64, 29])
            bc_tau = tau[:].unsqueeze(1).to_broadcast([64, 64, 29])
            S.op("dve", lambda e: e.tensor_tensor(out=PH[:], in0=bc_dg(an), in1=bc_tau, op=ALU.mult), [an, tau], [PH])
            S.op("dve", lambda e: e.tensor_tensor(out=MG[:], in0=bc_dg(xr), in1=bc_tau, op=ALU.mult), [xr, tau], [MG])
            S.op("act", lambda e: e.activation(out=MG[:], in_=MG[:], func=AF.Exp), [MG], [MG])
            for ri, off in ((0, 0.25), (1, 0.0)):
                S.op("dve", lambda e, off=off: e.tensor_scalar(out=Y1[:], in0=PH[:], scalar1=off, scalar2=MAGIC, op0=ALU.add, op1=ALU.add), [PH], [Y1])
                S.op("dve", lambda e: e.tensor_scalar(out=Y1[:], in0=Y1[:], scalar1=-MAGIC, scalar2=None, op0=ALU.add), [Y1], [Y1])
                S.op("dve", lambda e, off=off: e.scalar_tensor_tensor(out=Y2[:], in0=PH[:], scalar=off, in1=Y1[:], op0=ALU.add, op1=ALU.subtract), [PH, Y1], [Y2])
                S.op("act", lambda e: e.activation(out=Y2[:], in_=Y2[:], func=AF.Sin, scale=TWO_PI * 0.999999), [Y2], [Y2])
                S.op("dve", lambda e, ri=ri: e.tensor_tensor(out=PW[:, :, ri, :], in0=Y2[:], in1=MG[:], op=ALU.mult), [Y2, MG], [PW])
            for k_ in range(16):
                S.op("dve", lambda e, k_=k_: e.tensor_copy(out=PWr[:, :, :, k_], in_=PW[:, 32:64, :, 8 + 15 - k_]), [PW], [PWr])
            S.barrier(); pA.close()
            lr = lamT[:, 0, :]; li = lamT[:, 1, :]
            lbr = PW[:, :, 0, 1]; lbi = PW[:, :, 1, 1]
            den, rden, nre, cre, cim, t_a, t_b = (sm[:, i, :] for i in range(7))
            S.op("dve", lambda e: e.tensor_tensor(out=den, in0=lr, in1=lr, op=ALU.mult), [lamT], [sm])
            S.op("dve", lambda e: e.tensor_tensor(out=t_a, in0=li, in1=li, op=ALU.mult), [lamT], [sm])
            S.op("dve", lambda e: e.tensor_tensor(out=den, in0=den, in1=t_a, op=ALU.add), [sm], [sm])
            S.op("dve", lambda e: e.reciprocal(out=rden, in_=den), [sm], [sm])
            S.op("dve", lambda e: e.tensor_scalar(out=nre, in0=lbr, scalar1=-1.0, scalar2=None, op0=ALU.add), [PW], [sm])
            S.op("dve", lambda e: e.tensor_tensor(out=t_a, in0=nre, in1=lr, op=ALU.mult), [sm, lamT], [sm])
            S.op("dve", lambda e: e.tensor_tensor(out=t_b, in0=lbi, in1=li, op=ALU.mult), [PW, lamT], [sm])
            S.op("dve", lambda e: e.tensor_tensor(out=t_a, in0=t_a, in1=t_b, op=ALU.add), [sm], [sm])
            S.op("dve", lambda e: e.tensor_tensor(out=cre, in0=t_a, in1=rden, op=ALU.mult), [sm], [sm])
            S.op("dve", lambda e: e.tensor_tensor(out=t_a, in0=lbi, in1=lr, op=ALU.mult), [PW, lamT], [sm])
            S.op("dve", lambda e: e.tensor_tensor(out=t_b, in0=nre, in1=li, op=ALU.mult), [sm, lamT], [sm])
            S.op("dve", lambda e: e.tensor_tensor(out=t_a, in0=t_a, in1=t_b, op=ALU.subtract), [sm], [sm])
            S.op("dve", lambda e: e.tensor_tensor(out=cim, in0=t_a, in1=rden, op=ALU.mult), [sm], [sm])
            bch = lambda t: t.unsqueeze(2).to_broadcast([64, 64, 16])
            pB = ExitStack()
            Bn = S.sb("Bn", [64, 2, 64, 16], F32, pB)
            tb1 = S.sb("tb1", [64, 64, 16], F32, pB)
            for ri, nm in enumerate(("s5_b_re", "s5_b_im")):
                S.dma("sp", lambda e, ri=ri, nm=nm: e.dma_start(out=Bn[:, ri, :, :], in_=A[nm].rearrange("a n h -> n a h")), [DB["s5p"]], [Bn], Bn,
                      concurrent=(ri > 0))
            S.op("dve", lambda e: e.tensor_tensor(out=Bb[:, 0, :, :], in0=Bn[:, 0, :, :], in1=bch(cre), op=ALU.mult), [Bn, sm], [Bb])
            S.op("dve", lambda e: e.tensor_tensor(out=tb1[:], in0=Bn[:, 1, :, :], in1=bch(cim), op=ALU.mult), [Bn, sm], [tb1])
            S.op("dve", lambda e: e.tensor_tensor(out=Bb[:, 0, :, :], in0=Bb[:, 0, :, :], in1=tb1[:], op=ALU.subtract), [Bb, tb1], [Bb])
            S.op("dve", lambda e: e.tensor_tensor(out=Bb[:, 1, :, :], in0=Bn[:, 1, :, :], in1=bch(cre), op=ALU.mult), [Bn, sm], [Bb])
            S.op("dve", lambda e: e.tensor_tensor(out=tb1[:], in0=Bn[:, 0, :, :], in1=bch(cim), op=ALU.mult), [Bn, sm], [tb1])
            S.op("dve", lambda e: e.tensor_tensor(out=Bb[:, 1, :, :], in0=Bb[:, 1, :, :], in1=tb1[:], op=ALU.add), [Bb, tb1], [Bb])
            S.barrier(); pB.close()
            pC = ExitStack()
            cnat = S.sb("cnat", [128, 2, 2, 4, 64], F32, pC)
            CT = S.sb("CT", [64, 2, 64, 16], F32, pC)
            tx = S.sb("tx", [64, 32, 9, 16], F32, pC)
            tx2 = S.sb("tx2", [64, 32, 9, 16], F32, pC)
            for ri, nm in enumerate(("s5_c_re", "s5_c_im")):
                for d_ in range(2):
                    S.dma("sp", lambda e, ri=ri, nm=nm, d_=d_: e.dma_start(out=cnat[:, ri, d_, :, :], in_=A[nm][d_].rearrange("(t p) n -> p t n", p=128)),
                          [DB["s5p"]], [cnat], cnat, concurrent=(ri + d_ > 0))
            for ri in range(2):
                for d_ in range(2):
                    pb = PB[1 + ((ri * 2 + d_) % 2)]
                    for t4 in range(4):
                        S.op("pe", lambda e, pb=pb, ri=ri, d_=d_, t4=t4: e.transpose(pb[0:64, t4 * 128:(t4 + 1) * 128], cnat[:, ri, d_, t4, :], ident_f[:]),
                             [cnat, ident_f], [pb])
                    S.op("act", lambda e, pb=pb, ri=ri, d_=d_: e.activation(out=CT[:, ri, d_ * 32:(d_ + 1) * 32, :].rearrange("p a h -> p (a h)"), in_=pb[0:64, :], func=AF.Copy),
                         [pb], [CT])
            for d_ in range(2):
                dsl = slice(d_ * 32, (d_ + 1) * 32)
                cb = lambda ri, dsl=dsl: CT[:, ri, dsl, :].unsqueeze(2).to_broadcast([64, 32, 9, 16])
                pwb = lambda ri, dsl=dsl: PW[:, dsl, ri, 0:9].unsqueeze(3).to_broadcast([64, 32, 9, 16])
                S.op("dve", lambda e, cb=cb, pwb=pwb: e.tensor_tensor(out=tx[:], in0=cb(0), in1=pwb(0), op=ALU.mult), [CT, PW], [tx])
                S.op("pool", lambda e, cb=cb, pwb=pwb: e.tensor_tensor(out=tx2[:], in0=cb(1), in1=pwb(1), op=ALU.mult), [CT, PW], [tx2])
                S.op("dve", lambda e, dsl=dsl: e.tensor_tensor(out=XC[:, 0, dsl, :, :], in0=tx[:], in1=tx2[:], op=ALU.subtract), [tx, tx2], [XC])
                S.op("dve", lambda e, cb=cb, pwb=pwb: e.tensor_tensor(out=tx[:], in0=cb(0), in1=pwb(1), op=ALU.mult), [CT, PW], [tx])
                S.op("pool", lambda e, cb=cb, pwb=pwb: e.tensor_tensor(out=tx2[:], in0=cb(1), in1=pwb(0), op=ALU.mult), [CT, PW], [tx2])
                S.op("dve", lambda e, dsl=dsl: e.scalar_tensor_tensor(out=XC[:, 1, dsl, :, :], in0=tx[:], scalar=-1.0, in1=tx2[:], op0=ALU.mult, op1=ALU.subtract), [tx, tx2], [XC])
            S.barrier(); pC.close()
            for ri in range(2):
                S.op("act", lambda e, ri=ri: e.activation(out=W3_bf[:, 0:32, ri, :].rearrange("p a (j h) -> p a j h", h=16), in_=XC[:, ri, 0:32, 1:9, :], func=AF.Copy), [XC], [W3_bf])
                for j in range(8):
                    S.op("pool", lambda e, ri=ri, j=j: e.tensor_copy(out=W3_bf[:, 32:64, ri, j * 16:(j + 1) * 16], in_=XC[:, ri, 32:64, 8 - j, :]), [XC], [W3_bf])
            pD = ExitStack()
            W1T = S.sb("W1T", [64, 32, 2, 128], BF16, pD)
            tq = S.sb("tq", [64, 32, 8, 16], F32, pD)
            tq2 = S.sb("tq2", [64, 32, 8, 16], F32, pD)
            PWj = S.sb("PWj", [64, 64, 2, 8], F32, pD)
            for j in range(8):
                S.op("dve", lambda e, j=j: e.tensor_copy(out=PWj[:, 0:32, :, j], in_=PW[:, 0:32, :, 7 - j]), [PW], [PWj])
                S.op("dve", lambda e, j=j: e.tensor_copy(out=PWj[:, 32:64, :, j], in_=PW[:, 32:64, :, j]), [PW], [PWj])
            for d_ in range(2):
                dsl = slice(d_ * 32, (d_ + 1) * 32)
                pj = lambda ri, dsl=dsl: PWj[:, dsl, ri, :].unsqueeze(3).to_broadcast([64, 32, 8, 16])
                bj = lambda ri, dsl=dsl: Bb[:, ri, dsl, :].unsqueeze(2).to_broadcast([64, 32, 8, 16])
                w1v = lambda ri: W1T[:, :, ri, :].rearrange("p a (j h) -> p a j h", h=16)
                S.op("dve", lambda e, pj=pj, bj=bj: e.tensor_tensor(out=tq[:], in0=pj(0), in1=bj(0), op=ALU.mult), [PWj, Bb], [tq])
                S.op("pool", lambda e, pj=pj, bj=bj: e.tensor_tensor(out=tq2[:], in0=pj(1), in1=bj(1), op=ALU.mult), [PWj, Bb], [tq2])
                S.op("dve", lambda e, w1v=w1v: e.tensor_tensor(out=w1v(0), in0=tq[:], in1=tq2[:], op=ALU.subtract), [tq, tq2], [W1T])
                S.op("dve", lambda e, pj=pj, bj=bj: e.tensor_tensor(out=tq[:], in0=pj(0), in1=bj(1), op=ALU.mult), [PWj, Bb], [tq])
                S.op("pool", lambda e, pj=pj, bj=bj: e.tensor_tensor(out=tq2[:], in0=pj(1), in1=bj(0), op=ALU.mult), [PWj, Bb], [tq2])
                S.op("dve", lambda e, w1v=w1v: e.tensor_tensor(out=w1v(1), in0=tq[:], in1=tq2[:], op=ALU.add), [tq, tq2], [W1T])
                for g in range(32):
                    dg = d_ * 32 + g
                    pb = PB[dg % 2]
                    pbb = pb.t.bitcast(BF16)
                    for ri in range(2):
                        S.op("pe", lambda e, pbb=pbb, g=g, ri=ri: e.transpose(pbb[:, ri * 64:(ri + 1) * 64], W1T[:, g, ri, :], ident_bf[0:64, 0:64]), [W1T, ident_bf], [pb])
                    if dg % 2 == 0:
                        S.op("act", lambda e, pbb=pbb, dg=dg: e.activation(out=W1_bf[:, dg, :, :].rearrange("p a b -> p (a b)"), in_=pbb[:, 0:128], func=AF.Copy), [pb], [W1_bf])
                    else:
                        S.op("dve", lambda e, pbb=pbb, dg=dg: e.tensor_copy(out=W1_bf[:, dg, :, :].rearrange("p a b -> p (a b)"), in_=pbb[:, 0:128]), [pb], [W1_bf])
            S.barrier(); pD.close()
            Bpad = [S.sb("Bpad%d" % i, [64, 2, 2, 240], BF16, p0) for i in range(2)]
            Xpad = [S.sb("Xpad%d" % i, [64, 2, 2, 240], BF16, p0) for i in range(2)]
            for i in range(2):
                S.op("pool", lambda e, i=i: e.memset(Bpad[i][:], 0.0), [], [Bpad[i]])
                S.op("pool", lambda e, i=i: e.memset(Xpad[i][:], 0.0), [], [Xpad[i]])
            for g in range(32):
                bp = Bpad[g % 2]; xp = Xpad[g % 2]
                S.op("pool", lambda e, bp=bp, g=g: e.tensor_copy(out=bp[:, 0, :, 112:128], in_=Bb[:, :, g, :]), [Bb], [bp])
                S.op("pool", lambda e, bp=bp, g=g: e.tensor_copy(out=bp[:, 1, :, 112:128], in_=Bb[:, :, 32 + g, :]), [Bb], [bp])
                S.op("act", lambda e, xp=xp, g=g: e.activation(out=xp[:, 0, :, 112:240].rearrange("p r (t h) -> p r t h", h=16), in_=XC[:, :, g, 0:8, :], func=AF.Copy), [XC], [xp])
                for i_ in range(8):
                    S.op("pool", lambda e, xp=xp, g=g, i_=i_: e.tensor_copy(out=xp[:, 1, :, i_ * 16:(i_ + 1) * 16], in_=XC[:, :, 32 + g, 7 - i_, :]), [XC], [xp])
                pb = PB[2 + (g % 2)]
                n_mm = 0
                for d_ in range(2):
                    for ri in range(2):
                        for s_ in range(8):
                            w0 = (7 - s_) * 16
                            S.op("pe", lambda e, pb=pb, bp=bp, xp=xp, d_=d_, ri=ri, w0=w0, n_mm=n_mm: e.matmul(
                                pb[:, 0:128], lhsT=bp[:, d_, ri, w0:w0 + 128], rhs=xp[:, d_, ri, w0:w0 + 128], start=(n_mm == 0), stop=(n_mm == 31)),
                                [bp, xp], [pb])
                            n_mm += 1
                S.op("dve", lambda e, pb=pb, g=g: e.scalar_tensor_tensor(out=M_bf[:, g, :], in0=ident_f[:], scalar=Dcol[:, g:g + 1], in1=pb[:, 0:128],
                                                                       op0=ALU.mult, op1=ALU.add), [pb, ident_f, Dcol], [M_bf])
            S.barrier()
        uT_all = S.sb("uT_all", [128, 32, NCH], BF16, ph)
        if dbg and stage == 30:
            for nm, t_, shp in (("M", M_bf, [128, 32 * 128]), ("W1", W1_bf, [128, 64 * 128])):
                dd = dbg_tensor(nm, shp, BF16)
                S.dma("sp", lambda e, dd=dd, t_=t_: e.dma_start(out=dd, in_=t_[:].rearrange("p a b -> p (a b)") if len(t_.t.shape) == 3 else t_[:].rearrange("p a b c -> p (a b c)")),
                      [t_], [DB["dbg"]], t_, concurrent=True)
            dd = dbg_tensor("W3", [64, 64 * 256], BF16)
            S.dma("sp", lambda e: e.dma_start(out=dd, in_=W3_bf[:].rearrange("p a b c -> p (a b c)")), [W3_bf], [DB["dbg"]], W3_bf, concurrent=True)
            dd2 = dbg_tensor("PW", [64, 64 * 48], F32)
            S.dma("sp", lambda e: e.dma_start(out=dd2, in_=PW[:].rearrange("p a b c -> p (a b c)")), [PW], [DB["dbg"]], PW, concurrent=True)
            S.barrier()
            return
        with ExitStack() as pU:
            uc = S.sb("uc", [128, 5, 4096], BF16, pU)
            S.dma("sp", lambda e: e.dma_start(out=uc[0:32, 0, :], in_=U_d[0, 0:32, :]), [DB["U"]], [uc], uc)
            for ct in range(4):
                S.dma("sp", lambda e, ct=ct: e.dma_start(out=uc[:, 1 + ct, :], in_=U_d[1 + ct, :, :]), [DB["U"]], [uc], uc, concurrent=True)
            for g in range(32):
                pb = PB[g % 4]
                pbb = pb.t.bitcast(BF16)
                S.op("pe", lambda e, pbb=pbb, g=g: e.transpose(pbb[:, 0:32], uc[0:32, 0, g * 128:(g + 1) * 128], ident_bf[0:32, 0:32]), [uc, ident_bf], [pb])
                for ct in range(4):
                    S.op("pe", lambda e, pbb=pbb, g=g, ct=ct: e.transpose(pbb[:, 32 + ct * 128:32 + (ct + 1) * 128], uc[:, 1 + ct, g * 128:(g + 1) * 128], ident_bf[:]),
                         [uc, ident_bf], [pb])
                if g % 2 == 0:
                    S.op("act", lambda e, pbb=pbb, g=g: e.activation(out=uT_all[:, g, :], in_=pbb[:, 0:NCH], func=AF.Copy), [pb], [uT_all])
                else:
                    S.op("dve", lambda e, pbb=pbb, g=g: e.tensor_copy(out=uT_all[:, g, :], in_=pbb[:, 0:NCH]), [pb], [uT_all])
            S.barrier()
        G = 4
        Sbuf = [[S.sb("S%d%d" % (d_, ri), [64, G, NCH], F32, ph) for ri in range(2)] for d_ in range(2)]
        Hbf = [[S.sb("H%d%d" % (d_, ri), [64, G, NCH + 2], BF16, ph) for ri in range(2)] for d_ in range(2)]
        Cy = [[S.sb("Cy%d%d" % (d_, ri), [64, G, 36], F32, ph) for ri in range(2)] for d_ in range(2)]
        tl = [[S.sb("tl%d%d" % (d_, i), [64, G, 34], F32, ph) for i in range(4)] for d_ in range(2)]
        Zb = [[S.sb("Zb%d%d" % (d_, ri), [64, G, 34], F32, ph) for ri in range(2)] for d_ in range(2)]
        tf = [[S.sb("tf%d%d" % (d_, i), [64, 34, 16], F32, ph) for i in range(2)] for d_ in range(2)]
        ytm = [S.sb("ytm%d" % i, [128, 4, 8, 128], BF16, ph) for i in range(1)]
        yTs = [S.sb("yTs%d" % i, [128, L], BF16, ph) for i in range(1)]
        for d_ in range(2):
            for ri in range(2):
                S.op("pool", lambda e, d_=d_, ri=ri: e.memset(Hbf[d_][ri][:], 0.0), [], [Hbf[d_][ri]])
                S.op("pool", lambda e, d_=d_, ri=ri: e.memset(Cy[d_][ri][:], 0.0), [], [Cy[d_][ri]])
        ENG_D = ("dve", "pool")
        for bt in range(8):
            for d_ in range(2):
                en = ENG_D[d_]
                Sr, Si = Sbuf[d_]
                for gl in range(G):
                    g = bt * G + gl
                    dg = d_ * 32 + g
                    for ri in range(2):
                        pa = PB[4 + ((gl * 2 + ri) % 2) * 2]
                        pb2 = PB[5 + ((gl * 2 + ri) % 2) * 2]
                        lw = W1_bf[:, dg, ri, :]
                        if d_ == 0:
                            S.op("pe", lambda e, pa=pa, lw=lw, g=g: e.matmul(pa[0:64, 0:272], lhsT=lw, rhs=uT_all[:, g, 0:272], start=True, stop=True), [W1_bf, uT_all], [pa])
                            S.op("pe", lambda e, pb2=pb2, lw=lw, g=g: e.matmul(pb2[0:64, 0:272], lhsT=lw, rhs=uT_all[:, g, 272:544], start=True, stop=True), [W1_bf, uT_all], [pb2])
                        else:
                            S.op("pe", lambda e, pa=pa, lw=lw, g=g: e.matmul(pa[0:64, 0:272], lhsT=lw, rhs=uT_all[:, g, 32:304], start=True, stop=True), [W1_bf, uT_all], [pa])
                            S.op("pe", lambda e, pb2=pb2, lw=lw, g=g: e.matmul(pb2[0:64, 0:240], lhsT=lw, rhs=uT_all[:, g, 304:544], start=True, stop=True), [W1_bf, uT_all], [pb2])
                            S.op("pe", lambda e, pb2=pb2, lw=lw, g=g: e.matmul(pb2[0:64, 240:272], lhsT=lw, rhs=uT_all[:, g, 0:32], start=True, stop=True), [W1_bf, uT_all], [pb2])
                        dst = (Sr, Si)[ri]
                        S.op("act", lambda e, pa=pa, dst=dst, gl=gl: e.activation(out=dst[:, gl, 0:272], in_=pa[0:64, 0:272], func=AF.Copy), [pa], [dst])
                        S.op("act", lambda e, pb2=pb2, dst=dst, gl=gl: e.activation(out=dst[:, gl, 272:544], in_=pb2[0:64, 0:272], func=AF.Copy), [pb2], [dst])
                gsl = slice(d_ * 32 + bt * G, d_ * 32 + bt * G + G)
                Ar = PW[:, gsl, 0, 8:9].to_broadcast([64, G, 34]); Ai = PW[:, gsl, 1, 8:9].to_broadcast([64, G, 34])
                Srv = Sr[:].rearrange("p g (s i) -> p g s i", i=16); Siv = Si[:].rearrange("p g (s i) -> p g s i", i=16)
                t1, t2, t3, t4 = tl[d_]
                steps = range(1, 16) if d_ == 0 else range(14, -1, -1)
                for i in steps:
                    ip = i - 1 if d_ == 0 else i + 1
                    S.op(en, lambda e, ip=ip: e.tensor_tensor(out=t1[:], in0=Srv[:, :, :, ip], in1=Ar, op=ALU.mult), [Sr, PW], [t1])
                    S.op(en, lambda e, ip=ip: e.tensor_tensor(out=t2[:], in0=Siv[:, :, :, ip], in1=Ai, op=ALU.mult), [Si, PW], [t2])
                    S.op(en, lambda e, ip=ip: e.tensor_tensor(out=t3[:], in0=Siv[:, :, :, ip], in1=Ar, op=ALU.mult), [Si, PW], [t3])
                    S.op(en, lambda e, ip=ip: e.tensor_tensor(out=t4[:], in0=Srv[:, :, :, ip], in1=Ai, op=ALU.mult), [Sr, PW], [t4])
                    S.op(en, lambda e: e.tensor_tensor(out=t1[:], in0=t1[:], in1=t2[:], op=ALU.subtract), [t1, t2], [t1])
                    S.op(en, lambda e: e.tensor_tensor(out=t3[:], in0=t3[:], in1=t4[:], op=ALU.add), [t3, t4], [t3])
                    S.op(en, lambda e, i=i: e.tensor_tensor(out=Srv[:, :, :, i], in0=Srv[:, :, :, i], in1=t1[:], op=ALU.add), [Sr, t1], [Sr])
                    S.op(en, lambda e, i=i: e.tensor_tensor(out=Siv[:, :, :, i], in0=Siv[:, :, :, i], in1=t3[:], op=ALU.add), [Si, t3], [Si])
                Cr, Ci = Cy[d_]
                Zr, Zi = Zb[d_]
                c1, c2, c3, c4 = tl[d_]
                if d_ == 0:
                    cur = (Srv[:, :, :, 15], Siv[:, :, :, 15]); cur_b = [Sr, Si]
                    cyv = (Cr[:, :, 1:35], Ci[:, :, 1:35])
                else:
                    cur = (Srv[:, :, :, 0], Siv[:, :, :, 0]); cur_b = [Sr, Si]
                    cyv = (Cr[:, :, 0:34], Ci[:, :, 0:34])
                zv = (Zr[:, :, :], Zi[:, :, :])
                for k_ in range(6):
                    sh = 1 << k_
                    ti_ = 23 + k_
                    n_ = 34 - sh
                    Bkr = PW[:, gsl, 0, ti_:ti_ + 1].to_broadcast([64, G, n_]); Bki = PW[:, gsl, 1, ti_:ti_ + 1].to_broadcast([64, G, n_])
                    dst = zv if k_ % 2 == 0 else cyv
                    dst_b = [Zr, Zi] if k_ % 2 == 0 else [Cr, Ci]
                    if d_ == 0:
                        src_sl = slice(0, n_); out_sl = slice(sh, 34); keep_sl = slice(0, sh)
                    else:
                        src_sl = slice(sh, 34); out_sl = slice(0, n_); keep_sl = slice(n_, 34)
                    xr_s = cur[0][:, :, src_sl]; xi_s = cur[1][:, :, src_sl]
                    S.op(en, lambda e, xr_s=xr_s, Bkr=Bkr, n_=n_: e.tensor_tensor(out=c1[:, :, 0:n_], in0=xr_s, in1=Bkr, op=ALU.mult), cur_b + [PW], [c1])
                    S.op(en, lambda e, xi_s=xi_s, Bki=Bki, n_=n_: e.tensor_tensor(out=c2[:, :, 0:n_], in0=xi_s, in1=Bki, op=ALU.mult), cur_b + [PW], [c2])
                    S.op(en, lambda e, xi_s=xi_s, Bkr=Bkr, n_=n_: e.tensor_tensor(out=c3[:, :, 0:n_], in0=xi_s, in1=Bkr, op=ALU.mult), cur_b + [PW], [c3])
                    S.op(en, lambda e, xr_s=xr_s, Bki=Bki, n_=n_: e.tensor_tensor(out=c4[:, :, 0:n_], in0=xr_s, in1=Bki, op=ALU.mult), cur_b + [PW], [c4])
                    S.op(en, lambda e, n_=n_: e.tensor_tensor(out=c1[:, :, 0:n_], in0=c1[:, :, 0:n_], in1=c2[:, :, 0:n_], op=ALU.subtract), [c1, c2], [c1])
                    S.op(en, lambda e, n_=n_: e.tensor_tensor(out=c3[:, :, 0:n_], in0=c3[:, :, 0:n_], in1=c4[:, :, 0:n_], op=ALU.add), [c3, c4], [c3])
                    S.op(en, lambda e, dst=dst, cur=cur, out_sl=out_sl, n_=n_: e.tensor_tensor(out=dst[0][:, :, out_sl], in0=cur[0][:, :, out_sl], in1=c1[:, :, 0:n_], op=ALU.add), cur_b + [c1], [dst_b[0]])
                    S.op(en, lambda e, dst=dst, cur=cur, out_sl=out_sl, n_=n_: e.tensor_tensor(out=dst[1][:, :, out_sl], in0=cur[1][:, :, out_sl], in1=c3[:, :, 0:n_], op=ALU.add), cur_b + [c3], [dst_b[1]])
                    S.op(en, lambda e, dst=dst, cur=cur, keep_sl=keep_sl: e.tensor_copy(out=dst[0][:, :, keep_sl], in_=cur[0][:, :, keep_sl]), cur_b, [dst_b[0]])
                    S.op(en, lambda e, dst=dst, cur=cur, keep_sl=keep_sl: e.tensor_copy(out=dst[1][:, :, keep_sl], in_=cur[1][:, :, keep_sl]), cur_b, [dst_b[1]])
                    cur = dst; cur_b = dst_b
                f1, f2 = tf[d_]
                Hr, Hi = Hbf[d_]
                for gl in range(G):
                    g = bt * G + gl
                    if d_ == 0:
                        Pr = PW[:, g, 0, 8:24].unsqueeze(1).to_broadcast([64, 34, 16]); Pi = PW[:, g, 1, 8:24].unsqueeze(1).to_broadcast([64, 34, 16])
                        cyr = Cr[:, gl, 0:34].unsqueeze(2).to_broadcast([64, 34, 16]); cyi = Ci[:, gl, 0:34].unsqueeze(2).to_broadcast([64, 34, 16])
                        hro = Hr[:, gl, 1:545].rearrange("p (s i) -> p s i", i=16); hio = Hi[:, gl, 1:545].rearrange("p (s i) -> p s i", i=16)
                    else:
                        Pr = PWr[:, g, 0, :].unsqueeze(1).to_broadcast([64, 34, 16]); Pi = PWr[:, g, 1, :].unsqueeze(1).to_broadcast([64, 34, 16])
                        cyr = Cr[:, gl, 1:35].unsqueeze(2).to_broadcast([64, 34, 16]); cyi = Ci[:, gl, 1:35].unsqueeze(2).to_broadcast([64, 34, 16])
                        hro = Hr[:, gl, 0:544].rearrange("p (s i) -> p s i", i=16); hio = Hi[:, gl, 0:544].rearrange("p (s i) -> p s i", i=16)
                    srv = Srv[:, gl, :, :]; siv = Siv[:, gl, :, :]
                    en_f = "dve" if (d_ == 1 and gl >= 2) else en
                    S.op(en_f, lambda e, Pr=Pr, cyr=cyr: e.tensor_tensor(out=f1[:], in0=Pr, in1=cyr, op=ALU.mult), [PW, PWr, Cr], [f1])
                    S.op(en_f, lambda e, Pi=Pi, cyi=cyi: e.tensor_tensor(out=f2[:], in0=Pi, in1=cyi, op=ALU.mult), [PW, PWr, Ci], [f2])
                    S.op(en_f, lambda e: e.tensor_tensor(out=f1[:], in0=f1[:], in1=f2[:], op=ALU.subtract), [f1, f2], [f1])
                    S.op(en_f, lambda e, hro=hro, srv=srv: e.tensor_tensor(out=hro, in0=srv, in1=f1[:], op=ALU.add), [Sr, f1], [Hr])
                    S.op(en_f, lambda e, Pr=Pr, cyi=cyi: e.tensor_tensor(out=f1[:], in0=Pr, in1=cyi, op=ALU.mult), [PW, PWr, Ci], [f1])
                    S.op(en_f, lambda e, Pi=Pi, cyr=cyr: e.tensor_tensor(out=f2[:], in0=Pi, in1=cyr, op=ALU.mult), [PW, PWr, Cr], [f2])
                    S.op(en_f, lambda e: e.tensor_tensor(out=f1[:], in0=f1[:], in1=f2[:], op=ALU.add), [f1, f2], [f1])
                    S.op(en_f, lambda e, hio=hio, siv=siv: e.tensor_tensor(out=hio, in0=siv, in1=f1[:], op=ALU.add), [Si, f1], [Hi])
            yt = ytm[0]
            for ct in range(4):
                pb = PB[ct % 4]
                for gl in range(G):
                    g = bt * G + gl
                    osl = pb[:, gl * 128:(gl + 1) * 128]
                    c0 = 32 + ct * 128
                    S.op("pe", lambda e, osl=osl, g=g, c0=c0: e.matmul(osl, lhsT=uT_all[:, g, c0:c0 + 128], rhs=M_bf[:, g, :], start=True, stop=False), [uT_all, M_bf], [pb])
                    for ri in range(2):
                        S.op("pe", lambda e, osl=osl, g=g, gl=gl, ri=ri, c0=c0: e.matmul(osl, lhsT=Hbf[0][ri][:, gl, c0:c0 + 128], rhs=W3_bf[:, g, ri, :], start=False, stop=False),
                             [Hbf[0][ri], W3_bf], [pb])
                    for ri in range(2):
                        S.op("pe", lambda e, osl=osl, g=g, gl=gl, ri=ri, ct=ct: e.matmul(osl, lhsT=Hbf[1][ri][:, gl, ct * 128 + 1:ct * 128 + 129], rhs=W3_bf[:, 32 + g, ri, :],
                                                                                         start=False, stop=(ri == 1)), [Hbf[1][ri], W3_bf], [pb])
                off = (bt % 2) * 64
                S.op("act", lambda e, pb=pb, yt=yt, ct=ct, off=off: e.activation(out=yt[:, ct, :, off:off + 64].rearrange("p j (g h) -> p j g h", h=16),
                                                                                in_=pb[:, :].rearrange("p (g j h) -> p j g h", g=4, h=16), func=AF.Gelu), [pb], [yt])
            if bt % 2 == 1:
                pair = bt // 2
                ys = yTs[0]
                for ct in range(4):
                    for jh in range(2):
                        pb = PB[4 + ((ct * 2 + jh) % 4)]
                        pbb = pb.t.bitcast(BF16)
                        for jj in range(4):
                            S.op("pe", lambda e, pbb=pbb, yt=yt, ct=ct, jh=jh, jj=jj: e.transpose(pbb[:, jj * 128:(jj + 1) * 128], yt[:, ct, jh * 4 + jj, :], ident_bf[:]),
                                 [yt, ident_bf], [pb])
                        S.op("dve", lambda e, pbb=pbb, ys=ys, ct=ct, jh=jh: e.tensor_copy(
                            out=ys[:, ct * 1024:(ct + 1) * 1024].rearrange("p (c j) -> p c j", j=8)[:, :, jh * 4:jh * 4 + 4],
                            in_=pbb[:, 0:512].rearrange("p (j c) -> p c j", j=4)), [pb], [ys])
                S.dma("sp", lambda e, ys=ys, pair=pair: e.dma_start(out=YT_d[pair * 128:(pair + 1) * 128, :], in_=ys[:]), [ys], [DB_YT], ys, concurrent=True)
        S.barrier()
    with ExitStack() as pg:
        yT = S.sb("yT", [128, 4, L], BF16, pg)
        wg = S.sb("wglu", [128, 4, 512], BF16, pg)
        oring = Ring(S, "yao", [128, 512], BF16, 3, pg)
        sring = Ring(S, "sgl", [128, 512], F32, 3, pg)
        S.dma("pool", lambda e: e.dma_start(out=wg[:], in_=A["w_glu"].rearrange("(k p) n -> p k n", p=128)), [DB["w_glu"]], [wg], wg)
        for kt in range(4):
            S.dma("sp", lambda e, kt=kt: e.dma_start(out=yT[:, kt, :], in_=YT_d[kt * 128:(kt + 1) * 128, :]), [DB_YT], [yT], yT, concurrent=(kt > 0))
        n_ = 0
        for tb in range(8):
            for co in range(4):
                pb = PB[n_ % 4]; n_ += 1
                for kt in range(4):
                    S.op("pe", lambda e, pb=pb, kt=kt, co=co, tb=tb: e.matmul(pb[:, :], lhsT=wg[:, kt, co * 128:(co + 1) * 128], rhs=yT[:, kt, tb * 512:(tb + 1) * 512],
                                                                            start=(kt == 0), stop=(kt == 3)), [wg, yT], [pb])
                sg = sring.next(); ob = oring.next()
                S.op("act", lambda e, pb=pb, sg=sg: e.activation(out=sg[:], in_=pb[:, :], func=AF.Sigmoid), [pb], [sg])
                S.op("dve", lambda e, sg=sg, ob=ob, co=co, tb=tb: e.tensor_tensor(out=ob[:], in0=sg[:], in1=yT[:, co, tb * 512:(tb + 1) * 512], op=ALU.mult), [sg, yT], [ob])
                S.dma("sp", lambda e, ob=ob, co=co, tb=tb: e.dma_start(out=YA_d[co * 128:(co + 1) * 128, tb * 512:(tb + 1) * 512], in_=ob[:]), [ob], [DB["YA"]], ob, concurrent=True)
        S.barrier()
        if dbg and stage == 3:
            dd = dbg_tensor("YA", [512, L], BF16)
            stg = Ring(S, "dstg3", [128, L], BF16, 2, pg)
            for r0 in range(0, 512, 128):
                sg_ = stg.next()
                S.dma("sp", lambda e, sg_=sg_, r0=r0: e.dma_start(out=sg_[:], in_=YA_d[r0:r0 + 128, :]), [DB["YA"]], [sg_], sg_)
                S.dma("sp", lambda e, sg_=sg_, r0=r0: e.dma_start(out=dd[r0:r0 + 128, :], in_=sg_[:]), [sg_], [DB["dbg"]], sg_, concurrent=True)
            S.barrier()


def phase4(nc, S, A, DB, PB, K, stage, dbg, dbg_tensor):
    ident_bf = K["ident_bf"]; ones_f = K["ones_f"]; ones_bf = K["ones_bf"]
    QT_d = K["QT_d"]; KT_d = K["KT_d"]; V_d = K["V_d"]; YB_d = K["YB_d"]
    NK = NT
    with ExitStack() as ph:
        KT = S.sb("KT", [128, 4, LC + L], BF16, ph)
        Vp = S.sb("Vp", [128, NK, 4, 128], BF16, ph)
        lamr = S.sb("lamr", [1, 264], F32, ph)
        lamc = S.sb("lamc", [128, 1], F32, ph)
        subw = S.sb("subw", [128, 128], F32, ph)
        qring = Ring(S, "qtb", [128, 4, 512], BF16, 2, ph)
        pring = Ring(S, "pT", [128, 512], BF16, 6, ph)
        acc = S.sb("acc", [128, 4, 128], F32, ph)
        st4 = S.sb("st4", [128, 4, 8], F32, ph)
        ybt = Ring(S, "ybt", [128, 4, 128], BF16, 2, ph)
        ybo = Ring(S, "ybo", [128, 512], BF16, 3, ph)
        junk = S.sb("junk4", [128, 128], F32, ph)
        zeros_bf = S.sb("zeros4", [128, 128], BF16, ph)
        S.op("pool", lambda e: e.memset(zeros_bf[:], 0.0), [], [zeros_bf])
        for hd in range(4):
            S.dma("sp", lambda e, hd=hd: e.dma_start(out=KT[:, hd, :], in_=KT_d[hd * 128:(hd + 1) * 128, :]), [DB["KT"]], [KT], KT, concurrent=(hd > 0))
        for kt in range(NK):
            S.dma("sp", lambda e, kt=kt: e.dma_start(out=Vp[:, kt, :, 0:128], in_=V_d[kt * 128:(kt + 1) * 128, :].rearrange("p (h d) -> p h d", h=4)), [DB["V"]], [Vp], Vp,
                  concurrent=(kt > 0))
        S.dma("sp", lambda e: e.dma_start(out=lamr[:, 0:256], in_=A["da_lambda"]), [DB["da"]], [lamr], lamr)
        S.dma("sp", lambda e: e.dma_start(out=subw[:], in_=A["da_subln"].partition_broadcast(128)), [DB["da"]], [subw], subw)
        lv = lamr[0:1, 0:256].rearrange("p (a b d) -> p a b d", a=2, b=2)
        S.op("dve", lambda e: e.tensor_tensor(out=lv[:, :, 0, :], in0=lv[:, :, 0, :], in1=lv[:, :, 1, :], op=ALU.mult), [lamr], [lamr])
        S.op("dve", lambda e: e.reduce_sum(out=lamr[0:1, 256:258], in_=lv[:, :, 0, :], axis=AX.X), [lamr], [lamr])
        S.op("act", lambda e: e.activation(out=lamr[0:1, 256:258], in_=lamr[0:1, 256:258], func=AF.Exp), [lamr], [lamr])
        S.op("dve", lambda e: e.scalar_tensor_tensor(out=lamr[0:1, 258:259], in0=lamr[0:1, 256:257], scalar=0.2, in1=lamr[0:1, 257:258], op0=ALU.add, op1=ALU.subtract), [lamr], [lamr])
        S.op("dve", lambda e: e.tensor_scalar(out=lamr[0:1, 259:260], in0=lamr[0:1, 258:259], scalar1=-1.0, scalar2=None, op0=ALU.mult), [lamr], [lamr])
        pl = PB[3]
        S.op("pe", lambda e: e.matmul(pl[:, 0:1], lhsT=ones_f[0:1, :], rhs=lamr[0:1, 259:260], start=True, stop=True), [ones_f, lamr], [pl])
        S.op("dve", lambda e: e.tensor_copy(out=lamc[:], in_=pl[:, 0:1]), [pl], [lamc])
        S.op("dve", lambda e: e.tensor_scalar(out=subw[:], in0=subw[:], scalar1=0.8, scalar2=None, op0=ALU.mult), [subw], [subw])

        subcol = S.sb("subcol", [128, 1], F32, ph)
        S.dma("sp", lambda e: e.dma_start(out=subcol[:], in_=A["da_subln"].rearrange("o d -> d o"), allow_slow_non_contiguous=True), [DB["da"]], [subcol], subcol)
        S.op("dve", lambda e: e.tensor_scalar(out=subcol[:], in0=subcol[:], scalar1=0.8, scalar2=None, op0=ALU.mult), [subcol], [subcol])
        r1r = Ring(S, "r1_4", [128, 512], F32, 3, ph); r2r = Ring(S, "r2_4", [128, 512], F32, 3, ph)
        sqr = Ring(S, "sq_4", [128, 512], BF16, 3, ph)
        sT_i = [0]
        deferred = []

        def sbank():
            b = PB[sT_i[0] % 4]
            sT_i[0] += 1
            return b
        for qb in range(8):
            qt_b = qring.next()
            S.dma("sp", lambda e, qt_b=qt_b, qb=qb: e.dma_start(out=qt_b[:], in_=QT_d[:, qb * 512:(qb + 1) * 512].rearrange("(h p) t -> p h t", p=128)), [DB["QT"]], [qt_b], qt_b)
            for hd in range(4):
                OV = (PB[4], PB[5]); DEN = (PB[6], PB[7])
                pts = {}

                def score(kt, cp):
                    sT = sbank()
                    psl = slice(cp * 64, (cp + 1) * 64)
                    S.op("pe", lambda e, sT=sT, kt=kt, psl=psl: e.matmul(sT[:, :], lhsT=KT[psl, hd, kt * 128:(kt + 1) * 128], rhs=qt_b[psl, hd, :], start=True, stop=True), [KT, qt_b], [sT])
                    pT = pring.next()
                    S.op("act", lambda e, sT=sT, pT=pT: e.activation(out=pT[:], in_=sT[:, :], func=AF.Exp, scale=0.125), [sT], [pT])
                    pts[(kt, cp)] = pT
                score(0, 0); score(0, 1)
                for kt in range(NK):
                    if kt + 1 < NK:
                        score(kt + 1, 0); score(kt + 1, 1)
                    if kt == 6 and deferred:
                        deferred.pop(0)()
                    p0 = pts.pop((kt, 0)); p1 = pts.pop((kt, 1))
                    fl = dict(start=(kt == 0), stop=(kt == NK - 1))
                    S.op("pe", lambda e, p0=p0, kt=kt, fl=fl: e.matmul(OV[0][:, :], lhsT=Vp[:, kt, hd, 0:128], rhs=p0[:], **fl), [Vp, p0], [OV[0]])
                    S.op("pe", lambda e, p1=p1, kt=kt, fl=fl: e.matmul(OV[1][:, :], lhsT=Vp[:, kt, hd, 0:128], rhs=p1[:], **fl), [Vp, p1], [OV[1]])
                    S.op("pe", lambda e, p0=p0, fl=fl: e.matmul(DEN[0][:, :], lhsT=ones_bf[:], rhs=p0[:], **fl), [ones_bf, p0], [DEN[0]])
                    S.op("pe", lambda e, p1=p1, fl=fl: e.matmul(DEN[1][:, :], lhsT=ones_bf[:], rhs=p1[:], **fl), [ones_bf, p1], [DEN[1]])
                r1 = r1r.next(); r2 = r2r.next(); sq = sqr.next(); yo = ybo.next()
                S.op("dve", lambda e, r1=r1: e.reciprocal(out=r1[:], in_=DEN[0][:, :]), [DEN[0]], [r1])
                S.op("dve", lambda e, r2=r2: e.reciprocal(out=r2[:], in_=DEN[1][:, :]), [DEN[1]], [r2])
                S.op("dve", lambda e, r1=r1: e.tensor_tensor(out=r1[:], in0=OV[0][:, :], in1=r1[:], op=ALU.mult), [OV[0], r1], [r1])
                S.op("dve", lambda e, r2=r2: e.tensor_tensor(out=r2[:], in0=OV[1][:, :], in1=r2[:], op=ALU.mult), [OV[1], r2], [r2])
                S.op("dve", lambda e, r1=r1, r2=r2: e.scalar_tensor_tensor(out=r1[:], in0=r2[:], scalar=lamc[:, 0:1], in1=r1[:], op0=ALU.mult, op1=ALU.add), [r1, r2, lamc], [r1])
                S.op("act", lambda e, r1=r1, sq=sq: e.activation(out=sq[:], in_=r1[:], func=AF.Square), [r1], [sq])

                def tail(r1=r1, r2=r2, sq=sq, yo=yo, hd=hd, qb=qb):
                    pss = sbank()
                    S.op("pe", lambda e: e.matmul(pss[:, :], lhsT=ones_bf[:], rhs=sq[:], start=True, stop=True), [ones_bf, sq], [pss])
                    S.op("act", lambda e: e.activation(out=r2[:], in_=pss[:, :], func=AF.Sqrt, scale=1.0 / 128.0, bias=EPS), [pss], [r2])
                    S.op("dve", lambda e: e.reciprocal(out=r2[:], in_=r2[:]), [r2], [r2])
                    S.op("dve", lambda e: e.scalar_tensor_tensor(out=yo[:], in0=r1[:], scalar=subcol[:, 0:1], in1=r2[:], op0=ALU.mult, op1=ALU.mult), [r1, r2, subcol], [yo])
                    S.dma("sp", lambda e: e.dma_start(out=YB_d[hd * 128:(hd + 1) * 128, qb * 512:(qb + 1) * 512], in_=yo[:]), [yo], [DB["YB"]], yo, concurrent=True)
                deferred.append(tail)
        while deferred:
            deferred.pop(0)()
        S.barrier()
        if dbg and stage == 4:
            dd = dbg_tensor("YB", [512, L], BF16)
            stg = Ring(S, "dstg4", [128, L], BF16, 2, ph)
            for r0 in range(0, 512, 128):
                sg_ = stg.next()
                S.dma("sp", lambda e, sg_=sg_, r0=r0: e.dma_start(out=sg_[:], in_=YB_d[r0:r0 + 128, :]), [DB["YB"]], [sg_], sg_)
                S.dma("sp", lambda e, sg_=sg_, r0=r0: e.dma_start(out=dd[r0:r0 + 128, :], in_=sg_[:]), [sg_], [DB["dbg"]], sg_, concurrent=True)
            S.barrier()


def phase5(nc, S, A, DB, PB, K, stage, dbg, dbg_tensor):
    ident_f = K["ident_f"]; aff = K["aff"]
    YA_d = K["YA_d"]; YB_d = K["YB_d"]; SG_d = K["SG_d"]; X1_d = K["X1_d"]; H2_d = K["H2_d"]; ROWS_d = K["ROWS_d"]
    with ExitStack() as ph:
        wpa = S.sb("wpa", [128, 4, D], BF16, ph)
        wpb = S.sb("wpb", [128, 4, D], BF16, ph)
        wout = S.sb("wout", [128, 8, D], BF16, ph)
        wr = S.sb("wr", [128, 8, 16], F32, ph)
        rows = S.sb("rows5", [128, 4, D], F32, ph)
        S.dma("pool", lambda e: e.dma_start(out=wpa[:], in_=A["w_proj_a"].rearrange("(k p) n -> p k n", p=128)), [DB["w_proj"]], [wpa], wpa)
        S.dma("pool", lambda e: e.dma_start(out=wpb[:], in_=A["w_proj_b"].rearrange("(k p) n -> p k n", p=128)), [DB["w_proj"]], [wpb], wpb)
        S.dma("pool", lambda e: e.dma_start(out=wout[:], in_=A["w_out"].rearrange("(k p) n -> p k n", p=128)), [DB["w_out"]], [wout], wout)
        S.dma("sp", lambda e: e.dma_start(out=wr[:], in_=A["w_router"].rearrange("(k p) n -> p k n", p=128)), [DB["w_router"]], [wr], wr)
        S.dma("sp", lambda e: e.dma_start(out=rows[:].rearrange("p a b -> p (a b)"), in_=ROWS_d), [DB["ROWS"]], [rows], rows)
        yar = Ring(S, "ya5", [128, 4, 512], BF16, 2, ph)
        ybr = Ring(S, "yb5", [128, 4, 512], BF16, 2, ph)
        sgr = Ring(S, "sg5", [128, 16, 512], BF16, 2, ph)
        mT = Ring(S, "mT", [128, 8, 512], BF16, 2, ph)
        t1r = Ring(S, "t1_5", [128, 512], F32, 3, ph)
        t2r = Ring(S, "t2_5", [128, 512], F32, 3, ph)
        xr_ = Ring(S, "x5", [128, D], F32, 2, ph)
        x1r = Ring(S, "x1_5", [128, D], F32, 2, ph)
        h2r = Ring(S, "h2f", [128, D], F32, 5, ph)
        h2br = Ring(S, "h2b", [128, D], BF16, 2, ph)
        h2Tr = Ring(S, "h2T", [128, 8, 128], F32, 2, ph)
        junk = S.sb("junk5", [128, D], BF16, ph)
        st_ring = Ring(S, "st5", [128, 1, 12], F32, 8, ph)
        ex_ring = Ring(S, "ex5", [128, 16], F32, 3, ph)
        junk2 = S.sb("junk5b", [128, D], BF16, ph)
        def router_part(ti, h2, h2T, st):
            ex = ex_ring.next()
            for dt_ in range(8):
                pbk = PB[6 + dt_ // 4]
                S.op("pe", lambda e, pbk=pbk, dt_=dt_, h2=h2: e.transpose(pbk[:, (dt_ % 4) * 128:(dt_ % 4 + 1) * 128], h2[:, dt_ * 128:(dt_ + 1) * 128], ident_f[:]), [h2, ident_f], [pbk])
            S.op("act", lambda e, h2T=h2T: e.activation(out=h2T[:, 0:4, :].rearrange("p a b -> p (a b)"), in_=PB[6][:, :], func=AF.Copy), [PB[6]], [h2T])
            S.op("dve", lambda e, h2T=h2T: e.tensor_copy(out=h2T[:, 4:8, :].rearrange("p a b -> p (a b)"), in_=PB[7][:, :]), [PB[7]], [h2T])
            pl = PB[6]
            for dt_ in range(8):
                S.op("pe", lambda e, dt_=dt_, h2T=h2T: e.matmul(pl[:, 0:16], lhsT=h2T[:, dt_, :], rhs=wr[:, dt_, :], start=(dt_ == 0), stop=(dt_ == 7)), [h2T, wr], [pl])
            S.op("dve", lambda e, ti=ti: e.reduce_max(out=st[:, 0, 8:9], in_=pl[:, 0:16], axis=AX.X), [pl], [st])
            S.op("dve", lambda e, ti=ti: e.tensor_scalar(out=st[:, 0, 8:9], in0=st[:, 0, 8:9], scalar1=-1.0, scalar2=None, op0=ALU.mult), [st], [st])
            S.op("act", lambda e, ti=ti: e.activation(out=ex[:], in_=pl[:, 0:16], func=AF.Exp, bias=st[:, 0, 8:9], scale=1.0, accum_out=st[:, 0, 9:10]), [pl, st], [ex, st])
            S.op("dve", lambda e, ti=ti: e.reciprocal(out=st[:, 0, 10:11], in_=st[:, 0, 9:10]), [st], [st])
            S.op("dve", lambda e, ti=ti: e.tensor_scalar(out=aff[:, ti, :], in0=ex[:], scalar1=st[:, 0, 10:11], scalar2=None, op0=ALU.mult), [ex, st], [aff])
        pending = []
        blk_bufs = {}

        def load_block(tb):
            if tb >= 8:
                return
            ya = yar.next(); yb = ybr.next(); sg = sgr.next()
            tsl = slice(tb * 512, (tb + 1) * 512)
            S.dma("sp", lambda e: e.dma_start(out=ya[:], in_=YA_d[:, tsl].rearrange("(k p) t -> p k t", p=128)), [DB["YA"]], [ya], ya)
            S.dma("sp", lambda e: e.dma_start(out=yb[:], in_=YB_d[:, tsl].rearrange("(k p) t -> p k t", p=128)), [DB["YB"]], [yb], yb)
            S.dma("sp", lambda e: e.dma_start(out=sg[:], in_=SG_d[:, tsl].rearrange("(k p) t -> p k t", p=128)), [DB["SG"]], [sg], sg)
            blk_bufs[tb] = (ya, yb, sg)
        load_block(0)
        mo_i = [0]
        for tb in range(8):
            load_block(tb + 1)
            ya, yb, sg = blk_bufs.pop(tb)
            m_ = mT.next()
            for dm in range(8):
                pa = PB[0]; pb = PB[1]
                for kt in range(4):
                    S.op("pe", lambda e, pa=pa, kt=kt, dm=dm, ya=ya: e.matmul(pa[:, :], lhsT=wpa[:, kt, dm * 128:(dm + 1) * 128], rhs=ya[:, kt, :], start=(kt == 0), stop=(kt == 3)), [wpa, ya], [pa])
                for kt in range(4):
                    S.op("pe", lambda e, pb=pb, kt=kt, dm=dm, yb=yb: e.matmul(pb[:, :], lhsT=wpb[:, kt, dm * 128:(dm + 1) * 128], rhs=yb[:, kt, :], start=(kt == 0), stop=(kt == 3)), [wpb, yb], [pb])
                t1 = t1r.next(); t2 = t2r.next()
                S.op("dve", lambda e, pa=pa, t1=t1, sg=sg, dm=dm: e.tensor_tensor(out=t1[:], in0=pa[:, :], in1=sg[:, dm, :], op=ALU.mult), [pa, sg], [t1])
                S.op("dve", lambda e, pb=pb, t2=t2, sg=sg, dm=dm: e.tensor_tensor(out=t2[:], in0=pb[:, :], in1=sg[:, 8 + dm, :], op=ALU.mult), [pb, sg], [t2])
                S.op("pool", lambda e, t1=t1, t2=t2, m_=m_, dm=dm: e.tensor_tensor(out=m_[:, dm, :], in0=t1[:], in1=t2[:], op=ALU.add), [t1, t2], [m_])
            for tt in range(4):
                ti = tb * 4 + tt
                xt = xr_.next(); x1 = x1r.next(); h2 = h2r.next(); h2b = h2br.next(); h2T = h2Tr.next(); st = st_ring.next()
                S.dma("sp", lambda e, xt=xt, ti=ti: e.dma_start(out=xt[:], in_=A["x"][ti * 128:(ti + 1) * 128, :]), [DB["x"]], [xt], xt)
                mo = (PB[2], PB[3]) if mo_i[0] % 2 == 0 else (PB[4], PB[5])
                mo_i[0] += 1
                for half in range(2):
                    for dm in range(8):
                        S.op("pe", lambda e, half=half, dm=dm, tt=tt, m_=m_: e.matmul(mo[half][:, :], lhsT=m_[:, dm, tt * 128:(tt + 1) * 128], rhs=wout[:, dm, half * 512:(half + 1) * 512],
                                                                                start=(dm == 0), stop=(dm == 7)), [m_, wout], [mo[half]])
                if len(pending) > 2:
                    router_part(*pending.pop(0))
                for half in range(2):
                    S.op("act", lambda e, half=half, ti=ti: e.activation(out=junk[:, 0:512], in_=mo[half][:, :], func=AF.Square, accum_out=st[:, 0, half:half + 1]), [mo[half]], [junk, st])
                S.op("dve", lambda e, ti=ti: e.tensor_tensor(out=st[:, 0, 2:3], in0=st[:, 0, 0:1], in1=st[:, 0, 1:2], op=ALU.add), [st], [st])
                S.op("act", lambda e, ti=ti: e.activation(out=st[:, 0, 3:4], in_=st[:, 0, 2:3], func=AF.Sqrt, scale=1.0 / D, bias=EPS), [st], [st])
                S.op("dve", lambda e, ti=ti: e.reciprocal(out=st[:, 0, 4:5], in_=st[:, 0, 3:4]), [st], [st])
                for half in range(2):
                    hs = slice(half * 512, (half + 1) * 512)
                    S.op("dve", lambda e, half=half, hs=hs, x1=x1, ti=ti: e.scalar_tensor_tensor(out=x1[:, hs], in0=mo[half][:, :], scalar=st[:, 0, 4:5], in1=rows[:, 0, hs], op0=ALU.mult, op1=ALU.mult),
                         [mo[half], st, rows], [x1])
                S.op("pool", lambda e, x1=x1, xt=xt: e.tensor_tensor(out=x1[:], in0=x1[:], in1=xt[:], op=ALU.add), [x1, xt], [x1])
                S.dma("sp", lambda e, x1=x1, ti=ti: e.dma_start(out=X1_d[ti * 128:(ti + 1) * 128, :], in_=x1[:]), [x1], [DB["X1"]], x1, concurrent=True)
                S.op("act", lambda e, x1=x1, ti=ti: e.activation(out=junk2[:], in_=x1[:], func=AF.Square, accum_out=st[:, 0, 5:6]), [x1], [junk2, st])
                S.op("act", lambda e, ti=ti: e.activation(out=st[:, 0, 6:7], in_=st[:, 0, 5:6], func=AF.Sqrt, scale=1.0 / D, bias=EPS), [st], [st])
                S.op("dve", lambda e, ti=ti: e.reciprocal(out=st[:, 0, 7:8], in_=st[:, 0, 6:7]), [st], [st])
                S.op("dve", lambda e, x1=x1, h2=h2, ti=ti: e.scalar_tensor_tensor(out=h2[:], in0=x1[:], scalar=st[:, 0, 7:8], in1=rows[:, 1, :], op0=ALU.mult, op1=ALU.mult), [x1, st, rows], [h2])
                S.op("pool", lambda e, h2=h2: e.tensor_tensor(out=h2[:], in0=h2[:], in1=rows[:, 2, :], op=ALU.add), [h2, rows], [h2])
                S.op("act", lambda e, h2=h2, h2b=h2b: e.activation(out=h2b[:], in_=h2[:], func=AF.Copy), [h2], [h2b])
                S.dma("sp", lambda e, h2b=h2b, ti=ti: e.dma_start(out=H2_d[ti * 128:(ti + 1) * 128, :], in_=h2b[:]), [h2b], [DB["H2"]], h2b, concurrent=True)
                pending.append((ti, h2, h2T, st))
        while pending:
            router_part(*pending.pop(0))
        S.barrier()
        if dbg and stage == 5:
            dd = dbg_tensor("X1", [L, D], F32); dh = dbg_tensor("H2", [L, D], BF16); da = dbg_tensor("aff", [128, 512], F32)
            S.dma("sp", lambda e: e.dma_start(out=da, in_=aff[:].rearrange("p a b -> p (a b)")), [aff], [DB["dbg"]], aff, concurrent=True)
            for ti in range(32):
                xt = xr_.next(); hb = h2br.next()
                S.dma("sp", lambda e, xt=xt, ti=ti: e.dma_start(out=xt[:], in_=X1_d[ti * 128:(ti + 1) * 128, :]), [DB["X1"]], [xt], xt)
                S.dma("sp", lambda e, xt=xt, ti=ti: e.dma_start(out=dd[ti * 128:(ti + 1) * 128, :], in_=xt[:]), [xt], [DB["dbg"]], xt, concurrent=True)
                S.dma("sp", lambda e, hb=hb, ti=ti: e.dma_start(out=hb[:], in_=H2_d[ti * 128:(ti + 1) * 128, :]), [DB["H2"]], [hb], hb)
                S.dma("sp", lambda e, hb=hb, ti=ti: e.dma_start(out=dh[ti * 128:(ti + 1) * 128, :], in_=hb[:]), [hb], [DB["dbg"]], hb, concurrent=True)
            S.barrier()


def phase678(nc, S, A, DB, PB, K, stage, dbg, dbg_tensor):
    aff = K["aff"]; iota_f = K["iota_f"]; pidx = K["pidx"]; ones_f = K["ones_f"]; ones_bf = K["ones_bf"]; ident_bf = K["ident_bf"]
    H2_d = K["H2_d"]; X1_d = K["X1_d"]; F_d = K["F_d"]; ROWS_d = K["ROWS_d"]; out_ap = K["out"]
    CAP = 512
    with ExitStack() as ph:
        zt = S.sb("zt", [128, D], F32, ph)
        S.op("pool", lambda e: e.memset(zt[:], 0.0), [], [zt])
        for ti in range(32):
            S.dma("sp", lambda e, ti=ti: e.dma_start(out=F_d[ti * 128:(ti + 1) * 128, :], in_=zt[:]), [zt], [DB["F"]], zt, concurrent=True)
        lo = S.sb("lo", [128, 16], F32, ph); hi = S.sb("hi", [128, 16], F32, ph); mid = S.sb("mid", [128, 16], F32, ph)
        dd_ = S.sb("dd", [128, 16], F32, ph); sel = S.sb("sel", [128, 16], F32, ph); part = S.sb("part", [128, 16], F32, ph)
        cmp_ = S.sb("cmp", [128, 32, 16], F32, ph)
        maskf = S.sb("maskf", [128, 32, 16], F32, ph); maskb = S.sb("maskb", [128, 32, 16], BF16, ph)
        posm = S.sb("posm", [128, 32, 16], F32, ph); offs = S.sb("offs", [128, 32, 16], F32, ph)
        RH = S.sb("RH", [128, 32, 16, 4], BF16, ph); ahf = S.sb("ahf", [128, 32, 16], F32, ph)
        U_bf = S.sb("U_bf", [128, 128], BF16, ph)
        S.op("dve", lambda e: e.memset(lo[:], 0.0), [], [lo])
        S.op("dve", lambda e: e.memset(hi[:], 1.0), [], [hi])
        pc = PB[0]
        for it in range(27):
            S.op("dve", lambda e: e.tensor_tensor(out=mid[:], in0=lo[:], in1=hi[:], op=ALU.add), [lo, hi], [mid])
            S.op("dve", lambda e: e.tensor_scalar(out=mid[:], in0=mid[:], scalar1=0.5, scalar2=None, op0=ALU.mult), [mid], [mid])
            S.op("dve", lambda e: e.tensor_tensor(out=cmp_[:], in0=aff[:], in1=mid[:].unsqueeze(1).to_broadcast([128, 32, 16]), op=ALU.is_ge), [aff, mid], [cmp_])
            S.op("dve", lambda e: e.reduce_sum(out=part[:], in_=cmp_[:].rearrange("p t e -> p e t"), axis=AX.X), [cmp_], [part])
            S.op("pe", lambda e: e.matmul(pc[:, 0:16], lhsT=ones_f[:], rhs=part[:], start=True, stop=True), [ones_f, part], [pc])
            S.op("dve", lambda e: e.tensor_scalar(out=sel[:], in0=pc[:, 0:16], scalar1=float(CAP) - 0.5, scalar2=None, op0=ALU.is_ge), [pc], [sel])
            S.op("dve", lambda e: e.tensor_tensor(out=dd_[:], in0=mid[:], in1=lo[:], op=ALU.subtract), [mid, lo], [dd_])
            S.op("dve", lambda e: e.tensor_tensor(out=dd_[:], in0=dd_[:], in1=sel[:], op=ALU.mult), [dd_, sel], [dd_])
            S.op("dve", lambda e: e.tensor_tensor(out=lo[:], in0=lo[:], in1=dd_[:], op=ALU.add), [lo, dd_], [lo])
            S.op("dve", lambda e: e.tensor_tensor(out=dd_[:], in0=hi[:], in1=mid[:], op=ALU.subtract), [hi, mid], [dd_])
            S.op("dve", lambda e: e.tensor_tensor(out=dd_[:], in0=dd_[:], in1=sel[:], op=ALU.mult), [dd_, sel], [dd_])
            S.op("dve", lambda e: e.tensor_tensor(out=hi[:], in0=mid[:], in1=dd_[:], op=ALU.add), [mid, dd_], [hi])
        S.op("dve", lambda e: e.tensor_tensor(out=maskf[:], in0=aff[:], in1=lo[:].unsqueeze(1).to_broadcast([128, 32, 16]), op=ALU.is_ge), [aff, lo], [maskf])
        S.op("dve", lambda e: e.tensor_copy(out=maskb[:], in_=maskf[:]), [maskf], [maskb])
        S.op("dve", lambda e: e.tensor_scalar(out=U_bf[:], in0=iota_f[:, 0:128], scalar1=pidx[:, 0:1], scalar2=None, op0=ALU.is_gt), [iota_f, pidx], [U_bf])
        pcnt = PB[1]; ppos = PB[2]
        mb2 = maskb[:].rearrange("p t e -> p (t e)")
        S.op("pe", lambda e: e.matmul(pcnt[:, :], lhsT=ones_bf[:], rhs=mb2, start=True, stop=True), [ones_bf, maskb], [pcnt])
        S.op("pe", lambda e: e.matmul(ppos[:, :], lhsT=U_bf[:], rhs=mb2, start=True, stop=True), [U_bf, maskb], [ppos])
        S.op("dve", lambda e: e.memset(offs[:, 0, :], 0.0), [], [offs])
        cntv = pcnt[:, :].rearrange("p (t e) -> p t e", e=16)
        for tt in range(1, 32):
            S.op("dve", lambda e, tt=tt: e.tensor_tensor(out=offs[:, tt, :], in0=offs[:, tt - 1, :], in1=cntv[:, tt - 1, :], op=ALU.add), [offs, pcnt], [offs])
        S.op("dve", lambda e: e.tensor_tensor(out=posm[:].rearrange("p t e -> p (t e)"), in0=ppos[:, :], in1=offs[:].rearrange("p t e -> p (t e)"), op=ALU.add), [ppos, offs], [posm])
        S.op("dve", lambda e: e.scalar_tensor_tensor(out=posm[:], in0=posm[:], scalar=1.0, in1=maskf[:], op0=ALU.add, op1=ALU.mult), [posm, maskf], [posm])
        S.op("dve", lambda e: e.tensor_scalar(out=posm[:], in0=posm[:], scalar1=-1.0, scalar2=None, op0=ALU.add), [posm], [posm])
        S.op("dve", lambda e: e.tensor_copy(out=RH[:, :, :, 0], in_=iota_f[:, 0:32].unsqueeze(2).to_broadcast([128, 32, 16])), [iota_f], [RH])
        S.op("dve", lambda e: e.tensor_copy(out=RH[:, :, :, 1].rearrange("p t e -> p (t e)"), in_=pidx[:, 0:1].to_broadcast([128, 512])), [pidx], [RH])
        S.op("dve", lambda e: e.tensor_copy(out=RH[:, :, :, 2], in_=aff[:]), [aff], [RH])
        S.op("dve", lambda e: e.tensor_copy(out=ahf[:], in_=RH[:, :, :, 2]), [RH], [ahf])
        S.op("dve", lambda e: e.tensor_tensor(out=RH[:, :, :, 3], in0=aff[:], in1=ahf[:], op=ALU.subtract), [aff, ahf], [RH])
        if dbg and stage == 6:
            d1 = dbg_tensor("posm", [128, 512], F32); d2 = dbg_tensor("thr", [128, 16], F32); d3 = dbg_tensor("aff6", [128, 512], F32)
            S.dma("sp", lambda e: e.dma_start(out=d3, in_=aff[:].rearrange("p a b -> p (a b)")), [aff], [DB["dbg"]], aff, concurrent=True)
            S.dma("sp", lambda e: e.dma_start(out=d1, in_=posm[:].rearrange("p a b -> p (a b)")), [posm], [DB["dbg"]], posm, concurrent=True)
            S.dma("sp", lambda e: e.dma_start(out=d2, in_=lo[:]), [lo], [DB["dbg"]], lo, concurrent=True)
            S.barrier()
            return
        pm = ExitStack()
        mkm = S.mark()
        selr = Ring(S, "selT", [128, 512], BF16, 10, pm)
        zeros_m = S.sb("zeros_m", [128, 128], BF16, pm)
        S.op("pool", lambda e: e.memset(zeros_m[:], 0.0), [], [zeros_m])
        idxf = [S.sb("idxf%d" % i, [128, 4, 4], F32, pm) for i in range(3)]
        idxi = [S.sb("idxi%d" % i, [128, 4], I32, pm) for i in range(3)]
        gate = [S.sb("gate%d" % i, [128, 4], F32, pm) for i in range(3)]
        xs = [S.sb("xs%d" % i, [128, 4, D], BF16, pm) for i in range(3)]
        xsT = S.sb("xsT", [128, 8, 512], BF16, pm)
        wgr = Ring(S, "wg", [128, 8, 512], BF16, 3, pm)
        wur = Ring(S, "wu", [128, 8, 512], BF16, 3, pm)
        wdr = Ring(S, "wd", [128, 16, 512], BF16, 2, pm)
        hidT = S.sb("hidT", [128, 16, 512], BF16, pm)
        sgr = Ring(S, "sgm", [128, 512], F32, 2, pm)
        ysr = [S.sb("ys%d" % i, [128, D], F32, pm) for i in range(4)]

        def compaction_steps(e_):
            k = e_ % 3
            pcs = PB[0]
            steps = []
            dsteps = []

            def init():
                S.op("pe", lambda e: e.matmul(pcs[:, 0:16], lhsT=zeros_m[:], rhs=RH[:, 0, 0:4, :].rearrange("p a b -> p (a b)"), start=True, stop=False), [zeros_m, RH], [pcs])
            steps.append(init)
            sels = {}
            for tt in range(32):
                def dstep(tt=tt):
                    sl = selr.next()
                    S.op("dve", lambda e: e.tensor_scalar(out=sl[:], in0=iota_f[:, :], scalar1=posm[:, tt, e_:e_ + 1], scalar2=None, op0=ALU.is_equal), [iota_f, posm], [sl])
                    sels[tt] = sl

                def pstep(tt=tt):
                    sl = sels.pop(tt)
                    for st in range(4):
                        S.op("pe", lambda e, st=st: e.matmul(pcs[:, st * 4:(st + 1) * 4], lhsT=sl[:, st * 128:(st + 1) * 128], rhs=RH[:, tt, e_, :], start=False, stop=(tt == 31)),
                             [sl, RH], [pcs])
                dsteps.append(dstep); steps.append(pstep)

            def fin():
                S.op("dve", lambda e: e.tensor_copy(out=idxf[k][:].rearrange("p a b -> p (a b)"), in_=pcs[:, 0:16]), [pcs], [idxf[k]])
                S.op("dve", lambda e: e.scalar_tensor_tensor(out=idxf[k][:, :, 0], in0=idxf[k][:, :, 0], scalar=128.0, in1=idxf[k][:, :, 1], op0=ALU.mult, op1=ALU.add), [idxf[k]], [idxf[k]])
                S.op("dve", lambda e: e.tensor_scalar(out=idxf[k][:, :, 0], in0=idxf[k][:, :, 0], scalar1=8388608.0, scalar2=None, op0=ALU.add), [idxf[k]], [idxf[k]])
                S.op("dve", lambda e: e.tensor_single_scalar(out=idxi[k][:], in_=idxf[k][:, :, 0].bitcast(I32), scalar=0x7FFFFF, op=ALU.bitwise_and), [idxf[k]], [idxi[k]])
                S.op("dve", lambda e: e.tensor_tensor(out=gate[k][:], in0=idxf[k][:, :, 2], in1=idxf[k][:, :, 3], op=ALU.add), [idxf[k]], [gate[k]])
                for st in range(4):
                    S.dma("pool", lambda e, st=st: e.indirect_dma_start(out=xs[k][:, st, :], out_offset=None, in_=H2_d, in_offset=bass.IndirectOffsetOnAxis(ap=idxi[k][:, st:st + 1], axis=0)),
                          [DB["H2"], idxi[k]], [xs[k]], xs[k], concurrent=(st > 0))
            steps.append(fin)
            return steps, dsteps

        wtasks = []
        for e_ in range(16):
            for fb in range(4):
                wtasks.append(("gu", e_, fb))
            for half in range(2):
                wtasks.append(("d", e_, half))
        wbuf = {}
        nxt = [0]

        def prefetch(upto):
            while nxt[0] < len(wtasks) and nxt[0] <= upto:
                kind, e_, j = wtasks[nxt[0]]
                if kind == "gu":
                    wg = wgr.next(); wu = wur.next()
                    S.dma("pool", lambda e, wg=wg, e_=e_, j=j: e.dma_start(out=wg[:], in_=A["w_exp_gate"][e_, :, j * 512:(j + 1) * 512].rearrange("(k p) n -> p k n", p=128)), [DB["w_exp"]], [wg], wg)
                    S.dma("pool", lambda e, wu=wu, e_=e_, j=j: e.dma_start(out=wu[:], in_=A["w_exp_up"][e_, :, j * 512:(j + 1) * 512].rearrange("(k p) n -> p k n", p=128)), [DB["w_exp"]], [wu], wu)
                    wbuf[nxt[0]] = (wg, wu)
                else:
                    wd = wdr.next()
                    S.dma("pool", lambda e, wd=wd, e_=e_, j=j: e.dma_start(out=wd[:], in_=A["w_exp_down"][e_, :, j * 512:(j + 1) * 512].rearrange("(k p) n -> p k n", p=128)), [DB["w_exp"]], [wd], wd)
                    wbuf[nxt[0]] = (wd,)
                nxt[0] += 1

        for e0 in range(2):
            ps_, ds_ = compaction_steps(e0)
            ps_.pop(0)()
            while ds_:
                for _ in range(4):
                    if ds_:
                        ds_.pop(0)()
                for _ in range(4):
                    if len(ps_) > 1:
                        ps_.pop(0)()
            while ps_:
                ps_.pop(0)()
            if e0 == 0:
                prefetch(1)
        for e_ in range(16):
            k = e_ % 3
            csteps, dsteps_ = compaction_steps(e_ + 2) if e_ + 2 < 16 else ([], [])
            for _ in range(6):
                if dsteps_:
                    dsteps_.pop(0)()
            for dt_ in range(8):
                pt = PB[(1, 6, 7)[dt_ % 3]]
                ptb = pt.t.bitcast(BF16)
                for st in range(4):
                    S.op("pe", lambda e, ptb=ptb, st=st, dt_=dt_: e.transpose(ptb[:, st * 128:(st + 1) * 128], xs[k][:, st, dt_ * 128:(dt_ + 1) * 128], ident_bf[:]), [xs[k], ident_bf], [pt])
                if dt_ % 2 == 0:
                    S.op("act", lambda e, ptb=ptb, dt_=dt_: e.activation(out=xsT[:, dt_, :], in_=ptb[:, 0:512], func=AF.Copy), [pt], [xsT])
                else:
                    S.op("dve", lambda e, ptb=ptb, dt_=dt_: e.tensor_copy(out=xsT[:, dt_, :], in_=ptb[:, 0:512]), [pt], [xsT])
            base = e_ * 6
            for fb in range(4):
                prefetch(base + fb + 2)
                wg, wu = wbuf.pop(base + fb)
                for f4 in range(4):
                    ft = fb * 4 + f4
                    pg = PB[2 + 2 * (ft % 2)]; pu = PB[3 + 2 * (ft % 2)]
                    for kt in range(8):
                        S.op("pe", lambda e, pg=pg, wg=wg, kt=kt, f4=f4: e.matmul(pg[:, :], lhsT=wg[:, kt, f4 * 128:(f4 + 1) * 128], rhs=xsT[:, kt, :], start=(kt == 0), stop=(kt == 7)), [wg, xsT], [pg])
                    for kt in range(8):
                        S.op("pe", lambda e, pu=pu, wu=wu, kt=kt, f4=f4: e.matmul(pu[:, :], lhsT=wu[:, kt, f4 * 128:(f4 + 1) * 128], rhs=xsT[:, kt, :], start=(kt == 0), stop=(kt == 7)), [wu, xsT], [pu])
                    sg = sgr.next()
                    S.op("act", lambda e, pg=pg, sg=sg: e.activation(out=sg[:], in_=pg[:, :], func=AF.Silu), [pg], [sg])
                    S.op("dve", lambda e, pu=pu, sg=sg, ft=ft: e.tensor_tensor(out=hidT[:, ft, :], in0=sg[:], in1=pu[:, :], op=ALU.mult), [sg, pu], [hidT])
                    for _ in range(3):
                        if len(csteps) > 1:
                            csteps.pop(0)()
                        if dsteps_:
                            dsteps_.pop(0)()
            for half in range(2):
                prefetch(base + 4 + half + 2)
                (wd,) = wbuf.pop(base + 4 + half)
                for st in range(4):
                    po = PB[6 + (st % 2)]
                    for ft in range(16):
                        S.op("pe", lambda e, po=po, wd=wd, ft=ft, st=st: e.matmul(po[:, :], lhsT=hidT[:, ft, st * 128:(st + 1) * 128], rhs=wd[:, ft, :], start=(ft == 0), stop=(ft == 15)), [hidT, wd], [po])
                    S.op("act", lambda e, po=po, st=st, half=half: e.activation(out=ysr[st][:, half * 512:(half + 1) * 512], in_=po[:, :], func=AF.Copy, scale=gate[k][:, st:st + 1]), [po, gate[k]], [ysr[st]])
            while csteps:
                csteps.pop(0)()
            for st in range(4):
                S.dma("pool", lambda e, st=st: e.indirect_dma_start(out=F_d, out_offset=bass.IndirectOffsetOnAxis(ap=idxi[k][:, st:st + 1], axis=0), in_=ysr[st][:], in_offset=None, compute_op=ALU.add),
                      [ysr[st], idxi[k]], [DB["F"]], ysr[st], concurrent=(st > 0))
        S.release(mkm)
        pm.close()
        gwf = S.sb("gwf", [128, D], F32, ph)
        S.dma("sp", lambda e: e.dma_start(out=gwf[:], in_=ROWS_d[:, 3 * D:4 * D]), [DB["ROWS"]], [gwf], gwf)
        fr = Ring(S, "ft", [128, D], F32, 4, ph); x1r = Ring(S, "x1f", [128, D], F32, 4, ph); orr = Ring(S, "of", [128, D], F32, 4, ph)
        stf_ring = Ring(S, "stf", [128, 1, 4], F32, 4, ph)
        junk = S.sb("junk8", [128, D], BF16, ph)
        for ti in range(32):
            ft_ = fr.next(); x1 = x1r.next(); ot = orr.next(); stf = stf_ring.next()
            S.dma("sp", lambda e, ft_=ft_, ti=ti: e.dma_start(out=ft_[:], in_=F_d[ti * 128:(ti + 1) * 128, :]), [DB["F"]], [ft_], ft_)
            S.dma("sp", lambda e, x1=x1, ti=ti: e.dma_start(out=x1[:], in_=X1_d[ti * 128:(ti + 1) * 128, :]), [DB["X1"]], [x1], x1)
            S.op("act", lambda e, ft_=ft_, ti=ti: e.activation(out=junk[:], in_=ft_[:], func=AF.Square, accum_out=stf[:, 0, 0:1]), [ft_], [junk, stf])
            S.op("act", lambda e, ti=ti: e.activation(out=stf[:, 0, 1:2], in_=stf[:, 0, 0:1], func=AF.Sqrt, scale=1.0 / D, bias=EPS), [stf], [stf])
            S.op("dve", lambda e, ti=ti: e.reciprocal(out=stf[:, 0, 2:3], in_=stf[:, 0, 1:2]), [stf], [stf])
            S.op("dve", lambda e, ft_=ft_, ot=ot, ti=ti: e.scalar_tensor_tensor(out=ot[:], in0=ft_[:], scalar=stf[:, 0, 2:3], in1=gwf[:], op0=ALU.mult, op1=ALU.mult), [ft_, stf, gwf], [ot])
            S.op("pool", lambda e, ot=ot, x1=x1: e.tensor_tensor(out=ot[:], in0=ot[:], in1=x1[:], op=ALU.add), [ot, x1], [ot])
            S.dma("sp", lambda e, ot=ot, ti=ti: e.dma_start(out=out_ap[ti * 128:(ti + 1) * 128, :], in_=ot[:]), [ot], [DB["out"]], ot, concurrent=True)
        S.wait_all("sp", [DB["out"]])
        S.barrier()


_NC_CACHE = {}


def _core_inputs(inp, b):
    m = {}
    m["x"] = inp["x"][b]; m["ctx"] = inp["ctx"][b]
    m["c"] = inp["c"][b:b + 1]; m["c_ctx"] = np.asarray(inp["c_ctx"]).reshape(1, D)
    m["w_ada"] = inp["w_ada"][0]; m["b_ada"] = np.asarray(inp["b_ada"]).reshape(1, 6 * D)
    for n in ("norm_pre_mix", "norm_post_mix", "norm_pre_ffn", "norm_post_ffn"):
        m[n] = np.asarray(inp[n]).reshape(1, D)
    m["w_in"] = inp["w_in"][0]
    m["s5_lam_re"] = inp["s5_lam_re"][0].reshape(64, 64); m["s5_lam_im"] = inp["s5_lam_im"][0].reshape(64, 64)
    m["s5_log_dt"] = inp["s5_log_dt"][0].reshape(1, 64)
    m["s5_b_re"] = inp["s5_b_re"][0].reshape(64, 64, 16); m["s5_b_im"] = inp["s5_b_im"][0].reshape(64, 64, 16)
    m["s5_c_re"] = inp["s5_c_re"][0].reshape(2, 512, 64); m["s5_c_im"] = inp["s5_c_im"][0].reshape(2, 512, 64)
    m["s5_d"] = np.asarray(inp["s5_d"]).reshape(1, 512); m["w_glu"] = inp["w_glu"][0]
    m["da_lambda"] = inp["da_lambda"][0].reshape(1, 256); m["da_subln"] = np.asarray(inp["da_subln"]).reshape(1, 128)
    m["w_proj_a"] = inp["w_proj_a"][0]; m["w_proj_b"] = inp["w_proj_b"][0]; m["w_out"] = inp["w_out"][0]
    m["w_router"] = inp["w_router"][0]
    m["w_exp_gate"] = inp["w_exp_gate"][0]; m["w_exp_up"] = inp["w_exp_up"][0]; m["w_exp_down"] = inp["w_exp_down"][0]
    return {k: np.ascontiguousarray(np.asarray(v, dtype=np.float32)) for k, v in m.items()}


def kernel(**inputs):
    inp = {k: np.asarray(v) for k, v in inputs.items()}
    if "nc" not in _NC_CACHE:
        _NC_CACHE["nc"] = build()
    nc = _NC_CACHE["nc"]
    n = 8
    in_maps = [_core_inputs(inp, b) for b in range(n)]
    res = run_bass_kernel_spmd(nc, in_maps, core_ids=list(range(n)))
    return np.stack([np.asarray(r["out"], dtype=np.float32) for r in res.results], axis=0)
```

```python
import math
from contextlib import ExitStack
import numpy as np
import concourse.bass as bass
import concourse.mybir as mybir
from concourse.bass_utils import run_bass_kernel_spmd

F32 = mybir.dt.float32
BF16 = mybir.dt.bfloat16
I32 = mybir.dt.int32
AF = mybir.ActivationFunctionType
ALU = mybir.AluOpType
AX = mybir.AxisListType

D = 1024
L = 4096
LC = 256
NT = (L + LC) // 128
EPS = 1e-6
MAGIC = 12582912.0
TWO_PI = 2.0 * math.pi


class Buf:
    _n = 0

    def __init__(self, name, t=None):
        Buf._n += 1
        self.key = "b%d" % Buf._n
        self.name = name
        self.t = t
        self.w = None
        self.r = {}
        self.dsem = None
        self.dcnt = 0
        self.dw = {}
        self.dr = {}

    def __getitem__(self, idx):
        return self.t[idx]


class Sched:
    ENG = ("pe", "act", "dve", "pool", "sp")

    def __init__(self, nc, stack):
        self.nc = nc
        self.stack = stack
        self.eng = {"pe": nc.tensor, "act": nc.scalar, "dve": nc.vector, "pool": nc.gpsimd, "sp": nc.sync}
        self.sem = {}
        self.tick = {e: 0 for e in self.ENG}
        for e in self.ENG:
            self.sem[e] = stack.enter_context(nc.semaphore("sem_" + e))
        self.seen = {e: {} for e in self.ENG}
        self.n_dsem = 0
        self.ninst = {e: 0 for e in self.ENG}
        self.nwait = 0
        self.uid = 0
        self.live = []
        self.free_sems = []

    def sb(self, name, shape, dt, stack=None):
        self.uid += 1
        t = (stack or self.stack).enter_context(self.nc.sbuf_tensor("%s_%d" % (name, self.uid), list(shape), dt))
        b = Buf(name, t)
        self.live.append(b)
        return b

    def mark(self):
        return len(self.live)

    def release(self, mark):
        bufs = self.live[mark:]
        del self.live[mark:]
        for b in bufs:
            if b.dsem is not None:
                for e in self.ENG:
                    self._wait(e, b.key, b.dsem, b.dcnt)
        self.barrier()
        for b in bufs:
            if b.dsem is not None:
                self.free_sems.append((b.dsem, b.dcnt))
                b.dsem = None

    def _dsem(self, b):
        if b.dsem is None:
            if self.free_sems:
                b.dsem, b.dcnt = self.free_sems.pop()
            else:
                b.dsem = self.stack.enter_context(self.nc.semaphore("ds%d" % self.n_dsem))
                self.n_dsem += 1
        return b.dsem

    def _wait(self, e, semkey, semh, val):
        if val <= 0:
            return
        if self.seen[e].get(semkey, 0) >= val:
            return
        self.eng[e].wait_ge(semh, val)
        self.seen[e][semkey] = val
        self.nwait += 1

    def _wait_eng(self, e, other, tick):
        if other == e and e == "pe":
            return
        self._wait(e, other, self.sem[other], tick)

    def _deps(self, e, reads, writes):
        for b in reads:
            if b.w is not None:
                self._wait_eng(e, b.w[0], b.w[1])
            for k, (sh, v) in b.dw.items():
                self._wait(e, k, sh, v)
        for b in writes:
            if b.w is not None:
                self._wait_eng(e, b.w[0], b.w[1])
            for oe, tk in b.r.items():
                self._wait_eng(e, oe, tk)
            for k, (sh, v) in b.dw.items():
                self._wait(e, k, sh, v)
            for k, (sh, v) in b.dr.items():
                self._wait(e, k, sh, v)

    def op(self, e, fn, reads=(), writes=()):
        self._deps(e, reads, writes)
        inst = fn(self.eng[e])
        self.tick[e] += 1
        inst.then_inc(self.sem[e], 1)
        tk = self.tick[e]
        for b in reads:
            b.r[e] = tk
        for b in writes:
            b.w = (e, tk)
            b.r = {}
            b.dw = {}
            b.dr = {}
        self.ninst[e] += 1
        return inst

    def dma(self, q, fn, reads, writes, semb, concurrent=False):
        if concurrent:
            self._deps(q, reads, [])
            for b in writes:
                if b.w is not None:
                    self._wait_eng(q, b.w[0], b.w[1])
                for oe, tk in b.r.items():
                    self._wait_eng(q, oe, tk)
                for k, (sh, v) in b.dr.items():
                    self._wait(q, k, sh, v)
        else:
            self._deps(q, reads, writes)
        inst = fn(self.eng[q])
        sem = self._dsem(semb)
        semb.dcnt += 16
        inst.then_inc(sem, 16)
        ev = (sem, semb.dcnt)
        k = semb.key
        for b in reads:
            b.dr[k] = ev
        for b in writes:
            if concurrent:
                b.dw[k] = ev
            else:
                b.w = None
                b.r = {}
                b.dr = {}
                b.dw = {k: ev}
        self.ninst[q] += 1
        return inst

    def wait_all(self, e, bufs):
        self._deps(e, [], bufs)

    def barrier(self):
        for e in self.ENG:
            for o in self.ENG:
                if o != e:
                    self._wait_eng(e, o, self.tick[o])


class Ring:
    def __init__(self, S, name, shape, dt, n, stack=None):
        self.bufs = [S.sb("%s%d" % (name, i), shape, dt, stack) for i in range(n)]
        self.i = 0

    def next(self):
        b = self.bufs[self.i % len(self.bufs)]
        self.i += 1
        return b


def build(stage=99, dbg=False):
    nc = bass.Bass("TRN2", target_bir_lowering=False)

    def din(name, shape, dt=F32):
        return nc.dram_tensor(name, list(shape), dt, kind="ExternalInput").ap()

    def dscr(name, shape, dt):
        return nc.dram_tensor(name, list(shape), dt, kind="Internal").ap()

    A = {}
    A["x"] = din("x", [L, D]); A["ctx"] = din("ctx", [LC, D])
    A["c"] = din("c", [1, D]); A["c_ctx"] = din("c_ctx", [1, D])
    A["w_ada"] = din("w_ada", [D, 6 * D]); A["b_ada"] = din("b_ada", [1, 6 * D])
    for n_ in ("norm_pre_mix", "norm_post_mix", "norm_pre_ffn", "norm_post_ffn"):
        A[n_] = din(n_, [1, D])
    A["w_in"] = din("w_in", [D, 4096])
    A["s5_lam_re"] = din("s5_lam_re", [64, 64]); A["s5_lam_im"] = din("s5_lam_im", [64, 64])
    A["s5_log_dt"] = din("s5_log_dt", [1, 64])
    A["s5_b_re"] = din("s5_b_re", [64, 64, 16]); A["s5_b_im"] = din("s5_b_im", [64, 64, 16])
    A["s5_c_re"] = din("s5_c_re", [2, 512, 64]); A["s5_c_im"] = din("s5_c_im", [2, 512, 64])
    A["s5_d"] = din("s5_d", [1, 512]); A["w_glu"] = din("w_glu", [512, 512])
    A["da_lambda"] = din("da_lambda", [1, 256]); A["da_subln"] = din("da_subln", [1, 128])
    A["w_proj_a"] = din("w_proj_a", [512, D]); A["w_proj_b"] = din("w_proj_b", [512, D])
    A["w_out"] = din("w_out", [D, D]); A["w_router"] = din("w_router", [D, 16])
    A["w_exp_gate"] = din("w_exp_gate", [16, D, 2048]); A["w_exp_up"] = din("w_exp_up", [16, D, 2048])
    A["w_exp_down"] = din("w_exp_down", [16, 2048, D])
    out_ap = nc.dram_tensor("out", [L, D], F32, kind="ExternalOutput").ap()

    U_d = dscr("U_d", [5, 128, 4096], BF16)
    QT_d = dscr("QT_d", [512, L], BF16)
    KT_d = dscr("KT_d", [512, LC + L], BF16)
    V_d = dscr("V_d", [LC + L, 512], BF16)
    SG_d = dscr("SG_d", [2048, L], BF16)
    YA_d = dscr("YA_d", [512, L], BF16)
    YB_d = dscr("YB_d", [512, L], BF16)
    X1_d = dscr("X1_d", [L, D], F32)
    H2_d = dscr("H2_d", [L, D], BF16)
    F_d = dscr("F_d", [L, D], F32)
    DB = {}
    for k_ in ("x", "ctx", "c", "c_ctx", "w_ada", "b_ada", "norm_pre_mix", "norm_post_mix", "norm_pre_ffn",
               "norm_post_ffn", "w_in", "s5p", "w_glu", "da", "w_proj", "w_out", "w_router", "w_exp",
               "U", "QT", "KT", "V", "SG", "YA", "YB", "X1", "H2", "F", "out", "dbg"):
        DB[k_] = Buf("D_" + k_)

    dbg_out = {}

    def dbg_tensor(name, shape, dt=F32):
        ap = nc.dram_tensor("dbg_" + name, list(shape), dt, kind="ExternalOutput").ap()
        dbg_out[name] = ap
        return ap

    with ExitStack() as top:
        S = Sched(nc, top)
        psum = top.enter_context(nc.psum_tensor("psum", [128, 4096], F32))
        PB = [Buf("bank%d" % i, psum[:, i * 512:(i + 1) * 512]) for i in range(8)]

        ident_bf = S.sb("ident_bf", [128, 128], BF16)
        ident_f = S.sb("ident_f", [128, 128], F32)
        iota_f = S.sb("iota_f", [128, 512], F32)
        pidx = S.sb("pidx", [128, 1], F32)
        ones_f = S.sb("ones_f", [128, 128], F32)
        ones_bf = S.sb("ones_bf", [128, 128], BF16)
        S.op("pool", lambda e: e.iota(iota_f[:], pattern=[[1, 512]], base=0, channel_multiplier=0,
                                      allow_small_or_imprecise_dtypes=True), [], [iota_f])
        S.op("pool", lambda e: e.iota(pidx[:], pattern=[[0, 1]], base=0, channel_multiplier=1,
                                      allow_small_or_imprecise_dtypes=True), [], [pidx])
        S.op("dve", lambda e: e.tensor_scalar(out=ident_bf[:], in0=iota_f[:, 0:128], scalar1=pidx[:, 0:1], scalar2=None,
                                              op0=ALU.is_equal), [iota_f, pidx], [ident_bf])
        S.op("dve", lambda e: e.tensor_scalar(out=ident_f[:], in0=iota_f[:, 0:128], scalar1=pidx[:, 0:1], scalar2=None,
                                              op0=ALU.is_equal), [iota_f, pidx], [ident_f])
        S.op("dve", lambda e: e.memset(ones_f[:], 1.0), [], [ones_f])
        S.op("dve", lambda e: e.memset(ones_bf[:], 1.0), [], [ones_bf])

        modc = S.sb("modc", [128, 4, 8], F32)
        ROWS_d = dscr("ROWS_d", [128, 4 * D], F32)
        DB["ROWS"] = Buf("D_ROWS")
        aff = S.sb("aff", [128, 32, 16], F32)

        mk0 = S.mark()
        with ExitStack() as ph:
            cc = S.sb("cc", [128, 8, 2], F32, ph)
            sc = S.sb("sc", [128, 8, 2], F32, ph)
            bcol = S.sb("bcol", [128, 2, 8], F32, ph)
            ncol = S.sb("ncol", [128, 8], F32, ph)
            brow = S.sb("brow", [1, 6 * D], F32, ph)
            nrow = S.sb("nrow", [1, 3, D], F32, ph)
            mrow = S.sb("mrow", [1, 4, D], F32, ph)
            wring = Ring(S, "wada", [128, 8, 512], F32, 2, ph)
            tmpc = S.sb("tmpc", [128, 4, 8], F32, ph)
            rows = S.sb("rows", [128, 4, D], F32, ph)
            S.dma("sp", lambda e: e.dma_start(out=cc[:, :, 0], in_=A["c"].rearrange("o (k p) -> p (o k)", p=128),
                                              allow_slow_non_contiguous=True), [DB["c"]], [cc], cc)
            S.dma("sp", lambda e: e.dma_start(out=cc[:, :, 1], in_=A["c_ctx"].rearrange("o (k p) -> p (o k)", p=128),
                                              allow_slow_non_contiguous=True), [DB["c_ctx"]], [cc], cc, concurrent=True)
            S.dma("sp", lambda e: e.dma_start(out=bcol[:, :, :], in_=A["b_ada"][:, 0:2048].rearrange("o (j k p) -> p (o j) k", p=128, k=8),
                                              allow_slow_non_contiguous=True), [DB["b_ada"]], [bcol], bcol)
            S.dma("sp", lambda e: e.dma_start(out=ncol[:, :], in_=A["norm_pre_mix"].rearrange("o (k p) -> p (o k)", p=128),
                                              allow_slow_non_contiguous=True), [DB["norm_pre_mix"]], [ncol], ncol)
            S.dma("sp", lambda e: e.dma_start(out=brow[:, :], in_=A["b_ada"]), [DB["b_ada"]], [brow], brow)
            for i_, n_ in enumerate(("norm_post_mix", "norm_pre_ffn", "norm_post_ffn")):
                S.dma("sp", lambda e, i_=i_, n_=n_: e.dma_start(out=nrow[:, i_, :], in_=A[n_]), [DB[n_]], [nrow], nrow,
                      concurrent=(i_ > 0))
            S.op("act", lambda e: e.activation(out=sc[:], in_=cc[:], func=AF.Silu), [cc], [sc])
            wv = A["w_ada"].rearrange("(k p) n -> p k n", p=128)
            pcol = PB[0]
            for blk in range(4):
                wt = wring.next()
                S.dma("sp", lambda e, wt=wt, blk=blk: e.dma_start(out=wt[:], in_=wv[:, :, blk * 512:(blk + 1) * 512]),
                      [DB["w_ada"]], [wt], wt)
                for ct in range(4):
                    col = blk * 4 + ct
                    for kt in range(8):
                        S.op("pe", lambda e, wt=wt, ct=ct, kt=kt, col=col: e.matmul(
                            pcol[:, col * 2:col * 2 + 2], lhsT=wt[:, kt, ct * 128:(ct + 1) * 128], rhs=sc[:, kt, :],
                            start=(kt == 0), stop=(kt == 7)), [wt, sc], [pcol])
            pv = pcol[:, 0:32].rearrange("p (j k t) -> p j k t", j=2, k=8)
            for t_ in range(2):
                S.op("dve", lambda e, t_=t_: e.tensor_tensor(out=modc[:, 2 * t_ + 1, :], in0=pv[:, 0, :, t_], in1=bcol[:, 0, :], op=ALU.add),
                     [pcol, bcol], [modc])
                S.op("dve", lambda e, t_=t_: e.scalar_tensor_tensor(out=tmpc[:, t_, :], in0=pv[:, 1, :, t_], scalar=1.0, in1=bcol[:, 1, :],
                                                                   op0=ALU.add, op1=ALU.add), [pcol, bcol], [tmpc])
                S.op("dve", lambda e, t_=t_: e.tensor_tensor(out=modc[:, 2 * t_, :], in0=tmpc[:, t_, :], in1=ncol[:, :], op=ALU.mult),
                     [tmpc, ncol], [modc])
            for ch in range(4):
                for half in range(2):
                    blk = (2 + ch) * 2 + half
                    wt = wring.next()
                    S.dma("sp", lambda e, wt=wt, blk=blk: e.dma_start(out=wt[:], in_=wv[:, :, blk * 512:(blk + 1) * 512]),
                          [DB["w_ada"]], [wt], wt)
                    pr = PB[1 + (blk % 2)]
                    for kt in range(8):
                        S.op("pe", lambda e, wt=wt, kt=kt, pr=pr: e.matmul(pr[0:1, :], lhsT=sc[:, kt, 0:1], rhs=wt[:, kt, :],
                                                                          start=(kt == 0), stop=(kt == 7)), [wt, sc], [pr])
                    S.op("dve", lambda e, pr=pr, ch=ch, half=half, blk=blk: e.tensor_tensor(
                        out=mrow[0:1, ch, half * 512:(half + 1) * 512], in0=pr[0:1, :], in1=brow[0:1, blk * 512:(blk + 1) * 512], op=ALU.add),
                        [pr, brow], [mrow])
            S.op("dve", lambda e: e.tensor_tensor(out=mrow[0:1, 0, :], in0=mrow[0:1, 0, :], in1=nrow[0:1, 0, :], op=ALU.mult), [mrow, nrow], [mrow])
            S.op("dve", lambda e: e.scalar_tensor_tensor(out=mrow[0:1, 2, :], in0=mrow[0:1, 2, :], scalar=1.0, in1=nrow[0:1, 1, :],
                                                         op0=ALU.add, op1=ALU.mult), [mrow, nrow], [mrow])
            S.op("dve", lambda e: e.tensor_tensor(out=mrow[0:1, 3, :], in0=mrow[0:1, 3, :], in1=nrow[0:1, 2, :], op=ALU.mult), [mrow, nrow], [mrow])
            for ri, mi in enumerate((0, 2, 1, 3)):
                for half in range(2):
                    pr = PB[3 + ((ri * 2 + half) % 2)]
                    S.op("pe", lambda e, pr=pr, mi=mi, half=half: e.matmul(pr[:, :], lhsT=ones_f[0:1, :], rhs=mrow[0:1, mi, half * 512:(half + 1) * 512],
                                                                           start=True, stop=True), [ones_f, mrow], [pr])
                    S.op("act", lambda e, pr=pr, ri=ri, half=half: e.activation(out=rows[:, ri, half * 512:(half + 1) * 512], in_=pr[:, :], func=AF.Copy),
                         [pr], [rows])
            S.dma("sp", lambda e: e.dma_start(out=ROWS_d, in_=rows[:].rearrange("p a b -> p (a b)")), [rows], [DB["ROWS"]], rows)
            if dbg and stage <= 1:
                d1 = dbg_tensor("modc", [128, 32]); d2 = dbg_tensor("rows", [128, 4 * D])
                S.dma("sp", lambda e: e.dma_start(out=d1, in_=modc[:].rearrange("p a b -> p (a b)")), [modc], [DB["dbg"]], modc, concurrent=True)
                S.dma("sp", lambda e: e.dma_start(out=d2, in_=rows[:].rearrange("p a b -> p (a b)")), [rows], [DB["dbg"]], rows, concurrent=True)
            S.barrier()
        S.release(mk0)
        if stage == 0:
            return finish(nc, S, DB, dbg_out)

        PHASES(nc, S, A, DB, PB, top, dict(ident_bf=ident_bf, ident_f=ident_f, iota_f=iota_f, pidx=pidx, ones_f=ones_f, ones_bf=ones_bf,
                                           modc=modc, ROWS_d=ROWS_d, aff=aff, out=out_ap, U_d=U_d, QT_d=QT_d, KT_d=KT_d, V_d=V_d, SG_d=SG_d,
                                           YA_d=YA_d, YB_d=YB_d, X1_d=X1_d, H2_d=H2_d, F_d=F_d), stage, dbg, dbg_tensor)
        return finish(nc, S, DB, dbg_out)


def finish(nc, S, DB, dbg_out):
    S.wait_all("sp", [DB["out"], DB["dbg"]])
    S.barrier()
    nc._dbg_out = dbg_out
    nc._stats = (dict(S.ninst), S.nwait, S.n_dsem)
    return nc


def PHASES(nc, S, A, DB, PB, top, K, stage, dbg, dbg_tensor):
    def run(fn):
        mk_ = S.mark()
        fn(nc, S, A, DB, PB, K, stage, dbg, dbg_tensor)
        S.release(mk_)
    if stage != 30:
        run(phase12)
    if stage <= 2:
        return
    if stage != 4:
        run(phase3)
    if stage in (3, 30):
        return
    run(phase4)
    if stage == 4:
        return
    run(phase5)
    if stage == 5:
        return
    run(phase678)
    if stage == 6:
        return


def phase12(nc, S, A, DB, PB, K, stage, dbg, dbg_tensor):
    ident_bf = K["ident_bf"]; modc = K["modc"]; iota_f = K["iota_f"]; pidx = K["pidx"]
    with ExitStack() as ph:
        hT = S.sb("hT", [128, 8, NT * 128], BF16, ph)
        cosT = S.sb("cosT", [128, L], BF16, ph)
        sinT = S.sb("sinT", [128, L], BF16, ph)
        with ExitStack() as ph0:
            fi = S.sb("fi", [128, 8], F32, ph0)
            wcol = S.sb("wcol", [128, 4], F32, ph0)
            rc = S.sb("rc", [128, 2, 64], F32, ph0)
            ang = S.sb("ang", [128, 64, 64], F32, ph0)
            t1 = S.sb("t1", [128, L], F32, ph0)
            t2 = S.sb("t2", [128, L], F32, ph0)

            def pfloor(dst, div, off):
                S.op("dve", lambda e: e.tensor_scalar(out=dst, in0=pidx[:, 0:1], scalar1=1.0 / div, scalar2=-off, op0=ALU.mult, op1=ALU.add), [pidx], [fi])
                S.op("dve", lambda e: e.tensor_scalar(out=dst, in0=dst, scalar1=MAGIC, scalar2=None, op0=ALU.add), [fi], [fi])
                S.op("dve", lambda e: e.tensor_scalar(out=dst, in0=dst, scalar1=-MAGIC, scalar2=None, op0=ALU.add), [fi], [fi])
            pfloor(fi[:, 2:3], 16.0, 0.46875)
            pfloor(fi[:, 3:4], 32.0, 0.484375)
            pfloor(fi[:, 4:5], 64.0, 0.4921875)
            S.op("dve", lambda e: e.scalar_tensor_tensor(out=fi[:, 0:1], in0=fi[:, 2:3], scalar=-16.0, in1=pidx[:, 0:1], op0=ALU.mult, op1=ALU.add), [fi, pidx], [fi])
            S.op("dve", lambda e: e.scalar_tensor_tensor(out=fi[:, 1:2], in0=fi[:, 4:5], scalar=-2.0, in1=fi[:, 3:4], op0=ALU.mult, op1=ALU.add), [fi], [fi])
            S.op("act", lambda e: e.activation(out=wcol[:, 0:1], in_=fi[:, 0:1], func=AF.Exp, scale=-math.log(10000.0) / 16.0), [fi], [wcol])
            S.op("dve", lambda e: e.tensor_tensor(out=wcol[:, 2:3], in0=wcol[:, 0:1], in1=fi[:, 1:2], op=ALU.mult), [wcol, fi], [wcol])
            S.op("dve", lambda e: e.tensor_tensor(out=wcol[:, 1:2], in0=wcol[:, 0:1], in1=wcol[:, 2:3], op=ALU.subtract), [wcol], [wcol])
            S.op("dve", lambda e: e.tensor_scalar(out=rc[:, 0, :], in0=iota_f[:, 0:64], scalar1=wcol[:, 1:2], scalar2=1.0 / TWO_PI,
                                                  op0=ALU.mult, op1=ALU.mult), [iota_f, wcol], [rc])
            S.op("dve", lambda e: e.tensor_scalar(out=rc[:, 1, :], in0=iota_f[:, 0:64], scalar1=wcol[:, 2:3], scalar2=1.0 / TWO_PI,
                                                  op0=ALU.mult, op1=ALU.mult), [iota_f, wcol], [rc])
            S.op("dve", lambda e: e.tensor_tensor(out=ang[:], in0=rc[:, 0, :].unsqueeze(2).to_broadcast([128, 64, 64]),
                                                  in1=rc[:, 1, :].unsqueeze(1).to_broadcast([128, 64, 64]), op=ALU.add), [rc], [ang])
            angf = ang[:].rearrange("p a b -> p (a b)")
            for tab, off in ((sinT, 0.0), (cosT, 0.25)):
                S.op("dve", lambda e, off=off: e.tensor_scalar(out=t1[:], in0=angf, scalar1=off, scalar2=MAGIC, op0=ALU.add, op1=ALU.add), [ang], [t1])
                S.op("dve", lambda e: e.tensor_scalar(out=t1[:], in0=t1[:], scalar1=-MAGIC, scalar2=None, op0=ALU.add), [t1], [t1])
                S.op("dve", lambda e, off=off: e.scalar_tensor_tensor(out=t2[:], in0=angf, scalar=off, in1=t1[:], op0=ALU.add, op1=ALU.subtract),
                     [ang, t1], [t2])
                S.op("act", lambda e, tab=tab: e.activation(out=tab[:], in_=t2[:], func=AF.Sin, scale=TWO_PI * 0.999999), [t2], [tab])
            S.barrier()

        xring = Ring(S, "xt", [128, D], F32, 3, ph)
        xnring = Ring(S, "xn", [128, D], BF16, 2, ph)
        junk = S.sb("junk", [128, D], BF16, ph)
        stat = S.sb("stat", [128, NT, 4], F32, ph)
        for i in range(NT):
            xt = xring.next(); xn = xnring.next()
            src = A["ctx"][i * 128:(i + 1) * 128, :] if i < 2 else A["x"][(i - 2) * 128:(i - 1) * 128, :]
            srcb = DB["ctx"] if i < 2 else DB["x"]
            S.dma("sp", lambda e, xt=xt, src=src: e.dma_start(out=xt[:], in_=src), [srcb], [xt], xt)
            S.op("act", lambda e, xt=xt, i=i: e.activation(out=junk[:], in_=xt[:], func=AF.Square, accum_out=stat[:, i, 0:1]), [xt], [junk, stat])
            S.op("act", lambda e, i=i: e.activation(out=stat[:, i, 1:2], in_=stat[:, i, 0:1], func=AF.Sqrt, scale=1.0 / D, bias=EPS), [stat], [stat])
            S.op("dve", lambda e, i=i: e.reciprocal(out=stat[:, i, 2:3], in_=stat[:, i, 1:2]), [stat], [stat])
            S.op("dve", lambda e, xt=xt, xn=xn, i=i: e.tensor_scalar(out=xn[:], in0=xt[:], scalar1=stat[:, i, 2:3], scalar2=None, op0=ALU.mult),
                 [xt, stat], [xn])
            pt = PB[i % 2]
            ptb = pt.t.bitcast(BF16)
            for dt_ in range(8):
                S.op("pe", lambda e, ptb=ptb, xn=xn, dt_=dt_: e.transpose(ptb[:, dt_ * 128:(dt_ + 1) * 128], xn[:, dt_ * 128:(dt_ + 1) * 128], ident_bf[:]),
                     [xn, ident_bf], [pt])
            mi = 2 if i < 2 else 0
            pv = ptb.rearrange("p (a b) -> p a b", a=8)
            hv = hT[:, :, i * 128:(i + 1) * 128]
            S.op("dve", lambda e, pv=pv, hv=hv, mi=mi: e.tensor_tensor(out=hv, in0=pv, in1=modc[:, mi, :].unsqueeze(2).to_broadcast([128, 8, 128]), op=ALU.mult),
                 [pt, modc], [hT])
            S.op("dve", lambda e, hv=hv, mi=mi: e.tensor_tensor(out=hv, in0=hv, in1=modc[:, mi + 1, :].unsqueeze(2).to_broadcast([128, 8, 128]), op=ALU.add),
                 [hT, modc], [hT])
        if dbg and stage == 1:
            d1 = dbg_tensor("hT", [128, 8 * NT * 128], BF16)
            S.dma("sp", lambda e: e.dma_start(out=d1, in_=hT[:].rearrange("p a b -> p (a b)")), [hT], [DB["dbg"]], hT, concurrent=True)
            S.barrier()
            return
        S.barrier()

        U_d = K["U_d"]; QT_d = K["QT_d"]; KT_d = K["KT_d"]; V_d = K["V_d"]; SG_d = K["SG_d"]
        wv = A["w_in"].rearrange("(k p) n -> p k n", p=128)
        wring = Ring(S, "win", [128, 8, 512], BF16, 2, ph)
        wsw = S.sb("wsw", [128, 8, 512], BF16, ph)
        oring = Ring(S, "o2", [128, 512], BF16, 4, ph)
        tring = Ring(S, "t32", [128, 512], F32, 4, ph)
        bank_i = [0]

        def pbank():
            b = PB[2 + (bank_i[0] % 6)]
            bank_i[0] += 1
            return b

        def load_w(c0):
            wt = wring.next()
            S.dma("pool", lambda e: e.dma_start(out=wt[:], in_=wv[:, :, c0:c0 + 512]), [DB["w_in"]], [wt], wt)
            return wt

        def tok_major(c0, dst_rows):
            wt = load_w(c0)
            for i in range(NT):
                pb = pbank()
                for kt in range(8):
                    S.op("pe", lambda e, pb=pb, kt=kt, i=i: e.matmul(pb[:, :], lhsT=hT[:, kt, i * 128:(i + 1) * 128], rhs=wt[:, kt, :],
                                                                    start=(kt == 0), stop=(kt == 7)), [hT, wt], [pb])
                ob = oring.next()
                S.op("act", lambda e, pb=pb, ob=ob: e.activation(out=ob[:], in_=pb[:, :], func=AF.Copy), [pb], [ob])
                for (dap, dbuf) in dst_rows(i):
                    S.dma("sp", lambda e, dap=dap, ob=ob: e.dma_start(out=dap, in_=ob[:]), [ob], [dbuf], ob, concurrent=True)

        wt_u = load_w(0)
        ucm_ring = Ring(S, "ucm", [128, 32, 8, 16], BF16, 2, ph)
        for ct in range(5):
            nchunk = 32 if ct == 0 else 128
            t0_ = 0 if ct == 0 else LC + (ct - 1) * 1024
            ucm = ucm_ring.next()
            for j in range(8):
                pb = pbank()
                for kt in range(8):
                    lh = hT[:, kt, t0_:t0_ + nchunk * 8].rearrange("p (c j) -> p c j", j=8)[:, :, j]
                    S.op("pe", lambda e, pb=pb, kt=kt, lh=lh, nchunk=nchunk: e.matmul(pb[0:nchunk, :], lhsT=lh, rhs=wt_u[:, kt, :], start=(kt == 0), stop=(kt == 7)),
                         [hT, wt_u], [pb])
                if j % 2 == 0:
                    S.op("act", lambda e, pb=pb, ucm=ucm, j=j, nchunk=nchunk: e.activation(out=ucm[0:nchunk, :, j, :], in_=pb[0:nchunk, :].rearrange("p (g h) -> p g h", h=16), func=AF.Copy),
                         [pb], [ucm])
                else:
                    S.op("dve", lambda e, pb=pb, ucm=ucm, j=j, nchunk=nchunk: e.tensor_copy(out=ucm[0:nchunk, :, j, :], in_=pb[0:nchunk, :].rearrange("p (g h) -> p g h", h=16)),
                         [pb], [ucm])
            S.dma("sp", lambda e, ucm=ucm, ct=ct, nchunk=nchunk: e.dma_start(out=U_d[ct, 0:nchunk, :], in_=ucm[0:nchunk, :, :, :].rearrange("p g j h -> p (g j h)")),
                  [ucm], [DB["U"]], ucm, concurrent=True)
        tok_major(1536, lambda i: [(V_d[i * 128:(i + 1) * 128, :], DB["V"])])

        def rope_block(c0, dstT, dbuf, with_ctx):
            wt = load_w(c0)
            wtv = wt[:].rearrange("p k (a two h) -> p k a two h", two=2, h=16)
            wsv = wsw[:].rearrange("p k (a two h) -> p k a two h", two=2, h=16)
            S.op("pool", lambda e: e.tensor_scalar(out=wsv[:, :, :, 0, :], in0=wtv[:, :, :, 1, :], scalar1=-1.0, scalar2=None, op0=ALU.mult), [wt], [wsw])
            S.op("pool", lambda e: e.tensor_copy(out=wsv[:, :, :, 1, :], in_=wtv[:, :, :, 0, :]), [wt], [wsw])
            for ft in range(4):
                if with_ctx:
                    pb = pbank()
                    for kt in range(8):
                        S.op("pe", lambda e, pb=pb, kt=kt, ft=ft: e.matmul(pb[:, 0:LC], lhsT=wt[:, kt, ft * 128:(ft + 1) * 128], rhs=hT[:, kt, 0:LC],
                                                                         start=(kt == 0), stop=(kt == 7)), [hT, wt], [pb])
                    ob = oring.next()
                    S.op("act", lambda e, pb=pb, ob=ob: e.activation(out=ob[:, 0:LC], in_=pb[:, 0:LC], func=AF.Copy), [pb], [ob])
                    S.dma("sp", lambda e, ob=ob, ft=ft: e.dma_start(out=dstT[ft * 128:(ft + 1) * 128, 0:LC], in_=ob[:, 0:LC]), [ob], [dbuf], ob, concurrent=True)
                coff = LC if with_ctx else 0
                for tb in range(8):
                    p1 = pbank(); p2 = pbank()
                    t0_ = LC + tb * 512
                    for (pp, ww) in ((p1, wt), (p2, wsw)):
                        for kt in range(8):
                            S.op("pe", lambda e, pp=pp, ww=ww, kt=kt, ft=ft, t0_=t0_: e.matmul(pp[:, :], lhsT=ww[:, kt, ft * 128:(ft + 1) * 128],
                                                                                              rhs=hT[:, kt, t0_:t0_ + 512], start=(kt == 0), stop=(kt == 7)),
                                 [hT, ww], [pp])
                    ta = tring.next(); tb_ = tring.next(); ob = oring.next()
                    S.op("dve", lambda e, p1=p1, ta=ta, tb=tb: e.tensor_tensor(out=ta[:], in0=p1[:, :], in1=cosT[:, tb * 512:(tb + 1) * 512], op=ALU.mult),
                         [p1, cosT], [ta])
                    S.op("dve", lambda e, p2=p2, tb_=tb_, tb=tb: e.tensor_tensor(out=tb_[:], in0=p2[:, :], in1=sinT[:, tb * 512:(tb + 1) * 512], op=ALU.mult),
                         [p2, sinT], [tb_])
                    S.op("pool", lambda e, ta=ta, tb_=tb_, ob=ob: e.tensor_tensor(out=ob[:], in0=ta[:], in1=tb_[:], op=ALU.add), [ta, tb_], [ob])
                    S.dma("sp", lambda e, ob=ob, ft=ft, tb=tb, coff=coff: e.dma_start(out=dstT[ft * 128:(ft + 1) * 128, coff + tb * 512:coff + (tb + 1) * 512], in_=ob[:]),
                          [ob], [dbuf], ob, concurrent=True)

        rope_block(512, QT_d, DB["QT"], False)
        rope_block(1024, KT_d, DB["KT"], True)

        for j in range(4):
            wt = load_w(2048 + j * 512)
            for ft in range(4):
                for tb in range(8):
                    pb = pbank()
                    t0_ = LC + tb * 512
                    for kt in range(8):
                        S.op("pe", lambda e, pb=pb, kt=kt, ft=ft, t0_=t0_, wt=wt: e.matmul(pb[:, :], lhsT=wt[:, kt, ft * 128:(ft + 1) * 128],
                                                                                          rhs=hT[:, kt, t0_:t0_ + 512], start=(kt == 0), stop=(kt == 7)),
                             [hT, wt], [pb])
                    ob = oring.next()
                    S.op("act", lambda e, pb=pb, ob=ob: e.activation(out=ob[:], in_=pb[:, :], func=AF.Sigmoid), [pb], [ob])
                    r0 = (j * 4 + ft) * 128
                    S.dma("sp", lambda e, ob=ob, r0=r0, tb=tb: e.dma_start(out=SG_d[r0:r0 + 128, tb * 512:(tb + 1) * 512], in_=ob[:]), [ob], [DB["SG"]], ob,
                          concurrent=True)
        S.barrier()
        if dbg and stage == 2:
            stg = Ring(S, "dstg", [128, 2176], BF16, 2, ph)
            for (nm, src, dbk, rows_, cols_) in (("V", V_d, "V", LC + L, 512), ("QT", QT_d, "QT", 512, L),
                                               ("KT", KT_d, "KT", 512, LC + L), ("SG", SG_d, "SG", 2048, L)):
                dd = dbg_tensor(nm, [rows_, cols_], BF16)
                for r0 in range(0, rows_, 128):
                    for c0 in range(0, cols_, 2176):
                        cw = min(2176, cols_ - c0)
                        sg = stg.next()
                        S.dma("sp", lambda e, sg=sg, src=src, r0=r0, c0=c0, cw=cw: e.dma_start(out=sg[:, 0:cw], in_=src[r0:r0 + 128, c0:c0 + cw]), [DB[dbk]], [sg], sg)
                        S.dma("sp", lambda e, sg=sg, dd=dd, r0=r0, c0=c0, cw=cw: e.dma_start(out=dd[r0:r0 + 128, c0:c0 + cw], in_=sg[:, 0:cw]), [sg], [DB["dbg"]], sg,
                              concurrent=True)
            S.barrier()


def phase3(nc, S, A, DB, PB, K, stage, dbg, dbg_tensor):
    ident_bf = K["ident_bf"]; ident_f = K["ident_f"]; iota_f = K["iota_f"]
    U_d = K["U_d"]; YA_d = K["YA_d"]
    YT_d = nc.dram_tensor("YT_d", [512, L], BF16, kind="Internal").ap()
    DB_YT = Buf("D_YT")
    NCH = 544
    with ExitStack() as ph:
        M_bf = S.sb("M_bf", [128, 32, 128], BF16, ph)
        W1_bf = S.sb("W1_bf", [128, 64, 2, 64], BF16, ph)
        W3_bf = S.sb("W3_bf", [64, 64, 2, 128], BF16, ph)
        PW = S.sb("PW", [64, 64, 2, 29], F32, ph)
        PWr = S.sb("PWr", [64, 32, 2, 16], F32, ph)
        TAUS = list(range(9)) + [8 * k for k in range(2, 17)]
        with ExitStack() as p0:
            nat = S.sb("nat", [64, 2, 64], F32, p0)
            lamT = S.sb("lamT", [64, 2, 64], F32, p0)
            dtb = S.sb("dtb", [64, 64], F32, p0)
            sm = S.sb("sm", [64, 8, 64], F32, p0)
            Bb = S.sb("Bb", [64, 2, 64, 16], F32, p0)
            XC = S.sb("XC", [64, 2, 64, 9, 16], BF16, p0)
            Dcol = S.sb("Dcol", [128, 32], F32, p0)
            pA = ExitStack()
            xr = S.sb("xr", [64, 64], F32, pA)
            an = S.sb("an", [64, 64], F32, pA)
            tau = S.sb("tau", [64, 29], F32, pA)
            PH = S.sb("PH", [64, 64, 29], F32, pA)
            Y1 = S.sb("Y1", [64, 64, 29], F32, pA)
            Y2 = S.sb("Y2", [64, 64, 29], F32, pA)
            MG = S.sb("MG", [64, 64, 29], F32, pA)
            S.dma("sp", lambda e: e.dma_start(out=nat[:, 0, :], in_=A["s5_lam_re"]), [DB["s5p"]], [nat], nat)
            S.dma("sp", lambda e: e.dma_start(out=nat[:, 1, :], in_=A["s5_lam_im"]), [DB["s5p"]], [nat], nat, concurrent=True)
            S.dma("sp", lambda e: e.dma_start(out=dtb[:], in_=A["s5_log_dt"].partition_broadcast(64)), [DB["s5p"]], [dtb], dtb)
            for s_ in range(8):
                S.dma("sp", lambda e, s_=s_: e.dma_start(out=Dcol[16 * s_:16 * s_ + 16, :], in_=A["s5_d"][0, :].rearrange("(g h) -> h g", h=16),
                                                         allow_slow_non_contiguous=True), [DB["s5p"]], [Dcol], Dcol, concurrent=(s_ > 0))
            pb = PB[0]
            for ri in range(2):
                S.op("pe", lambda e, ri=ri: e.transpose(pb[0:64, ri * 64:(ri + 1) * 64], nat[:, ri, :], ident_f[0:64, 0:64]), [nat, ident_f], [pb])
            S.op("dve", lambda e: e.tensor_copy(out=lamT[:].rearrange("p a b -> p (a b)"), in_=pb[0:64, 0:128]), [pb], [lamT])
            S.op("dve", lambda e: e.tensor_scalar(out=lamT[:, 0, :], in0=lamT[:, 0, :], scalar1=-1e-4, scalar2=None, op0=ALU.min), [lamT], [lamT])
            S.op("act", lambda e: e.activation(out=dtb[:], in_=dtb[:], func=AF.Exp), [dtb], [dtb])
            S.op("dve", lambda e: e.tensor_tensor(out=xr[:], in0=lamT[:, 0, :], in1=dtb[:], op=ALU.mult), [lamT, dtb], [xr])
            S.op("dve", lambda e: e.tensor_scalar(out=an[:], in0=lamT[:, 1, :], scalar1=dtb[:, 0:1] if False else 1.0 / TWO_PI, scalar2=None, op0=ALU.mult), [lamT], [an])
            S.op("dve", lambda e: e.tensor_tensor(out=an[:], in0=an[:], in1=dtb[:], op=ALU.mult), [an, dtb], [an])
            S.op("dve", lambda e: e.tensor_copy(out=tau[:, 0:9], in_=iota_f[0:64, 0:9]), [iota_f], [tau])
            S.op("dve", lambda e: e.tensor_scalar(out=tau[:, 9:24], in0=iota_f[0:64, 2:17], scalar1=8.0, scalar2=None, op0=ALU.mult), [iota_f], [tau])
            for j_ in range(5):
                S.op("dve", lambda e, j_=j_: e.memset(tau[:, 24 + j_:25 + j_], float(256 * (2 ** j_))), [], [tau])
            bc_dg = lambda t: t[:].unsqueeze(2).to_broadcast([64, 64, 29])
            bc_tau = tau[:].unsqueeze(1).to_broadcast([64, 64, 29])
            S.op("dve", lambda e: e.tensor_tensor(out=PH[:], in0=bc_dg(an), in1=bc_tau, op=ALU.mult), [an, tau], [PH])
            S.op("dve", lambda e: e.tensor_tensor(out=MG[:], in0=bc_dg(xr), in1=bc_tau, op=ALU.mult), [xr, tau], [MG])
            S.op("act", lambda e: e.activation(out=MG[:], in_=MG[:], func=AF.Exp), [MG], [MG])
            for ri, off in ((0, 0.25), (1, 0.0)):
                S.op("dve", lambda e, off=off: e.tensor_scalar(out=Y1[:], in0=PH[:], scalar1=off, scalar2=MAGIC, op0=ALU.add, op1=ALU.add), [PH], [Y1])
                S.op("dve", lambda e: e.tensor_scalar(out=Y1[:], in0=Y1[:], scalar1=-MAGIC, scalar2=None, op0=ALU.add), [Y1], [Y1])
                S.op("dve", lambda e, off=off: e.scalar_tensor_tensor(out=Y2[:], in0=PH[:], scalar=off, in1=Y1[:], op0=ALU.add, op1=ALU.subtract), [PH, Y1], [Y2])
                S.op("act", lambda e: e.activation(out=Y2[:], in_=Y2[:], func=AF.Sin, scale=TWO_PI * 0.999999), [Y2], [Y2])
                S.op("dve", lambda e, ri=ri: e.tensor_tensor(out=PW[:, :, ri, :], in0=Y2[:], in1=MG[:], op=ALU.mult), [Y2, MG], [PW])
            for k_ in range(16):
                S.op("dve", lambda e, k_=k_: e.tensor_copy(out=PWr[:, :, :, k_], in_=PW[:, 32:64, :, 8 + 15 - k_]), [PW], [PWr])
            S.barrier(); pA.close()
            lr = lamT[:, 0, :]; li = lamT[:, 1, :]
            lbr = PW[:, :, 0, 1]; lbi = PW[:, :, 1, 1]
            den, rden, nre, cre, cim, t_a, t_b = (sm[:, i, :] for i in range(7))
            S.op("dve", lambda e: e.tensor_tensor(out=den, in0=lr, in1=lr, op=ALU.mult), [lamT], [sm])
            S.op("dve", lambda e: e.tensor_tensor(out=t_a, in0=li, in1=li, op=ALU.mult), [lamT], [sm])
            S.op("dve", lambda e: e.tensor_tensor(out=den, in0=den, in1=t_a, op=ALU.add), [sm], [sm])
            S.op("dve", lambda e: e.reciprocal(out=rden, in_=den), [sm], [sm])
            S.op("dve", lambda e: e.tensor_scalar(out=nre, in0=lbr, scalar1=-1.0, scalar2=None, op0=ALU.add), [PW], [sm])
            S.op("dve", lambda e: e.tensor_tensor(out=t_a, in0=nre, in1=lr, op=ALU.mult), [sm, lamT], [sm])
            S.op("dve", lambda e: e.tensor_tensor(out=t_b, in0=lbi, in1=li, op=ALU.mult), [PW, lamT], [sm])
            S.op("dve", lambda e: e.tensor_tensor(out=t_a, in0=t_a, in1=t_b, op=ALU.add), [sm], [sm])
            S.op("dve", lambda e: e.tensor_tensor(out=cre, in0=t_a, in1=rden, op=ALU.mult), [sm], [sm])
            S.op("dve", lambda e: e.tensor_tensor(out=t_a, in0=lbi, in1=lr, op=ALU.mult), [PW, lamT], [sm])
            S.op("dve", lambda e: e.tensor_tensor(out=t_b, in0=nre, in1=li, op=ALU.mult), [sm, lamT], [sm])
            S.op("dve", lambda e: e.tensor_tensor(out=t_a, in0=t_a, in1=t_b, op=ALU.subtract), [sm], [sm])
            S.op("dve", lambda e: e.tensor_tensor(out=cim, in0=t_a, in1=rden, op=ALU.mult), [sm], [sm])
            bch = lambda t: t.unsqueeze(2).to_broadcast([64, 64, 16])
            pB = ExitStack()
            Bn = S.sb("Bn", [64, 2, 64, 16], F32, pB)
            tb1 = S.sb("tb1", [64, 64, 16], F32, pB)
            for ri, nm in enumerate(("s5_b_re", "s5_b_im")):
                S.dma("sp", lambda e, ri=ri, nm=nm: e.dma_start(out=Bn[:, ri, :, :], in_=A[nm].rearrange("a n h -> n a h")), [DB["s5p"]], [Bn], Bn,
                      concurrent=(ri > 0))
            S.op("dve", lambda e: e.tensor_tensor(out=Bb[:, 0, :, :], in0=Bn[:, 0, :, :], in1=bch(cre), op=ALU.mult), [Bn, sm], [Bb])
            S.op("dve", lambda e: e.tensor_tensor(out=tb1[:], in0=Bn[:, 1, :, :], in1=bch(cim), op=ALU.mult), [Bn, sm], [tb1])
            S.op("dve", lambda e: e.tensor_tensor(out=Bb[:, 0, :, :], in0=Bb[:, 0, :, :], in1=tb1[:], op=ALU.subtract), [Bb, tb1], [Bb])
            S.op("dve", lambda e: e.tensor_tensor(out=Bb[:, 1, :, :], in0=Bn[:, 1, :, :], in1=bch(cre), op=ALU.mult), [Bn, sm], [Bb])
            S.op("dve", lambda e: e.tensor_tensor(out=tb1[:], in0=Bn[:, 0, :, :], in1=bch(cim), op=ALU.mult), [Bn, sm], [tb1])
            S.op("dve", lambda e: e.tensor_tensor(out=Bb[:, 1, :, :], in0=Bb[:, 1, :, :], in1=tb1[:], op=ALU.add), [Bb, tb1], [Bb])
            S.barrier(); pB.close()
            pC = ExitStack()
            cnat = S.sb("cnat", [128, 2, 2, 4, 64], F32, pC)
            CT = S.sb("CT", [64, 2, 64, 16], F32, pC)
            tx = S.sb("tx", [64, 32, 9, 16], F32, pC)
            tx2 = S.sb("tx2", [64, 32, 9, 16], F32, pC)
            for ri, nm in enumerate(("s5_c_re", "s5_c_im")):
                for d_ in range(2):
                    S.dma("sp", lambda e, ri=ri, nm=nm, d_=d_: e.dma_start(out=cnat[:, ri, d_, :, :], in_=A[nm][d_].rearrange("(t p) n -> p t n", p=128)),
                          [DB["s5p"]], [cnat], cnat, concurrent=(ri + d_ > 0))
            for ri in range(2):
                for d_ in range(2):
                    pb = PB[1 + ((ri * 2 + d_) % 2)]
                    for t4 in range(4):
                        S.op("pe", lambda e, pb=pb, ri=ri, d_=d_, t4=t4: e.transpose(pb[0:64, t4 * 128:(t4 + 1) * 128], cnat[:, ri, d_, t4, :], ident_f[:]),
                             [cnat, ident_f], [pb])
                    S.op("act", lambda e, pb=pb, ri=ri, d_=d_: e.activation(out=CT[:, ri, d_ * 32:(d_ + 1) * 32, :].rearrange("p a h -> p (a h)"), in_=pb[0:64, :], func=AF.Copy),
                         [pb], [CT])
            for d_ in range(2):
                dsl = slice(d_ * 32, (d_ + 1) * 32)
                cb = lambda ri, dsl=dsl: CT[:, ri, dsl, :].unsqueeze(2).to_broadcast([64, 32, 9, 16])
                pwb = lambda ri, dsl=dsl: PW[:, dsl, ri, 0:9].unsqueeze(3).to_broadcast([64, 32, 9, 16])
                S.op("dve", lambda e, cb=cb, pwb=pwb: e.tensor_tensor(out=tx[:], in0=cb(0), in1=pwb(0), op=ALU.mult), [CT, PW], [tx])
                S.op("pool", lambda e, cb=cb, pwb=pwb: e.tensor_tensor(out=tx2[:], in0=cb(1), in1=pwb(1), op=ALU.mult), [CT, PW], [tx2])
                S.op("dve", lambda e, dsl=dsl: e.tensor_tensor(out=XC[:, 0, dsl, :, :], in0=tx[:], in1=tx2[:], op=ALU.subtract), [tx, tx2], [XC])
                S.op("dve", lambda e, cb=cb, pwb=pwb: e.tensor_tensor(out=tx[:], in0=cb(0), in1=pwb(1), op=ALU.mult), [CT, PW], [tx])
                S.op("pool", lambda e, cb=cb, pwb=pwb: e.tensor_tensor(out=tx2[:], in0=cb(1), in1=pwb(0), op=ALU.mult), [CT, PW], [tx2])
                S.op("dve", lambda e, dsl=dsl: e.scalar_tensor_tensor(out=XC[:, 1, dsl, :, :], in0=tx[:], scalar=-1.0, in1=tx2[:], op0=ALU.mult, op1=ALU.subtract), [tx, tx2], [XC])
            S.barrier(); pC.close()
            for ri in range(2):
                S.op("act", lambda e, ri=ri: e.activation(out=W3_bf[:, 0:32, ri, :].rearrange("p a (j h) -> p a j h", h=16), in_=XC[:, ri, 0:32, 1:9, :], func=AF.Copy), [XC], [W3_bf])
                for j in range(8):
                    S.op("pool", lambda e, ri=ri, j=j: e.tensor_copy(out=W3_bf[:, 32:64, ri, j * 16:(j + 1) * 16], in_=XC[:, ri, 32:64, 8 - j, :]), [XC], [W3_bf])
            pD = ExitStack()
            W1T = S.sb("W1T", [64, 32, 2, 128], BF16, pD)
            tq = S.sb("tq", [64, 32, 8, 16], F32, pD)
            tq2 = S.sb("tq2", [64, 32, 8, 16], F32, pD)
            PWj = S.sb("PWj", [64, 64, 2, 8], F32, pD)
            for j in range(8):
                S.op("dve", lambda e, j=j: e.tensor_copy(out=PWj[:, 0:32, :, j], in_=PW[:, 0:32, :, 7 - j]), [PW], [PWj])
                S.op("dve", lambda e, j=j: e.tensor_copy(out=PWj[:, 32:64, :, j], in_=PW[:, 32:64, :, j]), [PW], [PWj])
            for d_ in range(2):
                dsl = slice(d_ * 32, (d_ + 1) * 32)
                pj = lambda ri, dsl=dsl: PWj[:, dsl, ri, :].unsqueeze(3).to_broadcast([64, 32, 8, 16])
                bj = lambda ri, dsl=dsl: Bb[:, ri, dsl, :].unsqueeze(2).to_broadcast([64, 32, 8, 16])
                w1v = lambda ri: W1T[:, :, ri, :].rearrange("p a (j h) -> p a j h", h=16)
                S.op("dve", lambda e, pj=pj, bj=bj: e.tensor_tensor(out=tq[:], in0=pj(0), in1=bj(0), op=ALU.mult), [PWj, Bb], [tq])
                S.op("pool", lambda e, pj=pj, bj=bj: e.tensor_tensor(out=tq2[:], in0=pj(1), in1=bj(1), op=ALU.mult), [PWj, Bb], [tq2])
                S.op("dve", lambda e, w1v=w1v: e.tensor_tensor(out=w1v(0), in0=tq[:], in1=tq2[:], op=ALU.subtract), [tq, tq2], [W1T])
                S.op("dve", lambda e, pj=pj, bj=bj: e.tensor_tensor(out=tq[:], in0=pj(0), in1=bj(1), op=ALU.mult), [PWj, Bb], [tq])
                S.op("pool", lambda e, pj=pj, bj=bj: e.tensor_tensor(out=tq2[:], in0=pj(1), in1=bj(0), op=ALU.mult), [PWj, Bb], [tq2])
                S.op("dve", lambda e, w1v=w1v: e.tensor_tensor(out=w1v(1), in0=tq[:], in1=tq2[:], op=ALU.add), [tq, tq2], [W1T])
                for g in range(32):
                    dg = d_ * 32 + g
                    pb = PB[dg % 2]
                    pbb = pb.t.bitcast(BF16)
                    for ri in range(2):
                        S.op("pe", lambda e, pbb=pbb, g=g, ri=ri: e.transpose(pbb[:, ri * 64:(ri + 1) * 64], W1T[:, g, ri, :], ident_bf[0:64, 0:64]), [W1T, ident_bf], [pb])
                    if dg % 2 == 0:
                        S.op("act", lambda e, pbb=pbb, dg=dg: e.activation(out=W1_bf[:, dg, :, :].rearrange("p a b -> p (a b)"), in_=pbb[:, 0:128], func=AF.Copy), [pb], [W1_bf])
                    else:
                        S.op("dve", lambda e, pbb=pbb, dg=dg: e.tensor_copy(out=W1_bf[:, dg, :, :].rearrange("p a b -> p (a b)"), in_=pbb[:, 0:128]), [pb], [W1_bf])
            S.barrier(); pD.close()
            Bpad = [S.sb("Bpad%d" % i, [64, 2, 2, 240], BF16, p0) for i in range(2)]
            Xpad = [S.sb("Xpad%d" % i, [64, 2, 2, 240], BF16, p0) for i in range(2)]
            for i in range(2):
                S.op("pool", lambda e, i=i: e.memset(Bpad[i][:], 0.0), [], [Bpad[i]])
                S.op("pool", lambda e, i=i: e.memset(Xpad[i][:], 0.0), [], [Xpad[i]])
            for g in range(32):
                bp = Bpad[g % 2]; xp = Xpad[g % 2]
                S.op("pool", lambda e, bp=bp, g=g: e.tensor_copy(out=bp[:, 0, :, 112:128], in_=Bb[:, :, g, :]), [Bb], [bp])
                S.op("pool", lambda e, bp=bp, g=g: e.tensor_copy(out=bp[:, 1, :, 112:128], in_=Bb[:, :, 32 + g, :]), [Bb], [bp])
                S.op("act", lambda e, xp=xp, g=g: e.activation(out=xp[:, 0, :, 112:240].rearrange("p r (t h) -> p r t h", h=16), in_=XC[:, :, g, 0:8, :], func=AF.Copy), [XC], [xp])
                for i_ in range(8):
                    S.op("pool", lambda e, xp=xp, g=g, i_=i_: e.tensor_copy(out=xp[:, 1, :, i_ * 16:(i_ + 1) * 16], in_=XC[:, :, 32 + g, 7 - i_, :]), [XC], [xp])
                pb = PB[2 + (g % 2)]
                n_mm = 0
                for d_ in range(2):
                    for ri in range(2):
                        for s_ in range(8):
                            w0 = (7 - s_) * 16
                            S.op("pe", lambda e, pb=pb, bp=bp, xp=xp, d_=d_, ri=ri, w0=w0, n_mm=n_mm: e.matmul(
                                pb[:, 0:128], lhsT=bp[:, d_, ri, w0:w0 + 128], rhs=xp[:, d_, ri, w0:w0 + 128], start=(n_mm == 0), stop=(n_mm == 31)),
                                [bp, xp], [pb])
                            n_mm += 1
                S.op("dve", lambda e, pb=pb, g=g: e.scalar_tensor_tensor(out=M_bf[:, g, :], in0=ident_f[:], scalar=Dcol[:, g:g + 1], in1=pb[:, 0:128],
                                                                       op0=ALU.mult, op1=ALU.add), [pb, ident_f, Dcol], [M_bf])
            S.barrier()
        uT_all = S.sb("uT_all", [128, 32, NCH], BF16, ph)
        if dbg and stage == 30:
            for nm, t_, shp in (("M", M_bf, [128, 32 * 128]), ("W1", W1_bf, [128, 64 * 128])):
                dd = dbg_tensor(nm, shp, BF16)
                S.dma("sp", lambda e, dd=dd, t_=t_: e.dma_start(out=dd, in_=t_[:].rearrange("p a b -> p (a b)") if len(t_.t.shape) == 3 else t_[:].rearrange("p a b c -> p (a b c)")),
                      [t_], [DB["dbg"]], t_, concurrent=True)
            dd = dbg_tensor("W3", [64, 64 * 256], BF16)
            S.dma("sp", lambda e: e.dma_start(out=dd, in_=W3_bf[:].rearrange("p a b c -> p (a b c)")), [W3_bf], [DB["dbg"]], W3_bf, concurrent=True)
            dd2 = dbg_tensor("PW", [64, 64 * 48], F32)
            S.dma("sp", lambda e: e.dma_start(out=dd2, in_=PW[:].rearrange("p a b c -> p (a b c)")), [PW], [DB["dbg"]], PW, concurrent=True)
            S.barrier()
            return
        with ExitStack() as pU:
            uc = S.sb("uc", [128, 5, 4096], BF16, pU)
            S.dma("sp", lambda e: e.dma_start(out=uc[0:32, 0, :], in_=U_d[0, 0:32, :]), [DB["U"]], [uc], uc)
            for ct in range(4):
                S.dma("sp", lambda e, ct=ct: e.dma_start(out=uc[:, 1 + ct, :], in_=U_d[1 + ct, :, :]), [DB["U"]], [uc], uc, concurrent=True)
            for g in range(32):
                pb = PB[g % 4]
                pbb = pb.t.bitcast(BF16)
                S.op("pe", lambda e, pbb=pbb, g=g: e.transpose(pbb[:, 0:32], uc[0:32, 0, g * 128:(g + 1) * 128], ident_bf[0:32, 0:32]), [uc, ident_bf], [pb])
                for ct in range(4):
                    S.op("pe", lambda e, pbb=pbb, g=g, ct=ct: e.transpose(pbb[:, 32 + ct * 128:32 + (ct + 1) * 128], uc[:, 1 + ct, g * 128:(g + 1) * 128], ident_bf[:]),
                         [uc, ident_bf], [pb])
                if g % 2 == 0:
                    S.op("act", lambda e, pbb=pbb, g=g: e.activation(out=uT_all[:, g, :], in_=pbb[:, 0:NCH], func=AF.Copy), [pb], [uT_all])
                else:
                    S.op("dve", lambda e, pbb=pbb, g=g: e.tensor_copy(out=uT_all[:, g, :], in_=pbb[:, 0:NCH]), [pb], [uT_all])
            S.barrier()
        G = 4
        Sbuf = [[S.sb("S%d%d" % (d_, ri), [64, G, NCH], F32, ph) for ri in range(2)] for d_ in range(2)]
        Hbf = [[S.sb("H%d%d" % (d_, ri), [64, G, NCH + 2], BF16, ph) for ri in range(2)] for d_ in range(2)]
        Cy = [[S.sb("Cy%d%d" % (d_, ri), [64, G, 36], F32, ph) for ri in range(2)] for d_ in range(2)]
        tl = [[S.sb("tl%d%d" % (d_, i), [64, G, 34], F32, ph) for i in range(4)] for d_ in range(2)]
        Zb = [[S.sb("Zb%d%d" % (d_, ri), [64, G, 34], F32, ph) for ri in range(2)] for d_ in range(2)]
        tf = [[S.sb("tf%d%d" % (d_, i), [64, 34, 16], F32, ph) for i in range(2)] for d_ in range(2)]
        ytm = [S.sb("ytm%d" % i, [128, 4, 8, 128], BF16, ph) for i in range(1)]
        yTs = [S.sb("yTs%d" % i, [128, L], BF16, ph) for i in range(1)]
        for d_ in range(2):
            for ri in range(2):
                S.op("pool", lambda e, d_=d_, ri=ri: e.memset(Hbf[d_][ri][:], 0.0), [], [Hbf[d_][ri]])
                S.op("pool", lambda e, d_=d_, ri=ri: e.memset(Cy[d_][ri][:], 0.0), [], [Cy[d_][ri]])
        ENG_D = ("dve", "pool")
        for bt in range(8):
            for d_ in range(2):
                en = ENG_D[d_]
                Sr, Si = Sbuf[d_]
                for gl in range(G):
                    g = bt * G + gl
                    dg = d_ * 32 + g
                    for ri in range(2):
                        pa = PB[4 + ((gl * 2 + ri) % 2) * 2]
                        pb2 = PB[5 + ((gl * 2 + ri) % 2) * 2]
                        lw = W1_bf[:, dg, ri, :]
                        if d_ == 0:
                            S.op("pe", lambda e, pa=pa, lw=lw, g=g: e.matmul(pa[0:64, 0:272], lhsT=lw, rhs=uT_all[:, g, 0:272], start=True, stop=True), [W1_bf, uT_all], [pa])
                            S.op("pe", lambda e, pb2=pb2, lw=lw, g=g: e.matmul(pb2[0:64, 0:272], lhsT=lw, rhs=uT_all[:, g, 272:544], start=True, stop=True), [W1_bf, uT_all], [pb2])
                        else:
                            S.op("pe", lambda e, pa=pa, lw=lw, g=g: e.matmul(pa[0:64, 0:272], lhsT=lw, rhs=uT_all[:, g, 32:304], start=True, stop=True), [W1_bf, uT_all], [pa])
                            S.op("pe", lambda e, pb2=pb2, lw=lw, g=g: e.matmul(pb2[0:64, 0:240], lhsT=lw, rhs=uT_all[:, g, 304:544], start=True, stop=True), [W1_bf, uT_all], [pb2])
                            S.op("pe", lambda e, pb2=pb2, lw=lw, g=g: e.matmul(pb2[0:64, 240:272], lhsT=lw, rhs=uT_all[:, g, 0:32], start=True, stop=True), [W1_bf, uT_all], [pb2])
                        dst = (Sr, Si)[ri]
                        S.op("act", lambda e, pa=pa, dst=dst, gl=gl: e.activation(out=dst[:, gl, 0:272], in_=pa[0:64, 0:272], func=AF.Copy), [pa], [dst])
                        S.op("act", lambda e, pb2=pb2, dst=dst, gl=gl: e.activation(out=dst[:, gl, 272:544], in_=pb2[0:64, 0:272], func=AF.Copy), [pb2], [dst])
                gsl = slice(d_ * 32 + bt * G, d_ * 32 + bt * G + G)
                Ar = PW[:, gsl, 0, 8:9].to_broadcast([64, G, 34]); Ai = PW[:, gsl, 1, 8:9].to_broadcast([64, G, 34])
                Srv = Sr[:].rearrange("p g (s i) -> p g s i", i=16); Siv = Si[:].rearrange("p g (s i) -> p g s i", i=16)
                t1, t2, t3, t4 = tl[d_]
                steps = range(1, 16) if d_ == 0 else range(14, -1, -1)
                for i in steps:
                    ip = i - 1 if d_ == 0 else i + 1
                    S.op(en, lambda e, ip=ip: e.tensor_tensor(out=t1[:], in0=Srv[:, :, :, ip], in1=Ar, op=ALU.mult), [Sr, PW], [t1])
                    S.op(en, lambda e, ip=ip: e.tensor_tensor(out=t2[:], in0=Siv[:, :, :, ip], in1=Ai, op=ALU.mult), [Si, PW], [t2])
                    S.op(en, lambda e, ip=ip: e.tensor_tensor(out=t3[:], in0=Siv[:, :, :, ip], in1=Ar, op=ALU.mult), [Si, PW], [t3])
                    S.op(en, lambda e, ip=ip: e.tensor_tensor(out=t4[:], in0=Srv[:, :, :, ip], in1=Ai, op=ALU.mult), [Sr, PW], [t4])
                    S.op(en, lambda e: e.tensor_tensor(out=t1[:], in0=t1[:], in1=t2[:], op=ALU.subtract), [t1, t2], [t1])
                    S.op(en, lambda e: e.tensor_tensor(out=t3[:], in0=t3[:], in1=t4[:], op=ALU.add), [t3, t4], [t3])
                    S.op(en, lambda e, i=i: e.tensor_tensor(out=Srv[:, :, :, i], in0=Srv[:, :, :, i], in1=t1[:], op=ALU.add), [Sr, t1], [Sr])
                    S.op(en, lambda e, i=i: e.tensor_tensor(out=Siv[:, :, :, i], in0=Siv[:, :, :, i], in1=t3[:], op=ALU.add), [Si, t3], [Si])
                Cr, Ci = Cy[d_]
                Zr, Zi = Zb[d_]
                c1, c2, c3, c4 = tl[d_]
                if d_ == 0:
                    cur = (Srv[:, :, :, 15], Siv[:, :, :, 15]); cur_b = [Sr, Si]
                    cyv = (Cr[:, :, 1:35], Ci[:, :, 1:35])
                else:
                    cur = (Srv[:, :, :, 0], Siv[:, :, :, 0]); cur_b = [Sr, Si]
                    cyv = (Cr[:, :, 0:34], Ci[:, :, 0:34])
                zv = (Zr[:, :, :], Zi[:, :, :])
                for k_ in range(6):
                    sh = 1 << k_
                    ti_ = 23 + k_
                    n_ = 34 - sh
                    Bkr = PW[:, gsl, 0, ti_:ti_ + 1].to_broadcast([64, G, n_]); Bki = PW[:, gsl, 1, ti_:ti_ + 1].to_broadcast([64, G, n_])
                    dst = zv if k_ % 2 == 0 else cyv
                    dst_b = [Zr, Zi] if k_ % 2 == 0 else [Cr, Ci]
                    if d_ == 0:
                        src_sl = slice(0, n_); out_sl = slice(sh, 34); keep_sl = slice(0, sh)
                    else:
                        src_sl = slice(sh, 34); out_sl = slice(0, n_); keep_sl = slice(n_, 34)
                    xr_s = cur[0][:, :, src_sl]; xi_s = cur[1][:, :, src_sl]
                    S.op(en, lambda e, xr_s=xr_s, Bkr=Bkr, n_=n_: e.tensor_tensor(out=c1[:, :, 0:n_], in0=xr_s, in1=Bkr, op=ALU.mult), cur_b + [PW], [c1])
                    S.op(en, lambda e, xi_s=xi_s, Bki=Bki, n_=n_: e.tensor_tensor(out=c2[:, :, 0:n_], in0=xi_s, in1=Bki, op=ALU.mult), cur_b + [PW], [c2])
                    S.op(en, lambda e, xi_s=xi_s, Bkr=Bkr, n_=n_: e.tensor_tensor(out=c3[:, :, 0:n_], in0=xi_s, in1=Bkr, op=ALU.mult), cur_b + [PW], [c3])
                    S.op(en, lambda e, xr_s=xr_s, Bki=Bki, n_=n_: e.tensor_tensor(out=c4[:, :, 0:n_], in0=xr_s, in1=Bki, op=ALU.mult), cur_b + [PW], [c4])
                    S.op(en, lambda e, n_=n_: e.tensor_tensor(out=c1[:, :, 0:n_], in0=c1[:, :, 0:n_], in1=c2[:, :, 0:n_], op=ALU.subtract), [c1, c2], [c1])
                    S.op(en, lambda e, n_=n_: e.tensor_tensor(out=c3[:, :, 0:n_], in0=c3[:, :, 0:n_], in1=c4[:, :, 0:n_], op=ALU.add), [c3, c4], [c3])
                    S.op(en, lambda e, dst=dst, cur=cur, out_sl=out_sl, n_=n_: e.tensor_tensor(out=dst[0][:, :, out_sl], in0=cur[0][:, :, out_sl], in1=c1[:, :, 0:n_], op=ALU.add), cur_b + [c1], [dst_b[0]])
                    S.op(en, lambda e, dst=dst, cur=cur, out_sl=out_sl, n_=n_: e.tensor_tensor(out=dst[1][:, :, out_sl], in0=cur[1][:, :, out_sl], in1=c3[:, :, 0:n_], op=ALU.add), cur_b + [c3], [dst_b[1]])
                    S.op(en, lambda e, dst=dst, cur=cur, keep_sl=keep_sl: e.tensor_copy(out=dst[0][:, :, keep_sl], in_=cur[0][:, :, keep_sl]), cur_b, [dst_b[0]])
                    S.op(en, lambda e, dst=dst, cur=cur, keep_sl=keep_sl: e.tensor_copy(out=dst[1][:, :, keep_sl], in_=cur[1][:, :, keep_sl]), cur_b, [dst_b[1]])
                    cur = dst; cur_b = dst_b
                f1, f2 = tf[d_]
                Hr, Hi = Hbf[d_]
                for gl in range(G):
                    g = bt * G + gl
                    if d_ == 0:
                        Pr = PW[:, g, 0, 8:24].unsqueeze(1).to_broadcast([64, 34, 16]); Pi = PW[:, g, 1, 8:24].unsqueeze(1).to_broadcast([64, 34, 16])
                        cyr = Cr[:, gl, 0:34].unsqueeze(2).to_broadcast([64, 34, 16]); cyi = Ci[:, gl, 0:34].unsqueeze(2).to_broadcast([64, 34, 16])
                        hro = Hr[:, gl, 1:545].rearrange("p (s i) -> p s i", i=16); hio = Hi[:, gl, 1:545].rearrange("p (s i) -> p s i", i=16)
                    else:
                        Pr = PWr[:, g, 0, :].unsqueeze(1).to_broadcast([64, 34, 16]); Pi = PWr[:, g, 1, :].unsqueeze(1).to_broadcast([64, 34, 16])
                        cyr = Cr[:, gl, 1:35].unsqueeze(2).to_broadcast([64, 34, 16]); cyi = Ci[:, gl, 1:35].unsqueeze(2).to_broadcast([64, 34, 16])
                        hro = Hr[:, gl, 0:544].rearrange("p (s i) -> p s i", i=16); hio = Hi[:, gl, 0:544].rearrange("p (s i) -> p s i", i=16)
                    srv = Srv[:, gl, :, :]; siv = Siv[:, gl, :, :]
                    en_f = "dve" if (d_ == 1 and gl >= 2) else en
                    S.op(en_f, lambda e, Pr=Pr, cyr=cyr: e.tensor_tensor(out=f1[:], in0=Pr, in1=cyr, op=ALU.mult), [PW, PWr, Cr], [f1])
                    S.op(en_f, lambda e, Pi=Pi, cyi=cyi: e.tensor_tensor(out=f2[:], in0=Pi, in1=cyi, op=ALU.mult), [PW, PWr, Ci], [f2])
                    S.op(en_f, lambda e: e.tensor_tensor(out=f1[:], in0=f1[:], in1=f2[:], op=ALU.subtract), [f1, f2], [f1])
                    S.op(en_f, lambda e, hro=hro, srv=srv: e.tensor_tensor(out=hro, in0=srv, in1=f1[:], op=ALU.add), [Sr, f1], [Hr])
                    S.op(en_f, lambda e, Pr=Pr, cyi=cyi: e.tensor_tensor(out=f1[:], in0=Pr, in1=cyi, op=ALU.mult), [PW, PWr, Ci], [f1])
                    S.op(en_f, lambda e, Pi=Pi, cyr=cyr: e.tensor_tensor(out=f2[:], in0=Pi, in1=cyr, op=ALU.mult), [PW, PWr, Cr], [f2])
                    S.op(en_f, lambda e: e.tensor_tensor(out=f1[:], in0=f1[:], in1=f2[:], op=ALU.add), [f1, f2], [f1])
                    S.op(en_f, lambda e, hio=hio, siv=siv: e.tensor_tensor(out=hio, in0=siv, in1=f1[:], op=ALU.add), [Si, f1], [Hi])
            yt = ytm[0]
            for ct in range(4):
                pb = PB[ct % 4]
                for gl in range(G):
                    g = bt * G + gl
                    osl = pb[:, gl * 128:(gl + 1) * 128]
                    c0 = 32 + ct * 128
                    S.op("pe", lambda e, osl=osl, g=g, c0=c0: e.matmul(osl, lhsT=uT_all[:, g, c0:c0 + 128], rhs=M_bf[:, g, :], start=True, stop=False), [uT_all, M_bf], [pb])
                    for ri in range(2):
                        S.op("pe", lambda e, osl=osl, g=g, gl=gl, ri=ri, c0=c0: e.matmul(osl, lhsT=Hbf[0][ri][:, gl, c0:c0 + 128], rhs=W3_bf[:, g, ri, :], start=False, stop=False),
                             [Hbf[0][ri], W3_bf], [pb])
                    for ri in range(2):
                        S.op("pe", lambda e, osl=osl, g=g, gl=gl, ri=ri, ct=ct: e.matmul(osl, lhsT=Hbf[1][ri][:, gl, ct * 128 + 1:ct * 128 + 129], rhs=W3_bf[:, 32 + g, ri, :],
                                                                                         start=False, stop=(ri == 1)), [Hbf[1][ri], W3_bf], [pb])
                off = (bt % 2) * 64
                S.op("act", lambda e, pb=pb, yt=yt, ct=ct, off=off: e.activation(out=yt[:, ct, :, off:off + 64].rearrange("p j (g h) -> p j g h", h=16),
                                                                                in_=pb[:, :].rearrange("p (g j h) -> p j g h", g=4, h=16), func=AF.Gelu), [pb], [yt])
            if bt % 2 == 1:
                pair = bt // 2
                ys = yTs[0]
                for ct in range(4):
                    for jh in range(2):
                        pb = PB[4 + ((ct * 2 + jh) % 4)]
                        pbb = pb.t.bitcast(BF16)
                        for jj in range(4):
                            S.op("pe", lambda e, pbb=pbb, yt=yt, ct=ct, jh=jh, jj=jj: e.transpose(pbb[:, jj * 128:(jj + 1) * 128], yt[:, ct, jh * 4 + jj, :], ident_bf[:]),
                                 [yt, ident_bf], [pb])
                        S.op("dve", lambda e, pbb=pbb, ys=ys, ct=ct, jh=jh: e.tensor_copy(
                            out=ys[:, ct * 1024:(ct + 1) * 1024].rearrange("p (c j) -> p c j", j=8)[:, :, jh * 4:jh * 4 + 4],
                            in_=pbb[:, 0:512].rearrange("p (j c) -> p c j", j=4)), [pb], [ys])
                S.dma("sp", lambda e, ys=ys, pair=pair: e.dma_start(out=YT_d[pair * 128:(pair + 1) * 128, :], in_=ys[:]), [ys], [DB_YT], ys, concurrent=True)
        S.barrier()
    with ExitStack() as pg:
        yT = S.sb("yT", [128, 4, L], BF16, pg)
        wg = S.sb("wglu", [128, 4, 512], BF16, pg)
        oring = Ring(S, "yao", [128, 512], BF16, 3, pg)
        sring = Ring(S, "sgl", [128, 512], F32, 3, pg)
        S.dma("pool", lambda e: e.dma_start(out=wg[:], in_=A["w_glu"].rearrange("(k p) n -> p k n", p=128)), [DB["w_glu"]], [wg], wg)
        for kt in range(4):
            S.dma("sp", lambda e, kt=kt: e.dma_start(out=yT[:, kt, :], in_=YT_d[kt * 128:(kt + 1) * 128, :]), [DB_YT], [yT], yT, concurrent=(kt > 0))
        n_ = 0
        for tb in range(8):
            for co in range(4):
                pb = PB[n_ % 4]; n_ += 1
                for kt in range(4):
                    S.op("pe", lambda e, pb=pb, kt=kt, co=co, tb=tb: e.matmul(pb[:, :], lhsT=wg[:, kt, co * 128:(co + 1) * 128], rhs=yT[:, kt, tb * 512:(tb + 1) * 512],
                                                                            start=(kt == 0), stop=(kt == 3)), [wg, yT], [pb])
                sg = sring.next(); ob = oring.next()
                S.op("act", lambda e, pb=pb, sg=sg: e.activation(out=sg[:], in_=pb[:, :], func=AF.Sigmoid), [pb], [sg])
                S.op("dve", lambda e, sg=sg, ob=ob, co=co, tb=tb: e.tensor_tensor(out=ob[:], in0=sg[:], in1=yT[:, co, tb * 512:(tb + 1) * 512], op=ALU.mult), [sg, yT], [ob])
                S.dma("sp", lambda e, ob=ob, co=co, tb=tb: e.dma_start(out=YA_d[co * 128:(co + 1) * 128, tb * 512:(tb + 1) * 512], in_=ob[:]), [ob], [DB["YA"]], ob, concurrent=True)
        S.barrier()
        if dbg and stage == 3:
            dd = dbg_tensor("YA", [512, L], BF16)
            stg = Ring(S, "dstg3", [128, L], BF16, 2, pg)
            for r0 in range(0, 512, 128):
                sg_ = stg.next()
                S.dma("sp", lambda e, sg_=sg_, r0=r0: e.dma_start(out=sg_[:], in_=YA_d[r0:r0 + 128, :]), [DB["YA"]], [sg_], sg_)
                S.dma("sp", lambda e, sg_=sg_, r0=r0: e.dma_start(out=dd[r0:r0 + 128, :], in_=sg_[:]), [sg_], [DB["dbg"]], sg_, concurrent=True)
            S.barrier()


def phase4(nc, S, A, DB, PB, K, stage, dbg, dbg_tensor):
    ident_bf = K["ident_bf"]; ones_f = K["ones_f"]; ones_bf = K["ones_bf"]
    QT_d = K["QT_d"]; KT_d = K["KT_d"]; V_d = K["V_d"]; YB_d = K["YB_d"]
    NK = NT
    with ExitStack() as ph:
        KT = S.sb("KT", [128, 4, LC + L], BF16, ph)
        Vp = S.sb("Vp", [128, NK, 4, 128], BF16, ph)
        lamr = S.sb("lamr", [1, 264], F32, ph)
        lamc = S.sb("lamc", [128, 1], F32, ph)
        subw = S.sb("subw", [128, 128], F32, ph)
        qring = Ring(S, "qtb", [128, 4, 512], BF16, 2, ph)
        pring = Ring(S, "pT", [128, 512], BF16, 6, ph)
        acc = S.sb("acc", [128, 4, 128], F32, ph)
        st4 = S.sb("st4", [128, 4, 8], F32, ph)
        ybt = Ring(S, "ybt", [128, 4, 128], BF16, 2, ph)
        ybo = Ring(S, "ybo", [128, 512], BF16, 3, ph)
        junk = S.sb("junk4", [128, 128], F32, ph)
        zeros_bf = S.sb("zeros4", [128, 128], BF16, ph)
        S.op("pool", lambda e: e.memset(zeros_bf[:], 0.0), [], [zeros_bf])
        for hd in range(4):
            S.dma("sp", lambda e, hd=hd: e.dma_start(out=KT[:, hd, :], in_=KT_d[hd * 128:(hd + 1) * 128, :]), [DB["KT"]], [KT], KT, concurrent=(hd > 0))
        for kt in range(NK):
            S.dma("sp", lambda e, kt=kt: e.dma_start(out=Vp[:, kt, :, 0:128], in_=V_d[kt * 128:(kt + 1) * 128, :].rearrange("p (h d) -> p h d", h=4)), [DB["V"]], [Vp], Vp,
                  concurrent=(kt > 0))
        S.dma("sp", lambda e: e.dma_start(out=lamr[:, 0:256], in_=A["da_lambda"]), [DB["da"]], [lamr], lamr)
        S.dma("sp", lambda e: e.dma_start(out=subw[:], in_=A["da_subln"].partition_broadcast(128)), [DB["da"]], [subw], subw)
        lv = lamr[0:1, 0:256].rearrange("p (a b d) -> p a b d", a=2, b=2)
        S.op("dve", lambda e: e.tensor_tensor(out=lv[:, :, 0, :], in0=lv[:, :, 0, :], in1=lv[:, :, 1, :], op=ALU.mult), [lamr], [lamr])
        S.op("dve", lambda e: e.reduce_sum(out=lamr[0:1, 256:258], in_=lv[:, :, 0, :], axis=AX.X), [lamr], [lamr])
        S.op("act", lambda e: e.activation(out=lamr[0:1, 256:258], in_=lamr[0:1, 256:258], func=AF.Exp), [lamr], [lamr])
        S.op("dve", lambda e: e.scalar_tensor_tensor(out=lamr[0:1, 258:259], in0=lamr[0:1, 256:257], scalar=0.2, in1=lamr[0:1, 257:258], op0=ALU.add, op1=ALU.subtract), [lamr], [lamr])
        S.op("dve", lambda e: e.tensor_scalar(out=lamr[0:1, 259:260], in0=lamr[0:1, 258:259], scalar1=-1.0, scalar2=None, op0=ALU.mult), [lamr], [lamr])
        pl = PB[3]
        S.op("pe", lambda e: e.matmul(pl[:, 0:1], lhsT=ones_f[0:1, :], rhs=lamr[0:1, 259:260], start=True, stop=True), [ones_f, lamr], [pl])
        S.op("dve", lambda e: e.tensor_copy(out=lamc[:], in_=pl[:, 0:1]), [pl], [lamc])
        S.op("dve", lambda e: e.tensor_scalar(out=subw[:], in0=subw[:], scalar1=0.8, scalar2=None, op0=ALU.mult), [subw], [subw])

        subcol = S.sb("subcol", [128, 1], F32, ph)
        S.dma("sp", lambda e: e.dma_start(out=subcol[:], in_=A["da_subln"].rearrange("o d -> d o"), allow_slow_non_contiguous=True), [DB["da"]], [subcol], subcol)
        S.op("dve", lambda e: e.tensor_scalar(out=subcol[:], in0=subcol[:], scalar1=0.8, scalar2=None, op0=ALU.mult), [subcol], [subcol])
        r1r = Ring(S, "r1_4", [128, 512], F32, 3, ph); r2r = Ring(S, "r2_4", [128, 512], F32, 3, ph)
        sqr = Ring(S, "sq_4", [128, 512], BF16, 3, ph)
        sT_i = [0]
        deferred = []

        def sbank():
            b = PB[sT_i[0] % 4]
            sT_i[0] += 1
            return b
        for qb in range(8):
            qt_b = qring.next()
            S.dma("sp", lambda e, qt_b=qt_b, qb=qb: e.dma_start(out=qt_b[:], in_=QT_d[:, qb * 512:(qb + 1) * 512].rearrange("(h p) t -> p h t", p=128)), [DB["QT"]], [qt_b], qt_b)
            for hd in range(4):
                OV = (PB[4], PB[5]); DEN = (PB[6], PB[7])
                pts = {}

                def score(kt, cp):
                    sT = sbank()
                    psl = slice(cp * 64, (cp + 1) * 64)
                    S.op("pe", lambda e, sT=sT, kt=kt, psl=psl: e.matmul(sT[:, :], lhsT=KT[psl, hd, kt * 128:(kt + 1) * 128], rhs=qt_b[psl, hd, :], start=True, stop=True), [KT, qt_b], [sT])
                    pT = pring.next()
                    S.op("act", lambda e, sT=sT, pT=pT: e.activation(out=pT[:], in_=sT[:, :], func=AF.Exp, scale=0.125), [sT], [pT])
                    pts[(kt, cp)] = pT
                score(0, 0); score(0, 1)
                for kt in range(NK):
                    if kt + 1 < NK:
                        score(kt + 1, 0); score(kt + 1, 1)
                    if kt == 6 and deferred:
                        deferred.pop(0)()
                    p0 = pts.pop((kt, 0)); p1 = pts.pop((kt, 1))
                    fl = dict(start=(kt == 0), stop=(kt == NK - 1))
                    S.op("pe", lambda e, p0=p0, kt=kt, fl=fl: e.matmul(OV[0][:, :], lhsT=Vp[:, kt, hd, 0:128], rhs=p0[:], **fl), [Vp, p0], [OV[0]])
                    S.op("pe", lambda e, p1=p1, kt=kt, fl=fl: e.matmul(OV[1][:, :], lhsT=Vp[:, kt, hd, 0:128], rhs=p1[:], **fl), [Vp, p1], [OV[1]])
                    S.op("pe", lambda e, p0=p0, fl=fl: e.matmul(DEN[0][:, :], lhsT=ones_bf[:], rhs=p0[:], **fl), [ones_bf, p0], [DEN[0]])
                    S.op("pe", lambda e, p1=p1, fl=fl: e.matmul(DEN[1][:, :], lhsT=ones_bf[:], rhs=p1[:], **fl), [ones_bf, p1], [DEN[1]])
                r1 = r1r.next(); r2 = r2r.next(); sq = sqr.next(); yo = ybo.next()
                S.op("dve", lambda e, r1=r1: e.reciprocal(out=r1[:], in_=DEN[0][:, :]), [DEN[0]], [r1])
                S.op("dve", lambda e, r2=r2: e.reciprocal(out=r2[:], in_=DEN[1][:, :]), [DEN[1]], [r2])
                S.op("dve", lambda e, r1=r1: e.tensor_tensor(out=r1[:], in0=OV[0][:, :], in1=r1[:], op=ALU.mult), [OV[0], r1], [r1])
                S.op("dve", lambda e, r2=r2: e.tensor_tensor(out=r2[:], in0=OV[1][:, :], in1=r2[:], op=ALU.mult), [OV[1], r2], [r2])
                S.op("dve", lambda e, r1=r1, r2=r2: e.scalar_tensor_tensor(out=r1[:], in0=r2[:], scalar=lamc[:, 0:1], in1=r1[:], op0=ALU.mult, op1=ALU.add), [r1, r2, lamc], [r1])
                S.op("act", lambda e, r1=r1, sq=sq: e.activation(out=sq[:], in_=r1[:], func=AF.Square), [r1], [sq])

                def tail(r1=r1, r2=r2, sq=sq, yo=yo, hd=hd, qb=qb):
                    pss = sbank()
                    S.op("pe", lambda e: e.matmul(pss[:, :], lhsT=ones_bf[:], rhs=sq[:], start=True, stop=True), [ones_bf, sq], [pss])
                    S.op("act", lambda e: e.activation(out=r2[:], in_=pss[:, :], func=AF.Sqrt, scale=1.0 / 128.0, bias=EPS), [pss], [r2])
                    S.op("dve", lambda e: e.reciprocal(out=r2[:], in_=r2[:]), [r2], [r2])
                    S.op("dve", lambda e: e.scalar_tensor_tensor(out=yo[:], in0=r1[:], scalar=subcol[:, 0:1], in1=r2[:], op0=ALU.mult, op1=ALU.mult), [r1, r2, subcol], [yo])
                    S.dma("sp", lambda e: e.dma_start(out=YB_d[hd * 128:(hd + 1) * 128, qb * 512:(qb + 1) * 512], in_=yo[:]), [yo], [DB["YB"]], yo, concurrent=True)
                deferred.append(tail)
        while deferred:
            deferred.pop(0)()
        S.barrier()
        if dbg and stage == 4:
            dd = dbg_tensor("YB", [512, L], BF16)
            stg = Ring(S, "dstg4", [128, L], BF16, 2, ph)
            for r0 in range(0, 512, 128):
                sg_ = stg.next()
                S.dma("sp", lambda e, sg_=sg_, r0=r0: e.dma_start(out=sg_[:], in_=YB_d[r0:r0 + 128, :]), [DB["YB"]], [sg_], sg_)
                S.dma("sp", lambda e, sg_=sg_, r0=r0: e.dma_start(out=dd[r0:r0 + 128, :], in_=sg_[:]), [sg_], [DB["dbg"]], sg_, concurrent=True)
            S.barrier()


def phase5(nc, S, A, DB, PB, K, stage, dbg, dbg_tensor):
    ident_f = K["ident_f"]; aff = K["aff"]
    YA_d = K["YA_d"]; YB_d = K["YB_d"]; SG_d = K["SG_d"]; X1_d = K["X1_d"]; H2_d = K["H2_d"]; ROWS_d = K["ROWS_d"]
    with ExitStack() as ph:
        wpa = S.sb("wpa", [128, 4, D], BF16, ph)
        wpb = S.sb("wpb", [128, 4, D], BF16, ph)
        wout = S.sb("wout", [128, 8, D], BF16, ph)
        wr = S.sb("wr", [128, 8, 16], F32, ph)
        rows = S.sb("rows5", [128, 4, D], F32, ph)
        S.dma("pool", lambda e: e.dma_start(out=wpa[:], in_=A["w_proj_a"].rearrange("(k p) n -> p k n", p=128)), [DB["w_proj"]], [wpa], wpa)
        S.dma("pool", lambda e: e.dma_start(out=wpb[:], in_=A["w_proj_b"].rearrange("(k p) n -> p k n", p=128)), [DB["w_proj"]], [wpb], wpb)
        S.dma("pool", lambda e: e.dma_start(out=wout[:], in_=A["w_out"].rearrange("(k p) n -> p k n", p=128)), [DB["w_out"]], [wout], wout)
        S.dma("sp", lambda e: e.dma_start(out=wr[:], in_=A["w_router"].rearrange("(k p) n -> p k n", p=128)), [DB["w_router"]], [wr], wr)
        S.dma("sp", lambda e: e.dma_start(out=rows[:].rearrange("p a b -> p (a b)"), in_=ROWS_d), [DB["ROWS"]], [rows], rows)
        yar = Ring(S, "ya5", [128, 4, 512], BF16, 2, ph)
        ybr = Ring(S, "yb5", [128, 4, 512], BF16, 2, ph)
        sgr = Ring(S, "sg5", [128, 16, 512], BF16, 2, ph)
        mT = Ring(S, "mT", [128, 8, 512], BF16, 2, ph)
        t1r = Ring(S, "t1_5", [128, 512], F32, 3, ph)
        t2r = Ring(S, "t2_5", [128, 512], F32, 3, ph)
        xr_ = Ring(S, "x5", [128, D], F32, 2, ph)
        x1r = Ring(S, "x1_5", [128, D], F32, 2, ph)
        h2r = Ring(S, "h2f", [128, D], F32, 5, ph)
        h2br = Ring(S, "h2b", [128, D], BF16, 2, ph)
        h2Tr = Ring(S, "h2T", [128, 8, 128], F32, 2, ph)
        junk = S.sb("junk5", [128, D], BF16, ph)
        st_ring = Ring(S, "st5", [128, 1, 12], F32, 8, ph)
        ex_ring = Ring(S, "ex5", [128, 16], F32, 3, ph)
        junk2 = S.sb("junk5b", [128, D], BF16, ph)
        def router_part(ti, h2, h2T, st):
            ex = ex_ring.next()
            for dt_ in range(8):
                pbk = PB[6 + dt_ // 4]
                S.op("pe", lambda e, pbk=pbk, dt_=dt_, h2=h2: e.transpose(pbk[:, (dt_ % 4) * 128:(dt_ % 4 + 1) * 128], h2[:, dt_ * 128:(dt_ + 1) * 128], ident_f[:]), [h2, ident_f], [pbk])
            S.op("act", lambda e, h2T=h2T: e.activation(out=h2T[:, 0:4, :].rearrange("p a b -> p (a b)"), in_=PB[6][:, :], func=AF.Copy), [PB[6]], [h2T])
            S.op("dve", lambda e, h2T=h2T: e.tensor_copy(out=h2T[:, 4:8, :].rearrange("p a b -> p (a b)"), in_=PB[7][:, :]), [PB[7]], [h2T])
            pl = PB[6]
            for dt_ in range(8):
                S.op("pe", lambda e, dt_=dt_, h2T=h2T: e.matmul(pl[:, 0:16], lhsT=h2T[:, dt_, :], rhs=wr[:, dt_, :], start=(dt_ == 0), stop=(dt_ == 7)), [h2T, wr], [pl])
            S.op("dve", lambda e, ti=ti: e.reduce_max(out=st[:, 0, 8:9], in_=pl[:, 0:16], axis=AX.X), [pl], [st])
            S.op("dve", lambda e, ti=ti: e.tensor_scalar(out=st[:, 0, 8:9], in0=st[:, 0, 8:9], scalar1=-1.0, scalar2=None, op0=ALU.mult), [st], [st])
            S.op("act", lambda e, ti=ti: e.activation(out=ex[:], in_=pl[:, 0:16], func=AF.Exp, bias=st[:, 0, 8:9], scale=1.0, accum_out=st[:, 0, 9:10]), [pl, st], [ex, st])
            S.op("dve", lambda e, ti=ti: e.reciprocal(out=st[:, 0, 10:11], in_=st[:, 0, 9:10]), [st], [st])
            S.op("dve", lambda e, ti=ti: e.tensor_scalar(out=aff[:, ti, :], in0=ex[:], scalar1=st[:, 0, 10:11], scalar2=None, op0=ALU.mult), [ex, st], [aff])
        pending = []
        blk_bufs = {}

        def load_block(tb):
            if tb >= 8:
                return
            ya = yar.next(); yb = ybr.next(); sg = sgr.next()
            tsl = slice(tb * 512, (tb + 1) * 512)
            S.dma("sp", lambda e: e.dma_start(out=ya[:], in_=YA_d[:, tsl].rearrange("(k p) t -> p k t", p=128)), [DB["YA"]], [ya], ya)
            S.dma("sp", lambda e: e.dma_start(out=yb[:], in_=YB_d[:, tsl].rearrange("(k p) t -> p k t", p=128)), [DB["YB"]], [yb], yb)
            S.dma("sp", lambda e: e.dma_start(out=sg[:], in_=SG_d[:, tsl].rearrange("(k p) t -> p k t", p=128)), [DB["SG"]], [sg], sg)
            blk_bufs[tb] = (ya, yb, sg)
        load_block(0)
        mo_i = [0]
        for tb in range(8):
            load_block(tb + 1)
            ya, yb, sg = blk_bufs.pop(tb)
            m_ = mT.next()
            for dm in range(8):
                pa = PB[0]; pb = PB[1]
                for kt in range(4):
                    S.op("pe", lambda e, pa=pa, kt=kt, dm=dm, ya=ya: e.matmul(pa[:, :], lhsT=wpa[:, kt, dm * 128:(dm + 1) * 128], rhs=ya[:, kt, :], start=(kt == 0), stop=(kt == 3)), [wpa, ya], [pa])
                for kt in range(4):
                    S.op("pe", lambda e, pb=pb, kt=kt, dm=dm, yb=yb: e.matmul(pb[:, :], lhsT=wpb[:, kt, dm * 128:(dm + 1) * 128], rhs=yb[:, kt, :], start=(kt == 0), stop=(kt == 3)), [wpb, yb], [pb])
                t1 = t1r.next(); t2 = t2r.next()
                S.op("dve", lambda e, pa=pa, t1=t1, sg=sg, dm=dm: e.tensor_tensor(out=t1[:], in0=pa[:, :], in1=sg[:, dm, :], op=ALU.mult), [pa, sg], [t1])
                S.op("dve", lambda e, pb=pb, t2=t2, sg=sg, dm=dm: e.tensor_tensor(out=t2[:], in0=pb[:, :], in1=sg[:, 8 + dm, :], op=ALU.mult), [pb, sg], [t2])
                S.op("pool", lambda e, t1=t1, t2=t2, m_=m_, dm=dm: e.tensor_tensor(out=m_[:, dm, :], in0=t1[:], in1=t2[:], op=ALU.add), [t1, t2], [m_])
            for tt in range(4):
                ti = tb * 4 + tt
                xt = xr_.next(); x1 = x1r.next(); h2 = h2r.next(); h2b = h2br.next(); h2T = h2Tr.next(); st = st_ring.next()
                S.dma("sp", lambda e, xt=xt, ti=ti: e.dma_start(out=xt[:], in_=A["x"][ti * 128:(ti + 1) * 128, :]), [DB["x"]], [xt], xt)
                mo = (PB[2], PB[3]) if mo_i[0] % 2 == 0 else (PB[4], PB[5])
                mo_i[0] += 1
                for half in range(2):
                    for dm in range(8):
                        S.op("pe", lambda e, half=half, dm=dm, tt=tt, m_=m_: e.matmul(mo[half][:, :], lhsT=m_[:, dm, tt * 128:(tt + 1) * 128], rhs=wout[:, dm, half * 512:(half + 1) * 512],
                                                                                start=(dm == 0), stop=(dm == 7)), [m_, wout], [mo[half]])
                if len(pending) > 2:
                    router_part(*pending.pop(0))
                for half in range(2):
                    S.op("act", lambda e, half=half, ti=ti: e.activation(out=junk[:, 0:512], in_=mo[half][:, :], func=AF.Square, accum_out=st[:, 0, half:half + 1]), [mo[half]], [junk, st])
                S.op("dve", lambda e, ti=ti: e.tensor_tensor(out=st[:, 0, 2:3], in0=st[:, 0, 0:1], in1=st[:, 0, 1:2], op=ALU.add), [st], [st])
                S.op("act", lambda e, ti=ti: e.activation(out=st[:, 0, 3:4], in_=st[:, 0, 2:3], func=AF.Sqrt, scale=1.0 / D, bias=EPS), [st], [st])
                S.op("dve", lambda e, ti=ti: e.reciprocal(out=st[:, 0, 4:5], in_=st[:, 0, 3:4]), [st], [st])
                for half in range(2):
                    hs = slice(half * 512, (half + 1) * 512)
                    S.op("dve", lambda e, half=half, hs=hs, x1=x1, ti=ti: e.scalar_tensor_tensor(out=x1[:, hs], in0=mo[half][:, :], scalar=st[:, 0, 4:5], in1=rows[:, 0, hs], op0=ALU.mult, op1=ALU.mult),
                         [mo[half], st, rows], [x1])
                S.op("pool", lambda e, x1=x1, xt=xt: e.tensor_tensor(out=x1[:], in0=x1[:], in1=xt[:], op=ALU.add), [x1, xt], [x1])
                S.dma("sp", lambda e, x1=x1, ti=ti: e.dma_start(out=X1_d[ti * 128:(ti + 1) * 128, :], in_=x1[:]), [x1], [DB["X1"]], x1, concurrent=True)
                S.op("act", lambda e, x1=x1, ti=ti: e.activation(out=junk2[:], in_=x1[:], func=AF.Square, accum_out=st[:, 0, 5:6]), [x1], [junk2, st])
                S.op("act", lambda e, ti=ti: e.activation(out=st[:, 0, 6:7], in_=st[:, 0, 5:6], func=AF.Sqrt, scale=1.0 / D, bias=EPS), [st], [st])
                S.op("dve", lambda e, ti=ti: e.reciprocal(out=st[:, 0, 7:8], in_=st[:, 0, 6:7]), [st], [st])
                S.op("dve", lambda e, x1=x1, h2=h2, ti=ti: e.scalar_tensor_tensor(out=h2[:], in0=x1[:], scalar=st[:, 0, 7:8], in1=rows[:, 1, :], op0=ALU.mult, op1=ALU.mult), [x1, st, rows], [h2])
                S.op("pool", lambda e, h2=h2: e.tensor_tensor(out=h2[:], in0=h2[:], in1=rows[:, 2, :], op=ALU.add), [h2, rows], [h2])
                S.op("act", lambda e, h2=h2, h2b=h2b: e.activation(out=h2b[:], in_=h2[:], func=AF.Copy), [h2], [h2b])
                S.dma("sp", lambda e, h2b=h2b, ti=ti: e.dma_start(out=H2_d[ti * 128:(ti + 1) * 128, :], in_=h2b[:]), [h2b], [DB["H2"]], h2b, concurrent=True)
                pending.append((ti, h2, h2T, st))
        while pending:
            router_part(*pending.pop(0))
        S.barrier()
        if dbg and stage == 5:
            dd = dbg_tensor("X1", [L, D], F32); dh = dbg_tensor("H2", [L, D], BF16); da = dbg_tensor("aff", [128, 512], F32)
            S.dma("sp", lambda e: e.dma_start(out=da, in_=aff[:].rearrange("p a b -> p (a b)")), [aff], [DB["dbg"]], aff, concurrent=True)
            for ti in range(32):
                xt = xr_.next(); hb = h2br.next()
                S.dma("sp", lambda e, xt=xt, ti=ti: e.dma_start(out=xt[:], in_=X1_d[ti * 128:(ti + 1) * 128, :]), [DB["X1"]], [xt], xt)
                S.dma("sp", lambda e, xt=xt, ti=ti: e.dma_start(out=dd[ti * 128:(ti + 1) * 128, :], in_=xt[:]), [xt], [DB["dbg"]], xt, concurrent=True)
                S.dma("sp", lambda e, hb=hb, ti=ti: e.dma_start(out=hb[:], in_=H2_d[ti * 128:(ti + 1) * 128, :]), [DB["H2"]], [hb], hb)
                S.dma("sp", lambda e, hb=hb, ti=ti: e.dma_start(out=dh[ti * 128:(ti + 1) * 128, :], in_=hb[:]), [hb], [DB["dbg"]], hb, concurrent=True)
            S.barrier()


def phase678(nc, S, A, DB, PB, K, stage, dbg, dbg_tensor):
    aff = K["aff"]; iota_f = K["iota_f"]; pidx = K["pidx"]; ones_f = K["ones_f"]; ones_bf = K["ones_bf"]; ident_bf = K["ident_bf"]
    H2_d = K["H2_d"]; X1_d = K["X1_d"]; F_d = K["F_d"]; ROWS_d = K["ROWS_d"]; out_ap = K["out"]
    CAP = 512
    with ExitStack() as ph:
        zt = S.sb("zt", [128, D], F32, ph)
        S.op("pool", lambda e: e.memset(zt[:], 0.0), [], [zt])
        for ti in range(32):
            S.dma("sp", lambda e, ti=ti: e.dma_start(out=F_d[ti * 128:(ti + 1) * 128, :], in_=zt[:]), [zt], [DB["F"]], zt, concurrent=True)
        lo = S.sb("lo", [128, 16], F32, ph); hi = S.sb("hi", [128, 16], F32, ph); mid = S.sb("mid", [128, 16], F32, ph)
        dd_ = S.sb("dd", [128, 16], F32, ph); sel = S.sb("sel", [128, 16], F32, ph); part = S.sb("part", [128, 16], F32, ph)
        cmp_ = S.sb("cmp", [128, 32, 16], F32, ph)
        maskf = S.sb("maskf", [128, 32, 16], F32, ph); maskb = S.sb("maskb", [128, 32, 16], BF16, ph)
        posm = S.sb("posm", [128, 32, 16], F32, ph); offs = S.sb("offs", [128, 32, 16], F32, ph)
        RH = S.sb("RH", [128, 32, 16, 4], BF16, ph); ahf = S.sb("ahf", [128, 32, 16], F32, ph)
        U_bf = S.sb("U_bf", [128, 128], BF16, ph)
        S.op("dve", lambda e: e.memset(lo[:], 0.0), [], [lo])
        S.op("dve", lambda e: e.memset(hi[:], 1.0), [], [hi])
        pc = PB[0]
        for it in range(27):
            S.op("dve", lambda e: e.tensor_tensor(out=mid[:], in0=lo[:], in1=hi[:], op=ALU.add), [lo, hi], [mid])
            S.op("dve", lambda e: e.tensor_scalar(out=mid[:], in0=mid[:], scalar1=0.5, scalar2=None, op0=ALU.mult), [mid], [mid])
            S.op("dve", lambda e: e.tensor_tensor(out=cmp_[:], in0=aff[:], in1=mid[:].unsqueeze(1).to_broadcast([128, 32, 16]), op=ALU.is_ge), [aff, mid], [cmp_])
            S.op("dve", lambda e: e.reduce_sum(out=part[:], in_=cmp_[:].rearrange("p t e -> p e t"), axis=AX.X), [cmp_], [part])
            S.op("pe", lambda e: e.matmul(pc[:, 0:16], lhsT=ones_f[:], rhs=part[:], start=True, stop=True), [ones_f, part], [pc])
            S.op("dve", lambda e: e.tensor_scalar(out=sel[:], in0=pc[:, 0:16], scalar1=float(CAP) - 0.5, scalar2=None, op0=ALU.is_ge), [pc], [sel])
            S.op("dve", lambda e: e.tensor_tensor(out=dd_[:], in0=mid[:], in1=lo[:], op=ALU.subtract), [mid, lo], [dd_])
            S.op("dve", lambda e: e.tensor_tensor(out=dd_[:], in0=dd_[:], in1=sel[:], op=ALU.mult), [dd_, sel], [dd_])
            S.op("dve", lambda e: e.tensor_tensor(out=lo[:], in0=lo[:], in1=dd_[:], op=ALU.add), [lo, dd_], [lo])
            S.op("dve", lambda e: e.tensor_tensor(out=dd_[:], in0=hi[:], in1=mid[:], op=ALU.subtract), [hi, mid], [dd_])
            S.op("dve", lambda e: e.tensor_tensor(out=dd_[:], in0=dd_[:], in1=sel[:], op=ALU.mult), [dd_, sel], [dd_])
            S.op("dve", lambda e: e.tensor_tensor(out=hi[:], in0=mid[:], in1=dd_[:], op=ALU.add), [mid, dd_], [hi])
        S.op("dve", lambda e: e.tensor_tensor(out=maskf[:], in0=aff[:], in1=lo[:].unsqueeze(1).to_broadcast([128, 32, 16]), op=ALU.is_ge), [aff, lo], [maskf])
        S.op("dve", lambda e: e.tensor_copy(out=maskb[:], in_=maskf[:]), [maskf], [maskb])
        S.op("dve", lambda e: e.tensor_scalar(out=U_bf[:], in0=iota_f[:, 0:128], scalar1=pidx[:, 0:1], scalar2=None, op0=ALU.is_gt), [iota_f, pidx], [U_bf])
        pcnt = PB[1]; ppos = PB[2]
        mb2 = maskb[:].rearrange("p t e -> p (t e)")
        S.op("pe", lambda e: e.matmul(pcnt[:, :], lhsT=ones_bf[:], rhs=mb2, start=True, stop=True), [ones_bf, maskb], [pcnt])
        S.op("pe", lambda e: e.matmul(ppos[:, :], lhsT=U_bf[:], rhs=mb2, start=True, stop=True), [U_bf, maskb], [ppos])
        S.op("dve", lambda e: e.memset(offs[:, 0, :], 0.0), [], [offs])
        cntv = pcnt[:, :].rearrange("p (t e) -> p t e", e=16)
        for tt in range(1, 32):
            S.op("dve", lambda e, tt=tt: e.tensor_tensor(out=offs[:, tt, :], in0=offs[:, tt - 1, :], in1=cntv[:, tt - 1, :], op=ALU.add), [offs, pcnt], [offs])
        S.op("dve", lambda e: e.tensor_tensor(out=posm[:].rearrange("p t e -> p (t e)"), in0=ppos[:, :], in1=offs[:].rearrange("p t e -> p (t e)"), op=ALU.add), [ppos, offs], [posm])
        S.op("dve", lambda e: e.scalar_tensor_tensor(out=posm[:], in0=posm[:], scalar=1.0, in1=maskf[:], op0=ALU.add, op1=ALU.mult), [posm, maskf], [posm])
        S.op("dve", lambda e: e.tensor_scalar(out=posm[:], in0=posm[:], scalar1=-1.0, scalar2=None, op0=ALU.add), [posm], [posm])
        S.op("dve", lambda e: e.tensor_copy(out=RH[:, :, :, 0], in_=iota_f[:, 0:32].unsqueeze(2).to_broadcast([128, 32, 16])), [iota_f], [RH])
        S.op("dve", lambda e: e.tensor_copy(out=RH[:, :, :, 1].rearrange("p t e -> p (t e)"), in_=pidx[:, 0:1].to_broadcast([128, 512])), [pidx], [RH])
        S.op("dve", lambda e: e.tensor_copy(out=RH[:, :, :, 2], in_=aff[:]), [aff], [RH])
        S.op("dve", lambda e: e.tensor_copy(out=ahf[:], in_=RH[:, :, :, 2]), [RH], [ahf])
        S.op("dve", lambda e: e.tensor_tensor(out=RH[:, :, :, 3], in0=aff[:], in1=ahf[:], op=ALU.subtract), [aff, ahf], [RH])
        if dbg and stage == 6:
            d1 = dbg_tensor("posm", [128, 512], F32); d2 = dbg_tensor("thr", [128, 16], F32); d3 = dbg_tensor("aff6", [128, 512], F32)
            S.dma("sp", lambda e: e.dma_start(out=d3, in_=aff[:].rearrange("p a b -> p (a b)")), [aff], [DB["dbg"]], aff, concurrent=True)
            S.dma("sp", lambda e: e.dma_start(out=d1, in_=posm[:].rearrange("p a b -> p (a b)")), [posm], [DB["dbg"]], posm, concurrent=True)
            S.dma("sp", lambda e: e.dma_start(out=d2, in_=lo[:]), [lo], [DB["dbg"]], lo, concurrent=True)
            S.barrier()
            return
        pm = ExitStack()
        mkm = S.mark()
        selr = Ring(S, "selT", [128, 512], BF16, 10, pm)
        zeros_m = S.sb("zeros_m", [128, 128], BF16, pm)
        S.op("pool", lambda e: e.memset(zeros_m[:], 0.0), [], [zeros_m])
        idxf = [S.sb("idxf%d" % i, [128, 4, 4], F32, pm) for i in range(3)]
        idxi = [S.sb("idxi%d" % i, [128, 4], I32, pm) for i in range(3)]
        gate = [S.sb("gate%d" % i, [128, 4], F32, pm) for i in range(3)]
        xs = [S.sb("xs%d" % i, [128, 4, D], BF16, pm) for i in range(3)]
        xsT = S.sb("xsT", [128, 8, 512], BF16, pm)
        wgr = Ring(S, "wg", [128, 8, 512], BF16, 3, pm)
        wur = Ring(S, "wu", [128, 8, 512], BF16, 3, pm)
        wdr = Ring(S, "wd", [128, 16, 512], BF16, 2, pm)
        hidT = S.sb("hidT", [128, 16, 512], BF16, pm)
        sgr = Ring(S, "sgm", [128, 512], F32, 2, pm)
        ysr = [S.sb("ys%d" % i, [128, D], F32, pm) for i in range(4)]

        def compaction_steps(e_):
            k = e_ % 3
            pcs = PB[0]
            steps = []
            dsteps = []

            def init():
                S.op("pe", lambda e: e.matmul(pcs[:, 0:16], lhsT=zeros_m[:], rhs=RH[:, 0, 0:4, :].rearrange("p a b -> p (a b)"), start=True, stop=False), [zeros_m, RH], [pcs])
            steps.append(init)
            sels = {}
            for tt in range(32):
                def dstep(tt=tt):
                    sl = selr.next()
                    S.op("dve", lambda e: e.tensor_scalar(out=sl[:], in0=iota_f[:, :], scalar1=posm[:, tt, e_:e_ + 1], scalar2=None, op0=ALU.is_equal), [iota_f, posm], [sl])
                    sels[tt] = sl

                def pstep(tt=tt):
                    sl = sels.pop(tt)
                    for st in range(4):
                        S.op("pe", lambda e, st=st: e.matmul(pcs[:, st * 4:(st + 1) * 4], lhsT=sl[:, st * 128:(st + 1) * 128], rhs=RH[:, tt, e_, :], start=False, stop=(tt == 31)),
                             [sl, RH], [pcs])
                dsteps.append(dstep); steps.append(pstep)

            def fin():
                S.op("dve", lambda e: e.tensor_copy(out=idxf[k][:].rearrange("p a b -> p (a b)"), in_=pcs[:, 0:16]), [pcs], [idxf[k]])
                S.op("dve", lambda e: e.scalar_tensor_tensor(out=idxf[k][:, :, 0], in0=idxf[k][:, :, 0], scalar=128.0, in1=idxf[k][:, :, 1], op0=ALU.mult, op1=ALU.add), [idxf[k]], [idxf[k]])
                S.op("dve", lambda e: e.tensor_scalar(out=idxf[k][:, :, 0], in0=idxf[k][:, :, 0], scalar1=8388608.0, scalar2=None, op0=ALU.add), [idxf[k]], [idxf[k]])
                S.op("dve", lambda e: e.tensor_single_scalar(out=idxi[k][:], in_=idxf[k][:, :, 0].bitcast(I32), scalar=0x7FFFFF, op=ALU.bitwise_and), [idxf[k]], [idxi[k]])
                S.op("dve", lambda e: e.tensor_tensor(out=gate[k][:], in0=idxf[k][:, :, 2], in1=idxf[k][:, :, 3], op=ALU.add), [idxf[k]], [gate[k]])
                for st in range(4):
                    S.dma("pool", lambda e, st=st: e.indirect_dma_start(out=xs[k][:, st, :], out_offset=None, in_=H2_d, in_offset=bass.IndirectOffsetOnAxis(ap=idxi[k][:, st:st + 1], axis=0)),
                          [DB["H2"], idxi[k]], [xs[k]], xs[k], concurrent=(st > 0))
            steps.append(fin)
            return steps, dsteps

        wtasks = []
        for e_ in range(16):
            for fb in range(4):
                wtasks.append(("gu", e_, fb))
            for half in range(2):
                wtasks.append(("d", e_, half))
        wbuf = {}
        nxt = [0]

        def prefetch(upto):
            while nxt[0] < len(wtasks) and nxt[0] <= upto:
                kind, e_, j = wtasks[nxt[0]]
                if kind == "gu":
                    wg = wgr.next(); wu = wur.next()
                    S.dma("pool", lambda e, wg=wg, e_=e_, j=j: e.dma_start(out=wg[:], in_=A["w_exp_gate"][e_, :, j * 512:(j + 1) * 512].rearrange("(k p) n -> p k n", p=128)), [DB["w_exp"]], [wg], wg)
                    S.dma("pool", lambda e, wu=wu, e_=e_, j=j: e.dma_start(out=wu[:], in_=A["w_exp_up"][e_, :, j * 512:(j + 1) * 512].rearrange("(k p) n -> p k n", p=128)), [DB["w_exp"]], [wu], wu)
                    wbuf[nxt[0]] = (wg, wu)
                else:
                    wd = wdr.next()
                    S.dma("pool", lambda e, wd=wd, e_=e_, j=j: e.dma_start(out=wd[:], in_=A["w_exp_down"][e_, :, j * 512:(j + 1) * 512].rearrange("(k p) n -> p k n", p=128)), [DB["w_exp"]], [wd], wd)
                    wbuf[nxt[0]] = (wd,)
                nxt[0] += 1

        for e0 in range(2):
            ps_, ds_ = compaction_steps(e0)
            ps_.pop(0)()
            while ds_:
                for _ in range(4):
                    if ds_:
                        ds_.pop(0)()
                for _ in range(4):
                    if len(ps_) > 1:
                        ps_.pop(0)()
            while ps_:
                ps_.pop(0)()
            if e0 == 0:
                prefetch(1)
        for e_ in range(16):
            k = e_ % 3
            csteps, dsteps_ = compaction_steps(e_ + 2) if e_ + 2 < 16 else ([], [])
            for _ in range(6):
                if dsteps_:
                    dsteps_.pop(0)()
            for dt_ in range(8):
                pt = PB[(1, 6, 7)[dt_ % 3]]
                ptb = pt.t.bitcast(BF16)
                for st in range(4):
                    S.op("pe", lambda e, ptb=ptb, st=st, dt_=dt_: e.transpose(ptb[:, st * 128:(st + 1) * 128], xs[k][:, st, dt_ * 128:(dt_ + 1) * 128], ident_bf[:]), [xs[k], ident_bf], [pt])
                if dt_ % 2 == 0:
                    S.op("act", lambda e, ptb=ptb, dt_=dt_: e.activation(out=xsT[:, dt_, :], in_=ptb[:, 0:512], func=AF.Copy), [pt], [xsT])
                else:
                    S.op("dve", lambda e, ptb=ptb, dt_=dt_: e.tensor_copy(out=xsT[:, dt_, :], in_=ptb[:, 0:512]), [pt], [xsT])
            base = e_ * 6
            for fb in range(4):
                prefetch(base + fb + 2)
                wg, wu = wbuf.pop(base + fb)
                for f4 in range(4):
                    ft = fb * 4 + f4
                    pg = PB[2 + 2 * (ft % 2)]; pu = PB[3 + 2 * (ft % 2)]
                    for kt in range(8):
                        S.op("pe", lambda e, pg=pg, wg=wg, kt=kt, f4=f4: e.matmul(pg[:, :], lhsT=wg[:, kt, f4 * 128:(f4 + 1) * 128], rhs=xsT[:, kt, :], start=(kt == 0), stop=(kt == 7)), [wg, xsT], [pg])
                    for kt in range(8):
                        S.op("pe", lambda e, pu=pu, wu=wu, kt=kt, f4=f4: e.matmul(pu[:, :], lhsT=wu[:, kt, f4 * 128:(f4 + 1) * 128], rhs=xsT[:, kt, :], start=(kt == 0), stop=(kt == 7)), [wu, xsT], [pu])
                    sg = sgr.next()
                    S.op("act", lambda e, pg=pg, sg=sg: e.activation(out=sg[:], in_=pg[:, :], func=AF.Silu), [pg], [sg])
                    S.op("dve", lambda e, pu=pu, sg=sg, ft=ft: e.tensor_tensor(out=hidT[:, ft, :], in0=sg[:], in1=pu[:, :], op=ALU.mult), [sg, pu], [hidT])
                    for _ in range(3):
                        if len(csteps) > 1:
                            csteps.pop(0)()
                        if dsteps_:
                            dsteps_.pop(0)()
            for half in range(2):
                prefetch(base + 4 + half + 2)
                (wd,) = wbuf.pop(base + 4 + half)
                for st in range(4):
                    po = PB[6 + (st % 2)]
                    for ft in range(16):
                        S.op("pe", lambda e, po=po, wd=wd, ft=ft, st=st: e.matmul(po[:, :], lhsT=hidT[:, ft, st * 128:(st + 1) * 128], rhs=wd[:, ft, :], start=(ft == 0), stop=(ft == 15)), [hidT, wd], [po])
                    S.op("act", lambda e, po=po, st=st, half=half: e.activation(out=ysr[st][:, half * 512:(half + 1) * 512], in_=po[:, :], func=AF.Copy, scale=gate[k][:, st:st + 1]), [po, gate[k]], [ysr[st]])
            while csteps:
                csteps.pop(0)()
            for st in range(4):
                S.dma("pool", lambda e, st=st: e.indirect_dma_start(out=F_d, out_offset=bass.IndirectOffsetOnAxis(ap=idxi[k][:, st:st + 1], axis=0), in_=ysr[st][:], in_offset=None, compute_op=ALU.add),
                      [ysr[st], idxi[k]], [DB["F"]], ysr[st], concurrent=(st > 0))
        S.release(mkm)
        pm.close()
        gwf = S.sb("gwf", [128, D], F32, ph)
        S.dma("sp", lambda e: e.dma_start(out=gwf[:], in_=ROWS_d[:, 3 * D:4 * D]), [DB["ROWS"]], [gwf], gwf)
        fr = Ring(S, "ft", [128, D], F32, 4, ph); x1r = Ring(S, "x1f", [128, D], F32, 4, ph); orr = Ring(S, "of", [128, D], F32, 4, ph)
        stf_ring = Ring(S, "stf", [128, 1, 4], F32, 4, ph)
        junk = S.sb("junk8", [128, D], BF16, ph)
        for ti in range(32):
            ft_ = fr.next(); x1 = x1r.next(); ot = orr.next(); stf = stf_ring.next()
            S.dma("sp", lambda e, ft_=ft_, ti=ti: e.dma_start(out=ft_[:], in_=F_d[ti * 128:(ti + 1) * 128, :]), [DB["F"]], [ft_], ft_)
            S.dma("sp", lambda e, x1=x1, ti=ti: e.dma_start(out=x1[:], in_=X1_d[ti * 128:(ti + 1) * 128, :]), [DB["X1"]], [x1], x1)
            S.op("act", lambda e, ft_=ft_, ti=ti: e.activation(out=junk[:], in_=ft_[:], func=AF.Square, accum_out=stf[:, 0, 0:1]), [ft_], [junk, stf])
            S.op("act", lambda e, ti=ti: e.activation(out=stf[:, 0, 1:2], in_=stf[:, 0, 0:1], func=AF.Sqrt, scale=1.0 / D, bias=EPS), [stf], [stf])
            S.op("dve", lambda e, ti=ti: e.reciprocal(out=stf[:, 0, 2:3], in_=stf[:, 0, 1:2]), [stf], [stf])
            S.op("dve", lambda e, ft_=ft_, ot=ot, ti=ti: e.scalar_tensor_tensor(out=ot[:], in0=ft_[:], scalar=stf[:, 0, 2:3], in1=gwf[:], op0=ALU.mult, op1=ALU.mult), [ft_, stf, gwf], [ot])
            S.op("pool", lambda e, ot=ot, x1=x1: e.tensor_tensor(out=ot[:], in0=ot[:], in1=x1[:], op=ALU.add), [ot, x1], [ot])
            S.dma("sp", lambda e, ot=ot, ti=ti: e.dma_start(out=out_ap[ti * 128:(ti + 1) * 128, :], in_=ot[:]), [ot], [DB["out"]], ot, concurrent=True)
        S.wait_all("sp", [DB["out"]])
        S.barrier()


_NC_CACHE = {}


def _core_inputs(inp, b):
    m = {}
    m["x"] = inp["x"][b]; m["ctx"] = inp["ctx"][b]
    m["c"] = inp["c"][b:b + 1]; m["c_ctx"] = np.asarray(inp["c_ctx"]).reshape(1, D)
    m["w_ada"] = inp["w_ada"][0]; m["b_ada"] = np.asarray(inp["b_ada"]).reshape(1, 6 * D)
    for n in ("norm_pre_mix", "norm_post_mix", "norm_pre_ffn", "norm_post_ffn"):
        m[n] = np.asarray(inp[n]).reshape(1, D)
    m["w_in"] = inp["w_in"][0]
    m["s5_lam_re"] = inp["s5_lam_re"][0].reshape(64, 64); m["s5_lam_im"] = inp["s5_lam_im"][0].reshape(64, 64)
    m["s5_log_dt"] = inp["s5_log_dt"][0].reshape(1, 64)
    m["s5_b_re"] = inp["s5_b_re"][0].reshape(64, 64, 16); m["s5_b_im"] = inp["s5_b_im"][0].reshape(64, 64, 16)
    m["s5_c_re"] = inp["s5_c_re"][0].reshape(2, 512, 64); m["s5_c_im"] = inp["s5_c_im"][0].reshape(2, 512, 64)
    m["s5_d"] = np.asarray(inp["s5_d"]).reshape(1, 512); m["w_glu"] = inp["w_glu"][0]
    m["da_lambda"] = inp["da_lambda"][0].reshape(1, 256); m["da_subln"] = np.asarray(inp["da_subln"]).reshape(1, 128)
    m["w_proj_a"] = inp["w_proj_a"][0]; m["w_proj_b"] = inp["w_proj_b"][0]; m["w_out"] = inp["w_out"][0]
    m["w_router"] = inp["w_router"][0]
    m["w_exp_gate"] = inp["w_exp_gate"][0]; m["w_exp_up"] = inp["w_exp_up"][0]; m["w_exp_down"] = inp["w_exp_down"][0]
    return {k: np.ascontiguousarray(np.asarray(v, dtype=np.float32)) for k, v in m.items()}


def kernel(**inputs):
    inp = {k: np.asarray(v) for k, v in inputs.items()}
    if "nc" not in _NC_CACHE:
        _NC_CACHE["nc"] = build()
    nc = _NC_CACHE["nc"]
    n = 8
    in_maps = [_core_inputs(inp, b) for b in range(n)]
    res = run_bass_kernel_spmd(nc, in_maps, core_ids=list(range(n)))
    return np.stack([np.asarray(r["out"], dtype=np.float32) for r in res.results], axis=0)
```

```python
import math
from contextlib import ExitStack
import numpy as np
import concourse.bass as bass
import concourse.mybir as mybir
from concourse.bass_utils import run_bass_kernel_spmd

F32 = mybir.dt.float32
BF16 = mybir.dt.bfloat16
I32 = mybir.dt.int32
AF = mybir.ActivationFunctionType
ALU = mybir.AluOpType
AX = mybir.AxisListType

D = 1024
L = 4096
LC = 256
NT = (L + LC) // 128
EPS = 1e-6
MAGIC = 12582912.0
TWO_PI = 2.0 * math.pi


class Buf:
    _n = 0

    def __init__(self, name, t=None):
        Buf._n += 1
        self.key = "b%d" % Buf._n
        self.name = name
        self.t = t
        self.w = None
        self.r = {}
        self.dsem = None
        self.dcnt = 0
        self.psem = None
        self.pcnt = 0
        self.dw = {}
        self.dr = {}

    def __getitem__(self, idx):
        return self.t[idx]


class Sched:
    ENG = ("pe", "act", "dve", "pool", "sp")

    def __init__(self, nc, stack):
        self.nc = nc
        self.stack = stack
        self.eng = {"pe": nc.tensor, "act": nc.scalar, "dve": nc.vector, "pool": nc.gpsimd, "sp": nc.sync}
        self.sem = {}
        self.tick = {e: 0 for e in self.ENG}
        for e in self.ENG:
            self.sem[e] = stack.enter_context(nc.semaphore("sem_" + e))
        self.seen = {e: {} for e in self.ENG}
        self.n_dsem = 0
        self.ninst = {e: 0 for e in self.ENG}
        self.nwait = 0
        self.uid = 0
        self.live = []
        self.free_sems = []

    def sb(self, name, shape, dt, stack=None):
        self.uid += 1
        t = (stack or self.stack).enter_context(self.nc.sbuf_tensor("%s_%d" % (name, self.uid), list(shape), dt))
        b = Buf(name, t)
        self.live.append(b)
        return b

    def mark(self):
        return len(self.live)

    def release(self, mark):
        bufs = self.live[mark:]
        del self.live[mark:]
        for b in bufs:
            for e in self.ENG:
                if b.dsem is not None:
                    self._wait(e, b.key, b.dsem, b.dcnt)
                if b.psem is not None:
                    self._wait(e, b.key + "p", b.psem, b.pcnt)
        self.barrier()
        for b in bufs:
            if b.dsem is not None:
                self.free_sems.append((b.dsem, b.dcnt))
                b.dsem = None

    def _dsem(self, b):
        if b.dsem is None:
            if self.free_sems:
                b.dsem, b.dcnt = self.free_sems.pop()
            else:
                b.dsem = self.stack.enter_context(self.nc.semaphore("ds%d" % self.n_dsem))
                self.n_dsem += 1
        return b.dsem

    def _wait(self, e, semkey, semh, val):
        if val <= 0:
            return
        if self.seen[e].get(semkey, 0) >= val:
            return
        self.eng[e].wait_ge(semh, val)
        self.seen[e][semkey] = val
        self.nwait += 1

    def _wait_eng(self, e, other, tick):
        if other == e and e == "pe":
            return
        self._wait(e, other, self.sem[other], tick)

    def _deps(self, e, reads, writes):
        for b in reads:
            if b.w is not None:
                self._wait_eng(e, b.w[0], b.w[1])
            for k, (sh, v) in b.dw.items():
                self._wait(e, k, sh, v)
        for b in writes:
            if b.w is not None:
                self._wait_eng(e, b.w[0], b.w[1])
            for oe, tk in b.r.items():
                self._wait_eng(e, oe, tk)
            for k, (sh, v) in b.dw.items():
                self._wait(e, k, sh, v)
            for k, (sh, v) in b.dr.items():
                self._wait(e, k, sh, v)

    def op(self, e, fn, reads=(), writes=()):
        self._deps(e, reads, writes)
        inst = fn(self.eng[e])
        self.tick[e] += 1
        inst.then_inc(self.sem[e], 1)
        tk = self.tick[e]
        for b in reads:
            b.r[e] = tk
        for b in writes:
            b.w = (e, tk)
            b.r = {}
            b.dw = {}
            b.dr = {}
        self.ninst[e] += 1
        return inst

    def dma(self, q, fn, reads, writes, semb, concurrent=False):
        if concurrent:
            self._deps(q, reads, [])
            for b in writes:
                if b.w is not None:
                    self._wait_eng(q, b.w[0], b.w[1])
                for oe, tk in b.r.items():
                    self._wait_eng(q, oe, tk)
                for k, (sh, v) in b.dr.items():
                    self._wait(q, k, sh, v)
        else:
            self._deps(q, reads, writes)
        inst = fn(self.eng[q])
        if q == "pool":
            if semb.psem is None:
                semb.psem = self.stack.enter_context(self.nc.semaphore("ps%d" % self.n_dsem))
                self.n_dsem += 1
            semb.pcnt += 16
            inst.then_inc(semb.psem, 16)
            ev = (semb.psem, semb.pcnt)
            k = semb.key + "p"
        else:
            sem = self._dsem(semb)
            semb.dcnt += 16
            inst.then_inc(sem, 16)
            ev = (sem, semb.dcnt)
            k = semb.key
        for b in reads:
            b.dr[k] = ev
        for b in writes:
            if concurrent:
                b.dw[k] = ev
            else:
                b.w = None
                b.r = {}
                b.dr = {}
                b.dw = {k: ev}
        self.ninst[q] += 1
        return inst

    def wait_all(self, e, bufs):
        self._deps(e, [], bufs)

    def barrier(self):
        for e in self.ENG:
            for o in self.ENG:
                if o != e:
                    self._wait_eng(e, o, self.tick[o])


class Ring:
    def __init__(self, S, name, shape, dt, n, stack=None):
        self.bufs = [S.sb("%s%d" % (name, i), shape, dt, stack) for i in range(n)]
        self.i = 0

    def next(self):
        b = self.bufs[self.i % len(self.bufs)]
        self.i += 1
        return b


def build(stage=99, dbg=False):
    nc = bass.Bass("TRN2", target_bir_lowering=False)

    def din(name, shape, dt=F32):
        return nc.dram_tensor(name, list(shape), dt, kind="ExternalInput").ap()

    def dscr(name, shape, dt):
        return nc.dram_tensor(name, list(shape), dt, kind="Internal").ap()

    A = {}
    A["x"] = din("x", [L, D]); A["ctx"] = din("ctx", [LC, D])
    A["c"] = din("c", [1, D]); A["c_ctx"] = din("c_ctx", [1, D])
    A["w_ada"] = din("w_ada", [D, 6 * D]); A["b_ada"] = din("b_ada", [1, 6 * D])
    for n_ in ("norm_pre_mix", "norm_post_mix", "norm_pre_ffn", "norm_post_ffn"):
        A[n_] = din(n_, [1, D])
    A["w_in"] = din("w_in", [D, 4096])
    A["s5_lam_re"] = din("s5_lam_re", [64, 64]); A["s5_lam_im"] = din("s5_lam_im", [64, 64])
    A["s5_log_dt"] = din("s5_log_dt", [1, 64])
    A["s5_b_re"] = din("s5_b_re", [64, 64, 16]); A["s5_b_im"] = din("s5_b_im", [64, 64, 16])
    A["s5_c_re"] = din("s5_c_re", [2, 512, 64]); A["s5_c_im"] = din("s5_c_im", [2, 512, 64])
    A["s5_d"] = din("s5_d", [1, 512]); A["w_glu"] = din("w_glu", [512, 512])
    A["da_lambda"] = din("da_lambda", [1, 256]); A["da_subln"] = din("da_subln", [1, 128])
    A["w_proj_a"] = din("w_proj_a", [512, D]); A["w_proj_b"] = din("w_proj_b", [512, D])
    A["w_out"] = din("w_out", [D, D]); A["w_router"] = din("w_router", [D, 16])
    A["w_exp_gate"] = din("w_exp_gate", [16, D, 2048]); A["w_exp_up"] = din("w_exp_up", [16, D, 2048])
    A["w_exp_down"] = din("w_exp_down", [16, 2048, D])
    out_ap = nc.dram_tensor("out", [L, D], F32, kind="ExternalOutput").ap()

    U_d = dscr("U_d", [5, 128, 4096], BF16)
    QT_d = dscr("QT_d", [512, L], BF16)
    KT_d = dscr("KT_d", [512, LC + L], BF16)
    V_d = dscr("V_d", [LC + L, 512], BF16)
    SG_d = dscr("SG_d", [2048, L], BF16)
    YA_d = dscr("YA_d", [512, L], BF16)
    YB_d = dscr("YB_d", [512, L], BF16)
    X1_d = dscr("X1_d", [L, D], F32)
    H2_d = dscr("H2_d", [L, D], BF16)
    F_d = dscr("F_d", [L, D], F32)
    DB = {}
    for k_ in ("x", "ctx", "c", "c_ctx", "w_ada", "b_ada", "norm_pre_mix", "norm_post_mix", "norm_pre_ffn",
               "norm_post_ffn", "w_in", "s5p", "w_glu", "da", "w_proj", "w_out", "w_router", "w_exp",
               "U", "QT", "KT", "V", "SG", "YA", "YB", "X1", "H2", "F", "out", "dbg"):
        DB[k_] = Buf("D_" + k_)

    dbg_out = {}

    def dbg_tensor(name, shape, dt=F32):
        ap = nc.dram_tensor("dbg_" + name, list(shape), dt, kind="ExternalOutput").ap()
        dbg_out[name] = ap
        return ap

    with ExitStack() as top:
        S = Sched(nc, top)
        psum = top.enter_context(nc.psum_tensor("psum", [128, 4096], F32))
        PB = [Buf("bank%d" % i, psum[:, i * 512:(i + 1) * 512]) for i in range(8)]

        ident_bf = S.sb("ident_bf", [128, 128], BF16)
        ident_f = S.sb("ident_f", [128, 128], F32)
        iota_f = S.sb("iota_f", [128, 512], F32)
        pidx = S.sb("pidx", [128, 1], F32)
        ones_f = S.sb("ones_f", [128, 128], F32)
        ones_bf = S.sb("ones_bf", [128, 128], BF16)
        S.op("pool", lambda e: e.iota(iota_f[:], pattern=[[1, 512]], base=0, channel_multiplier=0,
                                      allow_small_or_imprecise_dtypes=True), [], [iota_f])
        S.op("pool", lambda e: e.iota(pidx[:], pattern=[[0, 1]], base=0, channel_multiplier=1,
                                      allow_small_or_imprecise_dtypes=True), [], [pidx])
        S.op("dve", lambda e: e.tensor_scalar(out=ident_bf[:], in0=iota_f[:, 0:128], scalar1=pidx[:, 0:1], scalar2=None,
                                              op0=ALU.is_equal), [iota_f, pidx], [ident_bf])
        S.op("dve", lambda e: e.tensor_scalar(out=ident_f[:], in0=iota_f[:, 0:128], scalar1=pidx[:, 0:1], scalar2=None,
                                              op0=ALU.is_equal), [iota_f, pidx], [ident_f])
        S.op("dve", lambda e: e.memset(ones_f[:], 1.0), [], [ones_f])
        S.op("dve", lambda e: e.memset(ones_bf[:], 1.0), [], [ones_bf])

        modc = S.sb("modc", [128, 4, 8], F32)
        ROWS_d = dscr("ROWS_d", [128, 4 * D], F32)
        DB["ROWS"] = Buf("D_ROWS")
        aff = S.sb("aff", [128, 32, 16], F32)

        mk0 = S.mark()
        with ExitStack() as ph:
            cc = S.sb("cc", [128, 8, 2], F32, ph)
            sc = S.sb("sc", [128, 8, 2], F32, ph)
            bcol = S.sb("bcol", [128, 2, 8], F32, ph)
            ncol = S.sb("ncol", [128, 8], F32, ph)
            brow = S.sb("brow", [1, 6 * D], F32, ph)
            nrow = S.sb("nrow", [1, 3, D], F32, ph)
            mrow = S.sb("mrow", [1, 4, D], F32, ph)
            wring = Ring(S, "wada", [128, 8, 512], F32, 2, ph)
            tmpc = S.sb("tmpc", [128, 4, 8], F32, ph)
            rows = S.sb("rows", [128, 4, D], F32, ph)
            S.dma("sp", lambda e: e.dma_start(out=cc[:, :, 0], in_=A["c"].rearrange("o (k p) -> p (o k)", p=128),
                                              allow_slow_non_contiguous=True), [DB["c"]], [cc], cc)
            S.dma("sp", lambda e: e.dma_start(out=cc[:, :, 1], in_=A["c_ctx"].rearrange("o (k p) -> p (o k)", p=128),
                                              allow_slow_non_contiguous=True), [DB["c_ctx"]], [cc], cc, concurrent=True)
            S.dma("sp", lambda e: e.dma_start(out=bcol[:, :, :], in_=A["b_ada"][:, 0:2048].rearrange("o (j k p) -> p (o j) k", p=128, k=8),
                                              allow_slow_non_contiguous=True), [DB["b_ada"]], [bcol], bcol)
            S.dma("sp", lambda e: e.dma_start(out=ncol[:, :], in_=A["norm_pre_mix"].rearrange("o (k p) -> p (o k)", p=128),
                                              allow_slow_non_contiguous=True), [DB["norm_pre_mix"]], [ncol], ncol)
            S.dma("sp", lambda e: e.dma_start(out=brow[:, :], in_=A["b_ada"]), [DB["b_ada"]], [brow], brow)
            for i_, n_ in enumerate(("norm_post_mix", "norm_pre_ffn", "norm_post_ffn")):
                S.dma("sp", lambda e, i_=i_, n_=n_: e.dma_start(out=nrow[:, i_, :], in_=A[n_]), [DB[n_]], [nrow], nrow,
                      concurrent=(i_ > 0))
            S.op("act", lambda e: e.activation(out=sc[:], in_=cc[:], func=AF.Silu), [cc], [sc])
            wv = A["w_ada"].rearrange("(k p) n -> p k n", p=128)
            pcol = PB[0]
            for blk in range(4):
                wt = wring.next()
                S.dma("sp", lambda e, wt=wt, blk=blk: e.dma_start(out=wt[:], in_=wv[:, :, blk * 512:(blk + 1) * 512]),
                      [DB["w_ada"]], [wt], wt)
                for ct in range(4):
                    col = blk * 4 + ct
                    for kt in range(8):
                        S.op("pe", lambda e, wt=wt, ct=ct, kt=kt, col=col: e.matmul(
                            pcol[:, col * 2:col * 2 + 2], lhsT=wt[:, kt, ct * 128:(ct + 1) * 128], rhs=sc[:, kt, :],
                            start=(kt == 0), stop=(kt == 7)), [wt, sc], [pcol])
            pv = pcol[:, 0:32].rearrange("p (j k t) -> p j k t", j=2, k=8)
            for t_ in range(2):
                S.op("dve", lambda e, t_=t_: e.tensor_tensor(out=modc[:, 2 * t_ + 1, :], in0=pv[:, 0, :, t_], in1=bcol[:, 0, :], op=ALU.add),
                     [pcol, bcol], [modc])
                S.op("dve", lambda e, t_=t_: e.scalar_tensor_tensor(out=tmpc[:, t_, :], in0=pv[:, 1, :, t_], scalar=1.0, in1=bcol[:, 1, :],
                                                                   op0=ALU.add, op1=ALU.add), [pcol, bcol], [tmpc])
                S.op("dve", lambda e, t_=t_: e.tensor_tensor(out=modc[:, 2 * t_, :], in0=tmpc[:, t_, :], in1=ncol[:, :], op=ALU.mult),
                     [tmpc, ncol], [modc])
            for ch in range(4):
                for half in range(2):
                    blk = (2 + ch) * 2 + half
                    wt = wring.next()
                    S.dma("sp", lambda e, wt=wt, blk=blk: e.dma_start(out=wt[:], in_=wv[:, :, blk * 512:(blk + 1) * 512]),
                          [DB["w_ada"]], [wt], wt)
                    pr = PB[1 + (blk % 2)]
                    for kt in range(8):
                        S.op("pe", lambda e, wt=wt, kt=kt, pr=pr: e.matmul(pr[0:1, :], lhsT=sc[:, kt, 0:1], rhs=wt[:, kt, :],
                                                                          start=(kt == 0), stop=(kt == 7)), [wt, sc], [pr])
                    S.op("dve", lambda e, pr=pr, ch=ch, half=half, blk=blk: e.tensor_tensor(
                        out=mrow[0:1, ch, half * 512:(half + 1) * 512], in0=pr[0:1, :], in1=brow[0:1, blk * 512:(blk + 1) * 512], op=ALU.add),
                        [pr, brow], [mrow])
            S.op("dve", lambda e: e.tensor_tensor(out=mrow[0:1, 0, :], in0=mrow[0:1, 0, :], in1=nrow[0:1, 0, :], op=ALU.mult), [mrow, nrow], [mrow])
            S.op("dve", lambda e: e.scalar_tensor_tensor(out=mrow[0:1, 2, :], in0=mrow[0:1, 2, :], scalar=1.0, in1=nrow[0:1, 1, :],
                                                         op0=ALU.add, op1=ALU.mult), [mrow, nrow], [mrow])
            S.op("dve", lambda e: e.tensor_tensor(out=mrow[0:1, 3, :], in0=mrow[0:1, 3, :], in1=nrow[0:1, 2, :], op=ALU.mult), [mrow, nrow], [mrow])
            for ri, mi in enumerate((0, 2, 1, 3)):
                for half in range(2):
                    pr = PB[3 + ((ri * 2 + half) % 2)]
                    S.op("pe", lambda e, pr=pr, mi=mi, half=half: e.matmul(pr[:, :], lhsT=ones_f[0:1, :], rhs=mrow[0:1, mi, half * 512:(half + 1) * 512],
                                                                           start=True, stop=True), [ones_f, mrow], [pr])
                    S.op("act", lambda e, pr=pr, ri=ri, half=half: e.activation(out=rows[:, ri, half * 512:(half + 1) * 512], in_=pr[:, :], func=AF.Copy),
                         [pr], [rows])
            S.dma("sp", lambda e: e.dma_start(out=ROWS_d, in_=rows[:].rearrange("p a b -> p (a b)")), [rows], [DB["ROWS"]], rows)
            if dbg and stage <= 1:
                d1 = dbg_tensor("modc", [128, 32]); d2 = dbg_tensor("rows", [128, 4 * D])
                S.dma("sp", lambda e: e.dma_start(out=d1, in_=modc[:].rearrange("p a b -> p (a b)")), [modc], [DB["dbg"]], modc, concurrent=True)
                S.dma("sp", lambda e: e.dma_start(out=d2, in_=rows[:].rearrange("p a b -> p (a b)")), [rows], [DB["dbg"]], rows, concurrent=True)
            S.barrier()
        S.release(mk0)
        if stage == 0:
            return finish(nc, S, DB, dbg_out)

        PHASES(nc, S, A, DB, PB, top, dict(ident_bf=ident_bf, ident_f=ident_f, iota_f=iota_f, pidx=pidx, ones_f=ones_f, ones_bf=ones_bf,
                                           modc=modc, ROWS_d=ROWS_d, aff=aff, out=out_ap, U_d=U_d, QT_d=QT_d, KT_d=KT_d, V_d=V_d, SG_d=SG_d,
                                           YA_d=YA_d, YB_d=YB_d, X1_d=X1_d, H2_d=H2_d, F_d=F_d), stage, dbg, dbg_tensor)
        return finish(nc, S, DB, dbg_out)


def finish(nc, S, DB, dbg_out):
    S.wait_all("sp", [DB["out"], DB["dbg"]])
    S.barrier()
    nc._dbg_out = dbg_out
    nc._stats = (dict(S.ninst), S.nwait, S.n_dsem)
    return nc


def PHASES(nc, S, A, DB, PB, top, K, stage, dbg, dbg_tensor):
    def run(fn):
        mk_ = S.mark()
        fn(nc, S, A, DB, PB, K, stage, dbg, dbg_tensor)
        S.release(mk_)
    if stage != 30:
        run(phase12)
    if stage <= 2:
        return
    if stage != 4:
        run(phase3)
    if stage in (3, 30):
        return
    run(phase4)
    if stage == 4:
        return
    run(phase5)
    if stage == 5:
        return
    run(phase678)
    if stage == 6:
        return


def phase12(nc, S, A, DB, PB, K, stage, dbg, dbg_tensor):
    ident_bf = K["ident_bf"]; modc = K["modc"]; iota_f = K["iota_f"]; pidx = K["pidx"]
    with ExitStack() as ph:
        hT = S.sb("hT", [128, 8, NT * 128], BF16, ph)
        cosT = S.sb("cosT", [128, L], BF16, ph)
        sinT = S.sb("sinT", [128, L], BF16, ph)
        with ExitStack() as ph0:
            fi = S.sb("fi", [128, 8], F32, ph0)
            wcol = S.sb("wcol", [128, 4], F32, ph0)
            rc = S.sb("rc", [128, 2, 64], F32, ph0)
            ang = S.sb("ang", [128, 64, 64], F32, ph0)
            t1 = S.sb("t1", [128, L], F32, ph0)
            t2 = S.sb("t2", [128, L], F32, ph0)

            def pfloor(dst, div, off):
                S.op("dve", lambda e: e.tensor_scalar(out=dst, in0=pidx[:, 0:1], scalar1=1.0 / div, scalar2=-off, op0=ALU.mult, op1=ALU.add), [pidx], [fi])
                S.op("dve", lambda e: e.tensor_scalar(out=dst, in0=dst, scalar1=MAGIC, scalar2=None, op0=ALU.add), [fi], [fi])
                S.op("dve", lambda e: e.tensor_scalar(out=dst, in0=dst, scalar1=-MAGIC, scalar2=None, op0=ALU.add), [fi], [fi])
            pfloor(fi[:, 2:3], 16.0, 0.46875)
            pfloor(fi[:, 3:4], 32.0, 0.484375)
            pfloor(fi[:, 4:5], 64.0, 0.4921875)
            S.op("dve", lambda e: e.scalar_tensor_tensor(out=fi[:, 0:1], in0=fi[:, 2:3], scalar=-16.0, in1=pidx[:, 0:1], op0=ALU.mult, op1=ALU.add), [fi, pidx], [fi])
            S.op("dve", lambda e: e.scalar_tensor_tensor(out=fi[:, 1:2], in0=fi[:, 4:5], scalar=-2.0, in1=fi[:, 3:4], op0=ALU.mult, op1=ALU.add), [fi], [fi])
            S.op("act", lambda e: e.activation(out=wcol[:, 0:1], in_=fi[:, 0:1], func=AF.Exp, scale=-math.log(10000.0) / 16.0), [fi], [wcol])
            S.op("dve", lambda e: e.tensor_tensor(out=wcol[:, 2:3], in0=wcol[:, 0:1], in1=fi[:, 1:2], op=ALU.mult), [wcol, fi], [wcol])
            S.op("dve", lambda e: e.tensor_tensor(out=wcol[:, 1:2], in0=wcol[:, 0:1], in1=wcol[:, 2:3], op=ALU.subtract), [wcol], [wcol])
            S.op("dve", lambda e: e.tensor_scalar(out=rc[:, 0, :], in0=iota_f[:, 0:64], scalar1=wcol[:, 1:2], scalar2=1.0 / TWO_PI,
                                                  op0=ALU.mult, op1=ALU.mult), [iota_f, wcol], [rc])
            S.op("dve", lambda e: e.tensor_scalar(out=rc[:, 1, :], in0=iota_f[:, 0:64], scalar1=wcol[:, 2:3], scalar2=1.0 / TWO_PI,
                                                  op0=ALU.mult, op1=ALU.mult), [iota_f, wcol], [rc])
            S.op("dve", lambda e: e.tensor_tensor(out=ang[:], in0=rc[:, 0, :].unsqueeze(2).to_broadcast([128, 64, 64]),
                                                  in1=rc[:, 1, :].unsqueeze(1).to_broadcast([128, 64, 64]), op=ALU.add), [rc], [ang])
            angf = ang[:].rearrange("p a b -> p (a b)")
            for tab, off in ((sinT, 0.0), (cosT, 0.25)):
                S.op("dve", lambda e, off=off: e.tensor_scalar(out=t1[:], in0=angf, scalar1=off, scalar2=MAGIC, op0=ALU.add, op1=ALU.add), [ang], [t1])
                S.op("dve", lambda e: e.tensor_scalar(out=t1[:], in0=t1[:], scalar1=-MAGIC, scalar2=None, op0=ALU.add), [t1], [t1])
                S.op("dve", lambda e, off=off: e.scalar_tensor_tensor(out=t2[:], in0=angf, scalar=off, in1=t1[:], op0=ALU.add, op1=ALU.subtract),
                     [ang, t1], [t2])
                S.op("act", lambda e, tab=tab: e.activation(out=tab[:], in_=t2[:], func=AF.Sin, scale=TWO_PI * 0.999999), [t2], [tab])
            S.barrier()

        xring = Ring(S, "xt", [128, D], F32, 3, ph)
        xnring = Ring(S, "xn", [128, D], BF16, 2, ph)
        junk = S.sb("junk", [128, D], BF16, ph)
        stat_ring = Ring(S, "stat", [128, 1, 4], F32, 6, ph)
        for i in range(NT):
            xt = xring.next(); xn = xnring.next(); stat = stat_ring.next()
            src = A["ctx"][i * 128:(i + 1) * 128, :] if i < 2 else A["x"][(i - 2) * 128:(i - 1) * 128, :]
            srcb = DB["ctx"] if i < 2 else DB["x"]
            S.dma("sp", lambda e, xt=xt, src=src: e.dma_start(out=xt[:], in_=src), [srcb], [xt], xt)
            S.op("act", lambda e, xt=xt, i=i: e.activation(out=junk[:], in_=xt[:], func=AF.Square, accum_out=stat[:, 0, 0:1]), [xt], [junk, stat])
            S.op("act", lambda e, i=i: e.activation(out=stat[:, 0, 1:2], in_=stat[:, 0, 0:1], func=AF.Sqrt, scale=1.0 / D, bias=EPS), [stat], [stat])
            S.op("dve", lambda e, i=i: e.reciprocal(out=stat[:, 0, 2:3], in_=stat[:, 0, 1:2]), [stat], [stat])
            S.op("dve", lambda e, xt=xt, xn=xn, i=i: e.tensor_scalar(out=xn[:], in0=xt[:], scalar1=stat[:, 0, 2:3], scalar2=None, op0=ALU.mult),
                 [xt, stat], [xn])
            pt = PB[i % 2]
            ptb = pt.t.bitcast(BF16)
            for dt_ in range(8):
                S.op("pe", lambda e, ptb=ptb, xn=xn, dt_=dt_: e.transpose(ptb[:, dt_ * 128:(dt_ + 1) * 128], xn[:, dt_ * 128:(dt_ + 1) * 128], ident_bf[:]),
                     [xn, ident_bf], [pt])
            mi = 2 if i < 2 else 0
            pv = ptb.rearrange("p (a b) -> p a b", a=8)
            hv = hT[:, :, i * 128:(i + 1) * 128]
            S.op("dve", lambda e, pv=pv, hv=hv, mi=mi: e.tensor_tensor(out=hv, in0=pv, in1=modc[:, mi, :].unsqueeze(2).to_broadcast([128, 8, 128]), op=ALU.mult),
                 [pt, modc], [hT])
            S.op("dve", lambda e, hv=hv, mi=mi: e.tensor_tensor(out=hv, in0=hv, in1=modc[:, mi + 1, :].unsqueeze(2).to_broadcast([128, 8, 128]), op=ALU.add),
                 [hT, modc], [hT])
        if dbg and stage == 1:
            d1 = dbg_tensor("hT", [128, 8 * NT * 128], BF16)
            S.dma("sp", lambda e: e.dma_start(out=d1, in_=hT[:].rearrange("p a b -> p (a b)")), [hT], [DB["dbg"]], hT, concurrent=True)
            S.barrier()
            return
        S.barrier()

        U_d = K["U_d"]; QT_d = K["QT_d"]; KT_d = K["KT_d"]; V_d = K["V_d"]; SG_d = K["SG_d"]
        wv = A["w_in"].rearrange("(k p) n -> p k n", p=128)
        wring = Ring(S, "win", [128, 8, 512], BF16, 2, ph)
        wsw = S.sb("wsw", [128, 8, 512], BF16, ph)
        oring = Ring(S, "o2", [128, 512], BF16, 4, ph)
        tring = Ring(S, "t32", [128, 512], F32, 4, ph)
        bank_i = [0]

        def pbank():
            b = PB[2 + (bank_i[0] % 6)]
            bank_i[0] += 1
            return b

        def load_w(c0):
            wt = wring.next()
            S.dma("pool", lambda e: e.dma_start(out=wt[:], in_=wv[:, :, c0:c0 + 512]), [DB["w_in"]], [wt], wt)
            return wt

        def tok_major(c0, dst_rows):
            wt = load_w(c0)
            for i in range(NT):
                pb = pbank()
                for kt in range(8):
                    S.op("pe", lambda e, pb=pb, kt=kt, i=i: e.matmul(pb[:, :], lhsT=hT[:, kt, i * 128:(i + 1) * 128], rhs=wt[:, kt, :],
                                                                    start=(kt == 0), stop=(kt == 7)), [hT, wt], [pb])
                ob = oring.next()
                S.op("act", lambda e, pb=pb, ob=ob: e.activation(out=ob[:], in_=pb[:, :], func=AF.Copy), [pb], [ob])
                for (dap, dbuf) in dst_rows(i):
                    S.dma("sp", lambda e, dap=dap, ob=ob: e.dma_start(out=dap, in_=ob[:]), [ob], [dbuf], ob, concurrent=True)

        wt_u = load_w(0)
        ucm_ring = Ring(S, "ucm", [128, 32, 8, 16], BF16, 2, ph)
        for ct in range(5):
            nchunk = 32 if ct == 0 else 128
            t0_ = 0 if ct == 0 else LC + (ct - 1) * 1024
            ucm = ucm_ring.next()
            for j in range(8):
                pb = pbank()
                for kt in range(8):
                    lh = hT[:, kt, t0_:t0_ + nchunk * 8].rearrange("p (c j) -> p c j", j=8)[:, :, j]
                    S.op("pe", lambda e, pb=pb, kt=kt, lh=lh, nchunk=nchunk: e.matmul(pb[0:nchunk, :], lhsT=lh, rhs=wt_u[:, kt, :], start=(kt == 0), stop=(kt == 7)),
                         [hT, wt_u], [pb])
                if j % 2 == 0:
                    S.op("act", lambda e, pb=pb, ucm=ucm, j=j, nchunk=nchunk: e.activation(out=ucm[0:nchunk, :, j, :], in_=pb[0:nchunk, :].rearrange("p (g h) -> p g h", h=16), func=AF.Copy),
                         [pb], [ucm])
                else:
                    S.op("dve", lambda e, pb=pb, ucm=ucm, j=j, nchunk=nchunk: e.tensor_copy(out=ucm[0:nchunk, :, j, :], in_=pb[0:nchunk, :].rearrange("p (g h) -> p g h", h=16)),
                         [pb], [ucm])
            S.dma("sp", lambda e, ucm=ucm, ct=ct, nchunk=nchunk: e.dma_start(out=U_d[ct, 0:nchunk, :], in_=ucm[0:nchunk, :, :, :].rearrange("p g j h -> p (g j h)")),
                  [ucm], [DB["U"]], ucm, concurrent=True)
        tok_major(1536, lambda i: [(V_d[i * 128:(i + 1) * 128, :], DB["V"])])

        def rope_block(c0, dstT, dbuf, with_ctx):
            wt = load_w(c0)
            wtv = wt[:].rearrange("p k (a two h) -> p k a two h", two=2, h=16)
            wsv = wsw[:].rearrange("p k (a two h) -> p k a two h", two=2, h=16)
            S.op("pool", lambda e: e.tensor_scalar(out=wsv[:, :, :, 0, :], in0=wtv[:, :, :, 1, :], scalar1=-1.0, scalar2=None, op0=ALU.mult), [wt], [wsw])
            S.op("pool", lambda e: e.tensor_copy(out=wsv[:, :, :, 1, :], in_=wtv[:, :, :, 0, :]), [wt], [wsw])
            for ft in range(4):
                if with_ctx:
                    pb = pbank()
                    for kt in range(8):
                        S.op("pe", lambda e, pb=pb, kt=kt, ft=ft: e.matmul(pb[:, 0:LC], lhsT=wt[:, kt, ft * 128:(ft + 1) * 128], rhs=hT[:, kt, 0:LC],
                                                                         start=(kt == 0), stop=(kt == 7)), [hT, wt], [pb])
                    ob = oring.next()
                    S.op("act", lambda e, pb=pb, ob=ob: e.activation(out=ob[:, 0:LC], in_=pb[:, 0:LC], func=AF.Copy), [pb], [ob])
                    S.dma("sp", lambda e, ob=ob, ft=ft: e.dma_start(out=dstT[ft * 128:(ft + 1) * 128, 0:LC], in_=ob[:, 0:LC]), [ob], [dbuf], ob, concurrent=True)
                coff = LC if with_ctx else 0
                for tb in range(8):
                    p1 = pbank(); p2 = pbank()
                    t0_ = LC + tb * 512
                    for (pp, ww) in ((p1, wt), (p2, wsw)):
                        for kt in range(8):
                            S.op("pe", lambda e, pp=pp, ww=ww, kt=kt, ft=ft, t0_=t0_: e.matmul(pp[:, :], lhsT=ww[:, kt, ft * 128:(ft + 1) * 128],
                                                                                              rhs=hT[:, kt, t0_:t0_ + 512], start=(kt == 0), stop=(kt == 7)),
                                 [hT, ww], [pp])
                    ta = tring.next(); tb_ = tring.next(); ob = oring.next()
                    S.op("dve", lambda e, p1=p1, ta=ta, tb=tb: e.tensor_tensor(out=ta[:], in0=p1[:, :], in1=cosT[:, tb * 512:(tb + 1) * 512], op=ALU.mult),
                         [p1, cosT], [ta])
                    S.op("dve", lambda e, p2=p2, tb_=tb_, tb=tb: e.tensor_tensor(out=tb_[:], in0=p2[:, :], in1=sinT[:, tb * 512:(tb + 1) * 512], op=ALU.mult),
                         [p2, sinT], [tb_])
                    S.op("pool", lambda e, ta=ta, tb_=tb_, ob=ob: e.tensor_tensor(out=ob[:], in0=ta[:], in1=tb_[:], op=ALU.add), [ta, tb_], [ob])
                    S.dma("sp", lambda e, ob=ob, ft=ft, tb=tb, coff=coff: e.dma_start(out=dstT[ft * 128:(ft + 1) * 128, coff + tb * 512:coff + (tb + 1) * 512], in_=ob[:]),
                          [ob], [dbuf], ob, concurrent=True)

        rope_block(512, QT_d, DB["QT"], False)
        rope_block(1024, KT_d, DB["KT"], True)

        for j in range(4):
            wt = load_w(2048 + j * 512)
            for ft in range(4):
                for tb in range(8):
                    pb = pbank()
                    t0_ = LC + tb * 512
                    for kt in range(8):
                        S.op("pe", lambda e, pb=pb, kt=kt, ft=ft, t0_=t0_, wt=wt: e.matmul(pb[:, :], lhsT=wt[:, kt, ft * 128:(ft + 1) * 128],
                                                                                          rhs=hT[:, kt, t0_:t0_ + 512], start=(kt == 0), stop=(kt == 7)),
                             [hT, wt], [pb])
                    ob = oring.next()
                    S.op("act", lambda e, pb=pb, ob=ob: e.activation(out=ob[:], in_=pb[:, :], func=AF.Sigmoid), [pb], [ob])
                    r0 = (j * 4 + ft) * 128
                    S.dma("sp", lambda e, ob=ob, r0=r0, tb=tb: e.dma_start(out=SG_d[r0:r0 + 128, tb * 512:(tb + 1) * 512], in_=ob[:]), [ob], [DB["SG"]], ob,
                          concurrent=True)
        S.barrier()
        if dbg and stage == 2:
            stg = Ring(S, "dstg", [128, 2176], BF16, 2, ph)
            for (nm, src, dbk, rows_, cols_) in (("V", V_d, "V", LC + L, 512), ("QT", QT_d, "QT", 512, L),
                                               ("KT", KT_d, "KT", 512, LC + L), ("SG", SG_d, "SG", 2048, L)):
                dd = dbg_tensor(nm, [rows_, cols_], BF16)
                for r0 in range(0, rows_, 128):
                    for c0 in range(0, cols_, 2176):
                        cw = min(2176, cols_ - c0)
                        sg = stg.next()
                        S.dma("sp", lambda e, sg=sg, src=src, r0=r0, c0=c0, cw=cw: e.dma_start(out=sg[:, 0:cw], in_=src[r0:r0 + 128, c0:c0 + cw]), [DB[dbk]], [sg], sg)
                        S.dma("sp", lambda e, sg=sg, dd=dd, r0=r0, c0=c0, cw=cw: e.dma_start(out=dd[r0:r0 + 128, c0:c0 + cw], in_=sg[:, 0:cw]), [sg], [DB["dbg"]], sg,
                              concurrent=True)
            S.barrier()


def phase3(nc, S, A, DB, PB, K, stage, dbg, dbg_tensor):
    ident_bf = K["ident_bf"]; ident_f = K["ident_f"]; iota_f = K["iota_f"]
    U_d = K["U_d"]; YA_d = K["YA_d"]
    YT_d = nc.dram_tensor("YT_d", [512, L], BF16, kind="Internal").ap()
    DB_YT = Buf("D_YT")
    NCH = 544
    with ExitStack() as ph:
        M_bf = S.sb("M_bf", [128, 32, 128], BF16, ph)
        W1_bf = S.sb("W1_bf", [128, 64, 2, 64], BF16, ph)
        W3_bf = S.sb("W3_bf", [64, 64, 2, 128], BF16, ph)
        PW = S.sb("PW", [64, 64, 2, 29], F32, ph)
        PWr = S.sb("PWr", [64, 32, 2, 16], F32, ph)
        TAUS = list(range(9)) + [8 * k for k in range(2, 17)]
        with ExitStack() as p0:
            nat = S.sb("nat", [64, 2, 64], F32, p0)
            lamT = S.sb("lamT", [64, 2, 64], F32, p0)
            dtb = S.sb("dtb", [64, 64], F32, p0)
            sm = S.sb("sm", [64, 8, 64], F32, p0)
            Bb = S.sb("Bb", [64, 2, 64, 16], F32, p0)
            XC = S.sb("XC", [64, 2, 64, 9, 16], BF16, p0)
            Dcol = S.sb("Dcol", [128, 32], F32, p0)
            pA = ExitStack()
            xr = S.sb("xr", [64, 64], F32, pA)
            an = S.sb("an", [64, 64], F32, pA)
            tau = S.sb("tau", [64, 29], F32, pA)
            PH = S.sb("PH", [64, 64, 29], F32, pA)
            Y1 = S.sb("Y1", [64, 64, 29], F32, pA)
            Y2 = S.sb("Y2", [64, 64, 29], F32, pA)
            MG = S.sb("MG", [64, 64, 29], F32, pA)
            S.dma("sp", lambda e: e.dma_start(out=nat[:, 0, :], in_=A["s5_lam_re"]), [DB["s5p"]], [nat], nat)
            S.dma("sp", lambda e: e.dma_start(out=nat[:, 1, :], in_=A["s5_lam_im"]), [DB["s5p"]], [nat], nat, concurrent=True)
            S.dma("sp", lambda e: e.dma_start(out=dtb[:], in_=A["s5_log_dt"].partition_broadcast(64)), [DB["s5p"]], [dtb], dtb)
            for s_ in range(8):
                S.dma("sp", lambda e, s_=s_: e.dma_start(out=Dcol[16 * s_:16 * s_ + 16, :], in_=A["s5_d"][0, :].rearrange("(g h) -> h g", h=16),
                                                         allow_slow_non_contiguous=True), [DB["s5p"]], [Dcol], Dcol, concurrent=(s_ > 0))
            pb = PB[0]
            for ri in range(2):
                S.op("pe", lambda e, ri=ri: e.transpose(pb[0:64, ri * 64:(ri + 1) * 64], nat[:, ri, :], ident_f[0:64, 0:64]), [nat, ident_f], [pb])
            S.op("dve", lambda e: e.tensor_copy(out=lamT[:].rearrange("p a b -> p (a b)"), in_=pb[0:64, 0:128]), [pb], [lamT])
            S.op("dve", lambda e: e.tensor_scalar(out=lamT[:, 0, :], in0=lamT[:, 0, :], scalar1=-1e-4, scalar2=None, op0=ALU.min), [lamT], [lamT])
            S.op("act", lambda e: e.activation(out=dtb[:], in_=dtb[:], func=AF.Exp), [dtb], [dtb])
            S.op("dve", lambda e: e.tensor_tensor(out=xr[:], in0=lamT[:, 0, :], in1=dtb[:], op=ALU.mult), [lamT, dtb], [xr])
            S.op("dve", lambda e: e.tensor_scalar(out=an[:], in0=lamT[:, 1, :], scalar1=dtb[:, 0:1] if False else 1.0 / TWO_PI, scalar2=None, op0=ALU.mult), [lamT], [an])
            S.op("dve", lambda e: e.tensor_tensor(out=an[:], in0=an[:], in1=dtb[:], op=ALU.mult), [an, dtb], [an])
            S.op("dve", lambda e: e.tensor_copy(out=tau[:, 0:9], in_=iota_f[0:64, 0:9]), [iota_f], [tau])
            S.op("dve", lambda e: e.tensor_scalar(out=tau[:, 9:24], in0=iota_f[0:64, 2:17], scalar1=8.0, scalar2=None, op0=ALU.mult), [iota_f], [tau])
            for j_ in range(5):
                S.op("dve", lambda e, j_=j_: e.memset(tau[:, 24 + j_:25 + j_], float(256 * (2 ** j_))), [], [tau])
            bc_dg = lambda t: t[:].unsqueeze(2).to_broadcast([64, 64, 29])
            bc_tau = tau[:].unsqueeze(1).to_broadcast([64, 64, 29])
            S.op("dve", lambda e: e.tensor_tensor(out=PH[:], in0=bc_dg(an), in1=bc_tau, op=ALU.mult), [an, tau], [PH])
            S.op("dve", lambda e: e.tensor_tensor(out=MG[:], in0=bc_dg(xr), in1=bc_tau, op=ALU.mult), [xr, tau], [MG])
            S.op("act", lambda e: e.activation(out=MG[:], in_=MG[:], func=AF.Exp), [MG], [MG])
            for ri, off in ((0, 0.25), (1, 0.0)):
                S.op("dve", lambda e, off=off: e.tensor_scalar(out=Y1[:], in0=PH[:], scalar1=off, scalar2=MAGIC, op0=ALU.add, op1=ALU.add), [PH], [Y1])
                S.op("dve", lambda e: e.tensor_scalar(out=Y1[:], in0=Y1[:], scalar1=-MAGIC, scalar2=None, op0=ALU.add), [Y1], [Y1])
                S.op("dve", lambda e, off=off: e.scalar_tensor_tensor(out=Y2[:], in0=PH[:], scalar=off, in1=Y1[:], op0=ALU.add, op1=ALU.subtract), [PH, Y1], [Y2])
                S.op("act", lambda e: e.activation(out=Y2[:], in_=Y2[:], func=AF.Sin, scale=TWO_PI * 0.999999), [Y2], [Y2])
                S.op("dve", lambda e, ri=ri: e.tensor_tensor(out=PW[:, :, ri, :], in0=Y2[:], in1=MG[:], op=ALU.mult), [Y2, MG], [PW])
            for k_ in range(16):
                S.op("dve", lambda e, k_=k_: e.tensor_copy(out=PWr[:, :, :, k_], in_=PW[:, 32:64, :, 8 + 15 - k_]), [PW], [PWr])
            S.barrier(); pA.close()
            lr = lamT[:, 0, :]; li = lamT[:, 1, :]
            lbr = PW[:, :, 0, 1]; lbi = PW[:, :, 1, 1]
            den, rden, nre, cre, cim, t_a, t_b = (sm[:, i, :] for i in range(7))
            S.op("dve", lambda e: e.tensor_tensor(out=den, in0=lr, in1=lr, op=ALU.mult), [lamT], [sm])
            S.op("dve", lambda e: e.tensor_tensor(out=t_a, in0=li, in1=li, op=ALU.mult), [lamT], [sm])
            S.op("dve", lambda e: e.tensor_tensor(out=den, in0=den, in1=t_a, op=ALU.add), [sm], [sm])
            S.op("dve", lambda e: e.reciprocal(out=rden, in_=den), [sm], [sm])
            S.op("dve", lambda e: e.tensor_scalar(out=nre, in0=lbr, scalar1=-1.0, scalar2=None, op0=ALU.add), [PW], [sm])
            S.op("dve", lambda e: e.tensor_tensor(out=t_a, in0=nre, in1=lr, op=ALU.mult), [sm, lamT], [sm])
            S.op("dve", lambda e: e.tensor_tensor(out=t_b, in0=lbi, in1=li, op=ALU.mult), [PW, lamT], [sm])
            S.op("dve", lambda e: e.tensor_tensor(out=t_a, in0=t_a, in1=t_b, op=ALU.add), [sm], [sm])
            S.op("dve", lambda e: e.tensor_tensor(out=cre, in0=t_a, in1=rden, op=ALU.mult), [sm], [sm])
            S.op("dve", lambda e: e.tensor_tensor(out=t_a, in0=lbi, in1=lr, op=ALU.mult), [PW, lamT], [sm])
            S.op("dve", lambda e: e.tensor_tensor(out=t_b, in0=nre, in1=li, op=ALU.mult), [sm, lamT], [sm])
            S.op("dve", lambda e: e.tensor_tensor(out=t_a, in0=t_a, in1=t_b, op=ALU.subtract), [sm], [sm])
            S.op("dve", lambda e: e.tensor_tensor(out=cim, in0=t_a, in1=rden, op=ALU.mult), [sm], [sm])
            bch = lambda t: t.unsqueeze(2).to_broadcast([64, 64, 16])
            pB = ExitStack()
            Bn = S.sb("Bn", [64, 2, 64, 16], F32, pB)
            tb1 = S.sb("tb1", [64, 64, 16], F32, pB)
            for ri, nm in enumerate(("s5_b_re", "s5_b_im")):
                S.dma("sp", lambda e, ri=ri, nm=nm: e.dma_start(out=Bn[:, ri, :, :], in_=A[nm].rearrange("a n h -> n a h")), [DB["s5p"]], [Bn], Bn,
                      concurrent=(ri > 0))
            S.op("dve", lambda e: e.tensor_tensor(out=Bb[:, 0, :, :], in0=Bn[:, 0, :, :], in1=bch(cre), op=ALU.mult), [Bn, sm], [Bb])
            S.op("dve", lambda e: e.tensor_tensor(out=tb1[:], in0=Bn[:, 1, :, :], in1=bch(cim), op=ALU.mult), [Bn, sm], [tb1])
            S.op("dve", lambda e: e.tensor_tensor(out=Bb[:, 0, :, :], in0=Bb[:, 0, :, :], in1=tb1[:], op=ALU.subtract), [Bb, tb1], [Bb])
            S.op("dve", lambda e: e.tensor_tensor(out=Bb[:, 1, :, :], in0=Bn[:, 1, :, :], in1=bch(cre), op=ALU.mult), [Bn, sm], [Bb])
            S.op("dve", lambda e: e.tensor_tensor(out=tb1[:], in0=Bn[:, 0, :, :], in1=bch(cim), op=ALU.mult), [Bn, sm], [tb1])
            S.op("dve", lambda e: e.tensor_tensor(out=Bb[:, 1, :, :], in0=Bb[:, 1, :, :], in1=tb1[:], op=ALU.add), [Bb, tb1], [Bb])
            S.barrier(); pB.close()
            pC = ExitStack()
            cnat = S.sb("cnat", [128, 2, 2, 4, 64], F32, pC)
            CT = S.sb("CT", [64, 2, 64, 16], F32, pC)
            tx = S.sb("tx", [64, 32, 9, 16], F32, pC)
            tx2 = S.sb("tx2", [64, 32, 9, 16], F32, pC)
            for ri, nm in enumerate(("s5_c_re", "s5_c_im")):
                for d_ in range(2):
                    S.dma("sp", lambda e, ri=ri, nm=nm, d_=d_: e.dma_start(out=cnat[:, ri, d_, :, :], in_=A[nm][d_].rearrange("(t p) n -> p t n", p=128)),
                          [DB["s5p"]], [cnat], cnat, concurrent=(ri + d_ > 0))
            for ri in range(2):
                for d_ in range(2):
                    pb = PB[1 + ((ri * 2 + d_) % 2)]
                    for t4 in range(4):
                        S.op("pe", lambda e, pb=pb, ri=ri, d_=d_, t4=t4: e.transpose(pb[0:64, t4 * 128:(t4 + 1) * 128], cnat[:, ri, d_, t4, :], ident_f[:]),
                             [cnat, ident_f], [pb])
                    S.op("act", lambda e, pb=pb, ri=ri, d_=d_: e.activation(out=CT[:, ri, d_ * 32:(d_ + 1) * 32, :].rearrange("p a h -> p (a h)"), in_=pb[0:64, :], func=AF.Copy),
                         [pb], [CT])
            for d_ in range(2):
                dsl = slice(d_ * 32, (d_ + 1) * 32)
                cb = lambda ri, dsl=dsl: CT[:, ri, dsl, :].unsqueeze(2).to_broadcast([64, 32, 9, 16])
                pwb = lambda ri, dsl=dsl: PW[:, dsl, ri, 0:9].unsqueeze(3).to_broadcast([64, 32, 9, 16])
                S.op("dve", lambda e, cb=cb, pwb=pwb: e.tensor_tensor(out=tx[:], in0=cb(0), in1=pwb(0), op=ALU.mult), [CT, PW], [tx])
                S.op("pool", lambda e, cb=cb, pwb=pwb: e.tensor_tensor(out=tx2[:], in0=cb(1), in1=pwb(1), op=ALU.mult), [CT, PW], [tx2])
                S.op("dve", lambda e, dsl=dsl: e.tensor_tensor(out=XC[:, 0, dsl, :, :], in0=tx[:], in1=tx2[:], op=ALU.subtract), [tx, tx2], [XC])
                S.op("dve", lambda e, cb=cb, pwb=pwb: e.tensor_tensor(out=tx[:], in0=cb(0), in1=pwb(1), op=ALU.mult), [CT, PW], [tx])
                S.op("pool", lambda e, cb=cb, pwb=pwb: e.tensor_tensor(out=tx2[:], in0=cb(1), in1=pwb(0), op=ALU.mult), [CT, PW], [tx2])
                S.op("dve", lambda e, dsl=dsl: e.scalar_tensor_tensor(out=XC[:, 1, dsl, :, :], in0=tx[:], scalar=-1.0, in1=tx2[:], op0=ALU.mult, op1=ALU.subtract), [tx, tx2], [XC])
            S.barrier(); pC.close()
            for ri in range(2):
                S.op("act", lambda e, ri=ri: e.activation(out=W3_bf[:, 0:32, ri, :].rearrange("p a (j h) -> p a j h", h=16), in_=XC[:, ri, 0:32, 1:9, :], func=AF.Copy), [XC], [W3_bf])
                for j in range(8):
                    S.op("pool", lambda e, ri=ri, j=j: e.tensor_copy(out=W3_bf[:, 32:64, ri, j * 16:(j + 1) * 16], in_=XC[:, ri, 32:64, 8 - j, :]), [XC], [W3_bf])
            pD = ExitStack()
            W1T = S.sb("W1T", [64, 32, 2, 128], BF16, pD)
            tq = S.sb("tq", [64, 32, 8, 16], F32, pD)
            tq2 = S.sb("tq2", [64, 32, 8, 16], F32, pD)
            PWj = S.sb("PWj", [64, 64, 2, 8], F32, pD)
            for j in range(8):
                S.op("dve", lambda e, j=j: e.tensor_copy(out=PWj[:, 0:32, :, j], in_=PW[:, 0:32, :, 7 - j]), [PW], [PWj])
                S.op("dve", lambda e, j=j: e.tensor_copy(out=PWj[:, 32:64, :, j], in_=PW[:, 32:64, :, j]), [PW], [PWj])
            for d_ in range(2):
                dsl = slice(d_ * 32, (d_ + 1) * 32)
                pj = lambda ri, dsl=dsl: PWj[:, dsl, ri, :].unsqueeze(3).to_broadcast([64, 32, 8, 16])
                bj = lambda ri, dsl=dsl: Bb[:, ri, dsl, :].unsqueeze(2).to_broadcast([64, 32, 8, 16])
                w1v = lambda ri: W1T[:, :, ri, :].rearrange("p a (j h) -> p a j h", h=16)
                S.op("dve", lambda e, pj=pj, bj=bj: e.tensor_tensor(out=tq[:], in0=pj(0), in1=bj(0), op=ALU.mult), [PWj, Bb], [tq])
                S.op("pool", lambda e, pj=pj, bj=bj: e.tensor_tensor(out=tq2[:], in0=pj(1), in1=bj(1), op=ALU.mult), [PWj, Bb], [tq2])
                S.op("dve", lambda e, w1v=w1v: e.tensor_tensor(out=w1v(0), in0=tq[:], in1=tq2[:], op=ALU.subtract), [tq, tq2], [W1T])
                S.op("dve", lambda e, pj=pj, bj=bj: e.tensor_tensor(out=tq[:], in0=pj(0), in1=bj(1), op=ALU.mult), [PWj, Bb], [tq])
                S.op("pool", lambda e, pj=pj, bj=bj: e.tensor_tensor(out=tq2[:], in0=pj(1), in1=bj(0), op=ALU.mult), [PWj, Bb], [tq2])
                S.op("dve", lambda e, w1v=w1v: e.tensor_tensor(out=w1v(1), in0=tq[:], in1=tq2[:], op=ALU.add), [tq, tq2], [W1T])
                for g in range(32):
                    dg = d_ * 32 + g
                    pb = PB[dg % 2]
                    pbb = pb.t.bitcast(BF16)
                    for ri in range(2):
                        S.op("pe", lambda e, pbb=pbb, g=g, ri=ri: e.transpose(pbb[:, ri * 64:(ri + 1) * 64], W1T[:, g, ri, :], ident_bf[0:64, 0:64]), [W1T, ident_bf], [pb])
                    if dg % 2 == 0:
                        S.op("act", lambda e, pbb=pbb, dg=dg: e.activation(out=W1_bf[:, dg, :, :].rearrange("p a b -> p (a b)"), in_=pbb[:, 0:128], func=AF.Copy), [pb], [W1_bf])
                    else:
                        S.op("dve", lambda e, pbb=pbb, dg=dg: e.tensor_copy(out=W1_bf[:, dg, :, :].rearrange("p a b -> p (a b)"), in_=pbb[:, 0:128]), [pb], [W1_bf])
            S.barrier(); pD.close()
            Bpad = [S.sb("Bpad%d" % i, [64, 2, 2, 240], BF16, p0) for i in range(2)]
            Xpad = [S.sb("Xpad%d" % i, [64, 2, 2, 240], BF16, p0) for i in range(2)]
            for i in range(2):
                S.op("pool", lambda e, i=i: e.memset(Bpad[i][:], 0.0), [], [Bpad[i]])
                S.op("pool", lambda e, i=i: e.memset(Xpad[i][:], 0.0), [], [Xpad[i]])
            for g in range(32):
                bp = Bpad[g % 2]; xp = Xpad[g % 2]
                S.op("pool", lambda e, bp=bp, g=g: e.tensor_copy(out=bp[:, 0, :, 112:128], in_=Bb[:, :, g, :]), [Bb], [bp])
                S.op("pool", lambda e, bp=bp, g=g: e.tensor_copy(out=bp[:, 1, :, 112:128], in_=Bb[:, :, 32 + g, :]), [Bb], [bp])
                S.op("act", lambda e, xp=xp, g=g: e.activation(out=xp[:, 0, :, 112:240].rearrange("p r (t h) -> p r t h", h=16), in_=XC[:, :, g, 0:8, :], func=AF.Copy), [XC], [xp])
                for i_ in range(8):
                    S.op("pool", lambda e, xp=xp, g=g, i_=i_: e.tensor_copy(out=xp[:, 1, :, i_ * 16:(i_ + 1) * 16], in_=XC[:, :, 32 + g, 7 - i_, :]), [XC], [xp])
                pb = PB[2 + (g % 2)]
                n_mm = 0
                for d_ in range(2):
                    for ri in range(2):
                        for s_ in range(8):
                            w0 = (7 - s_) * 16
                            S.op("pe", lambda e, pb=pb, bp=bp, xp=xp, d_=d_, ri=ri, w0=w0, n_mm=n_mm: e.matmul(
                                pb[:, 0:128], lhsT=bp[:, d_, ri, w0:w0 + 128], rhs=xp[:, d_, ri, w0:w0 + 128], start=(n_mm == 0), stop=(n_mm == 31)),
                                [bp, xp], [pb])
                            n_mm += 1
                S.op("dve", lambda e, pb=pb, g=g: e.scalar_tensor_tensor(out=M_bf[:, g, :], in0=ident_f[:], scalar=Dcol[:, g:g + 1], in1=pb[:, 0:128],
                                                                       op0=ALU.mult, op1=ALU.add), [pb, ident_f, Dcol], [M_bf])
            S.barrier()
        uT_all = S.sb("uT_all", [128, 32, NCH], BF16, ph)
        if dbg and stage == 30:
            for nm, t_, shp in (("M", M_bf, [128, 32 * 128]), ("W1", W1_bf, [128, 64 * 128])):
                dd = dbg_tensor(nm, shp, BF16)
                S.dma("sp", lambda e, dd=dd, t_=t_: e.dma_start(out=dd, in_=t_[:].rearrange("p a b -> p (a b)") if len(t_.t.shape) == 3 else t_[:].rearrange("p a b c -> p (a b c)")),
                      [t_], [DB["dbg"]], t_, concurrent=True)
            dd = dbg_tensor("W3", [64, 64 * 256], BF16)
            S.dma("sp", lambda e: e.dma_start(out=dd, in_=W3_bf[:].rearrange("p a b c -> p (a b c)")), [W3_bf], [DB["dbg"]], W3_bf, concurrent=True)
            dd2 = dbg_tensor("PW", [64, 64 * 48], F32)
            S.dma("sp", lambda e: e.dma_start(out=dd2, in_=PW[:].rearrange("p a b c -> p (a b c)")), [PW], [DB["dbg"]], PW, concurrent=True)
            S.barrier()
            return
        with ExitStack() as pU:
            uc = S.sb("uc", [128, 5, 4096], BF16, pU)
            S.dma("sp", lambda e: e.dma_start(out=uc[0:32, 0, :], in_=U_d[0, 0:32, :]), [DB["U"]], [uc], uc)
            for ct in range(4):
                S.dma("sp", lambda e, ct=ct: e.dma_start(out=uc[:, 1 + ct, :], in_=U_d[1 + ct, :, :]), [DB["U"]], [uc], uc, concurrent=True)
            for g in range(32):
                pb = PB[g % 4]
                pbb = pb.t.bitcast(BF16)
                S.op("pe", lambda e, pbb=pbb, g=g: e.transpose(pbb[:, 0:32], uc[0:32, 0, g * 128:(g + 1) * 128], ident_bf[0:32, 0:32]), [uc, ident_bf], [pb])
                for ct in range(4):
                    S.op("pe", lambda e, pbb=pbb, g=g, ct=ct: e.transpose(pbb[:, 32 + ct * 128:32 + (ct + 1) * 128], uc[:, 1 + ct, g * 128:(g + 1) * 128], ident_bf[:]),
                         [uc, ident_bf], [pb])
                if g % 2 == 0:
                    S.op("act", lambda e, pbb=pbb, g=g: e.activation(out=uT_all[:, g, :], in_=pbb[:, 0:NCH], func=AF.Copy), [pb], [uT_all])
                else:
                    S.op("dve", lambda e, pbb=pbb, g=g: e.tensor_copy(out=uT_all[:, g, :], in_=pbb[:, 0:NCH]), [pb], [uT_all])
            S.barrier()
        G = 4
        Sbuf = [[S.sb("S%d%d" % (d_, ri), [64, G, NCH], F32, ph) for ri in range(2)] for d_ in range(2)]
        Hbf = [[S.sb("H%d%d" % (d_, ri), [64, G, NCH + 2], BF16, ph) for ri in range(2)] for d_ in range(2)]
        Cy = [[S.sb("Cy%d%d" % (d_, ri), [64, G, 36], F32, ph) for ri in range(2)] for d_ in range(2)]
        tl = [[S.sb("tl%d%d" % (d_, i), [64, G, 34], F32, ph) for i in range(4)] for d_ in range(2)]
        Zb = [[S.sb("Zb%d%d" % (d_, ri), [64, G, 34], F32, ph) for ri in range(2)] for d_ in range(2)]
        tf = [[S.sb("tf%d%d" % (d_, i), [64, 34, 16], F32, ph) for i in range(2)] for d_ in range(2)]
        ytm = [S.sb("ytm%d" % i, [128, 4, 8, 128], BF16, ph) for i in range(1)]
        yTs = [S.sb("yTs%d" % i, [128, L], BF16, ph) for i in range(1)]
        for d_ in range(2):
            for ri in range(2):
                S.op("pool", lambda e, d_=d_, ri=ri: e.memset(Hbf[d_][ri][:], 0.0), [], [Hbf[d_][ri]])
                S.op("pool", lambda e, d_=d_, ri=ri: e.memset(Cy[d_][ri][:], 0.0), [], [Cy[d_][ri]])
        ENG_D = ("dve", "pool")
        for bt in range(8):
            for d_ in range(2):
                en = ENG_D[d_]
                Sr, Si = Sbuf[d_]
                for gl in range(G):
                    g = bt * G + gl
                    dg = d_ * 32 + g
                    for ri in range(2):
                        pa = PB[4 + ((gl * 2 + ri) % 2) * 2]
                        pb2 = PB[5 + ((gl * 2 + ri) % 2) * 2]
                        lw = W1_bf[:, dg, ri, :]
                        if d_ == 0:
                            S.op("pe", lambda e, pa=pa, lw=lw, g=g: e.matmul(pa[0:64, 0:272], lhsT=lw, rhs=uT_all[:, g, 0:272], start=True, stop=True), [W1_bf, uT_all], [pa])
                            S.op("pe", lambda e, pb2=pb2, lw=lw, g=g: e.matmul(pb2[0:64, 0:272], lhsT=lw, rhs=uT_all[:, g, 272:544], start=True, stop=True), [W1_bf, uT_all], [pb2])
                        else:
                            S.op("pe", lambda e, pa=pa, lw=lw, g=g: e.matmul(pa[0:64, 0:272], lhsT=lw, rhs=uT_all[:, g, 32:304], start=True, stop=True), [W1_bf, uT_all], [pa])
                            S.op("pe", lambda e, pb2=pb2, lw=lw, g=g: e.matmul(pb2[0:64, 0:240], lhsT=lw, rhs=uT_all[:, g, 304:544], start=True, stop=True), [W1_bf, uT_all], [pb2])
                            S.op("pe", lambda e, pb2=pb2, lw=lw, g=g: e.matmul(pb2[0:64, 240:272], lhsT=lw, rhs=uT_all[:, g, 0:32], start=True, stop=True), [W1_bf, uT_all], [pb2])
                        dst = (Sr, Si)[ri]
                        S.op("act", lambda e, pa=pa, dst=dst, gl=gl: e.activation(out=dst[:, gl, 0:272], in_=pa[0:64, 0:272], func=AF.Copy), [pa], [dst])
                        S.op("act", lambda e, pb2=pb2, dst=dst, gl=gl: e.activation(out=dst[:, gl, 272:544], in_=pb2[0:64, 0:272], func=AF.Copy), [pb2], [dst])
                gsl = slice(d_ * 32 + bt * G, d_ * 32 + bt * G + G)
                Ar = PW[:, gsl, 0, 8:9].to_broadcast([64, G, 34]); Ai = PW[:, gsl, 1, 8:9].to_broadcast([64, G, 34])
                Srv = Sr[:].rearrange("p g (s i) -> p g s i", i=16); Siv = Si[:].rearrange("p g (s i) -> p g s i", i=16)
                t1, t2, t3, t4 = tl[d_]
                steps = range(1, 16) if d_ == 0 else range(14, -1, -1)
                for i in steps:
                    ip = i - 1 if d_ == 0 else i + 1
                    S.op(en, lambda e, ip=ip: e.tensor_tensor(out=t1[:], in0=Srv[:, :, :, ip], in1=Ar, op=ALU.mult), [Sr, PW], [t1])
                    S.op(en, lambda e, ip=ip: e.tensor_tensor(out=t2[:], in0=Siv[:, :, :, ip], in1=Ai, op=ALU.mult), [Si, PW], [t2])
                    S.op(en, lambda e, ip=ip: e.tensor_tensor(out=t3[:], in0=Siv[:, :, :, ip], in1=Ar, op=ALU.mult), [Si, PW], [t3])
                    S.op(en, lambda e, ip=ip: e.tensor_tensor(out=t4[:], in0=Srv[:, :, :, ip], in1=Ai, op=ALU.mult), [Sr, PW], [t4])
                    S.op(en, lambda e: e.tensor_tensor(out=t1[:], in0=t1[:], in1=t2[:], op=ALU.subtract), [t1, t2], [t1])
                    S.op(en, lambda e: e.tensor_tensor(out=t3[:], in0=t3[:], in1=t4[:], op=ALU.add), [t3, t4], [t3])
                    S.op(en, lambda e, i=i: e.tensor_tensor(out=Srv[:, :, :, i], in0=Srv[:, :, :, i], in1=t1[:], op=ALU.add), [Sr, t1], [Sr])
                    S.op(en, lambda e, i=i: e.tensor_tensor(out=Siv[:, :, :, i], in0=Siv[:, :, :, i], in1=t3[:], op=ALU.add), [Si, t3], [Si])
                Cr, Ci = Cy[d_]
                Zr, Zi = Zb[d_]
                c1, c2, c3, c4 = tl[d_]
                if d_ == 0:
                    cur = (Srv[:, :, :, 15], Siv[:, :, :, 15]); cur_b = [Sr, Si]
                    cyv = (Cr[:, :, 1:35], Ci[:, :, 1:35])
                else:
                    cur = (Srv[:, :, :, 0], Siv[:, :, :, 0]); cur_b = [Sr, Si]
                    cyv = (Cr[:, :, 0:34], Ci[:, :, 0:34])
                zv = (Zr[:, :, :], Zi[:, :, :])
                for k_ in range(6):
                    sh = 1 << k_
                    ti_ = 23 + k_
                    n_ = 34 - sh
                    Bkr = PW[:, gsl, 0, ti_:ti_ + 1].to_broadcast([64, G, n_]); Bki = PW[:, gsl, 1, ti_:ti_ + 1].to_broadcast([64, G, n_])
                    dst = zv if k_ % 2 == 0 else cyv
                    dst_b = [Zr, Zi] if k_ % 2 == 0 else [Cr, Ci]
                    if d_ == 0:
                        src_sl = slice(0, n_); out_sl = slice(sh, 34); keep_sl = slice(0, sh)
                    else:
                        src_sl = slice(sh, 34); out_sl = slice(0, n_); keep_sl = slice(n_, 34)
                    xr_s = cur[0][:, :, src_sl]; xi_s = cur[1][:, :, src_sl]
                    S.op(en, lambda e, xr_s=xr_s, Bkr=Bkr, n_=n_: e.tensor_tensor(out=c1[:, :, 0:n_], in0=xr_s, in1=Bkr, op=ALU.mult), cur_b + [PW], [c1])
                    S.op(en, lambda e, xi_s=xi_s, Bki=Bki, n_=n_: e.tensor_tensor(out=c2[:, :, 0:n_], in0=xi_s, in1=Bki, op=ALU.mult), cur_b + [PW], [c2])
                    S.op(en, lambda e, xi_s=xi_s, Bkr=Bkr, n_=n_: e.tensor_tensor(out=c3[:, :, 0:n_], in0=xi_s, in1=Bkr, op=ALU.mult), cur_b + [PW], [c3])
                    S.op(en, lambda e, xr_s=xr_s, Bki=Bki, n_=n_: e.tensor_tensor(out=c4[:, :, 0:n_], in0=xr_s, in1=Bki, op=ALU.mult), cur_b + [PW], [c4])
                    S.op(en, lambda e, n_=n_: e.tensor_tensor(out=c1[:, :, 0:n_], in0=c1[:, :, 0:n_], in1=c2[:, :, 0:n_], op=ALU.subtract), [c1, c2], [c1])
                    S.op(en, lambda e, n_=n_: e.tensor_tensor(out=c3[:, :, 0:n_], in0=c3[:, :, 0:n_], in1=c4[:, :, 0:n_], op=ALU.add), [c3, c4], [c3])
                    S.op(en, lambda e, dst=dst, cur=cur, out_sl=out_sl, n_=n_: e.tensor_tensor(out=dst[0][:, :, out_sl], in0=cur[0][:, :, out_sl], in1=c1[:, :, 0:n_], op=ALU.add), cur_b + [c1], [dst_b[0]])
                    S.op(en, lambda e, dst=dst, cur=cur, out_sl=out_sl, n_=n_: e.tensor_tensor(out=dst[1][:, :, out_sl], in0=cur[1][:, :, out_sl], in1=c3[:, :, 0:n_], op=ALU.add), cur_b + [c3], [dst_b[1]])
                    S.op(en, lambda e, dst=dst, cur=cur, keep_sl=keep_sl: e.tensor_copy(out=dst[0][:, :, keep_sl], in_=cur[0][:, :, keep_sl]), cur_b, [dst_b[0]])
                    S.op(en, lambda e, dst=dst, cur=cur, keep_sl=keep_sl: e.tensor_copy(out=dst[1][:, :, keep_sl], in_=cur[1][:, :, keep_sl]), cur_b, [dst_b[1]])
                    cur = dst; cur_b = dst_b
                f1, f2 = tf[d_]
                Hr, Hi = Hbf[d_]
                for gl in range(G):
                    g = bt * G + gl
                    if d_ == 0:
                        Pr = PW[:, g, 0, 8:24].unsqueeze(1).to_broadcast([64, 34, 16]); Pi = PW[:, g, 1, 8:24].unsqueeze(1).to_broadcast([64, 34, 16])
                        cyr = Cr[:, gl, 0:34].unsqueeze(2).to_broadcast([64, 34, 16]); cyi = Ci[:, gl, 0:34].unsqueeze(2).to_broadcast([64, 34, 16])
                        hro = Hr[:, gl, 1:545].rearrange("p (s i) -> p s i", i=16); hio = Hi[:, gl, 1:545].rearrange("p (s i) -> p s i", i=16)
                    else:
                        Pr = PWr[:, g, 0, :].unsqueeze(1).to_broadcast([64, 34, 16]); Pi = PWr[:, g, 1, :].unsqueeze(1).to_broadcast([64, 34, 16])
                        cyr = Cr[:, gl, 1:35].unsqueeze(2).to_broadcast([64, 34, 16]); cyi = Ci[:, gl, 1:35].unsqueeze(2).to_broadcast([64, 34, 16])
                        hro = Hr[:, gl, 0:544].rearrange("p (s i) -> p s i", i=16); hio = Hi[:, gl, 0:544].rearrange("p (s i) -> p s i", i=16)
                    srv = Srv[:, gl, :, :]; siv = Siv[:, gl, :, :]
                    en_f = "dve" if (d_ == 1 and gl >= 2) else en
                    S.op(en_f, lambda e, Pr=Pr, cyr=cyr: e.tensor_tensor(out=f1[:], in0=Pr, in1=cyr, op=ALU.mult), [PW, PWr, Cr], [f1])
                    S.op(en_f, lambda e, Pi=Pi, cyi=cyi: e.tensor_tensor(out=f2[:], in0=Pi, in1=cyi, op=ALU.mult), [PW, PWr, Ci], [f2])
                    S.op(en_f, lambda e: e.tensor_tensor(out=f1[:], in0=f1[:], in1=f2[:], op=ALU.subtract), [f1, f2], [f1])
                    S.op(en_f, lambda e, hro=hro, srv=srv: e.tensor_tensor(out=hro, in0=srv, in1=f1[:], op=ALU.add), [Sr, f1], [Hr])
                    S.op(en_f, lambda e, Pr=Pr, cyi=cyi: e.tensor_tensor(out=f1[:], in0=Pr, in1=cyi, op=ALU.mult), [PW, PWr, Ci], [f1])
                    S.op(en_f, lambda e, Pi=Pi, cyr=cyr: e.tensor_tensor(out=f2[:], in0=Pi, in1=cyr, op=ALU.mult), [PW, PWr, Cr], [f2])
                    S.op(en_f, lambda e: e.tensor_tensor(out=f1[:], in0=f1[:], in1=f2[:], op=ALU.add), [f1, f2], [f1])
                    S.op(en_f, lambda e, hio=hio, siv=siv: e.tensor_tensor(out=hio, in0=siv, in1=f1[:], op=ALU.add), [Si, f1], [Hi])
            yt = ytm[0]
            for ct in range(4):
                pb = PB[ct % 4]
                for gl in range(G):
                    g = bt * G + gl
                    osl = pb[:, gl * 128:(gl + 1) * 128]
                    c0 = 32 + ct * 128
                    S.op("pe", lambda e, osl=osl, g=g, c0=c0: e.matmul(osl, lhsT=uT_all[:, g, c0:c0 + 128], rhs=M_bf[:, g, :], start=True, stop=False), [uT_all, M_bf], [pb])
                    for ri in range(2):
                        S.op("pe", lambda e, osl=osl, g=g, gl=gl, ri=ri, c0=c0: e.matmul(osl, lhsT=Hbf[0][ri][:, gl, c0:c0 + 128], rhs=W3_bf[:, g, ri, :], start=False, stop=False),
                             [Hbf[0][ri], W3_bf], [pb])
                    for ri in range(2):
                        S.op("pe", lambda e, osl=osl, g=g, gl=gl, ri=ri, ct=ct: e.matmul(osl, lhsT=Hbf[1][ri][:, gl, ct * 128 + 1:ct * 128 + 129], rhs=W3_bf[:, 32 + g, ri, :],
                                                                                         start=False, stop=(ri == 1)), [Hbf[1][ri], W3_bf], [pb])
                off = (bt % 2) * 64
                S.op("act", lambda e, pb=pb, yt=yt, ct=ct, off=off: e.activation(out=yt[:, ct, :, off:off + 64].rearrange("p j (g h) -> p j g h", h=16),
                                                                                in_=pb[:, :].rearrange("p (g j h) -> p j g h", g=4, h=16), func=AF.Gelu), [pb], [yt])
            if bt % 2 == 1:
                pair = bt // 2
                ys = yTs[0]
                for ct in range(4):
                    for jh in range(2):
                        pb = PB[4 + ((ct * 2 + jh) % 4)]
                        pbb = pb.t.bitcast(BF16)
                        for jj in range(4):
                            S.op("pe", lambda e, pbb=pbb, yt=yt, ct=ct, jh=jh, jj=jj: e.transpose(pbb[:, jj * 128:(jj + 1) * 128], yt[:, ct, jh * 4 + jj, :], ident_bf[:]),
                                 [yt, ident_bf], [pb])
                        S.op("dve", lambda e, pbb=pbb, ys=ys, ct=ct, jh=jh: e.tensor_copy(
                            out=ys[:, ct * 1024:(ct + 1) * 1024].rearrange("p (c j) -> p c j", j=8)[:, :, jh * 4:jh * 4 + 4],
                            in_=pbb[:, 0:512].rearrange("p (j c) -> p c j", j=4)), [pb], [ys])
                S.dma("sp", lambda e, ys=ys, pair=pair: e.dma_start(out=YT_d[pair * 128:(pair + 1) * 128, :], in_=ys[:]), [ys], [DB_YT], ys, concurrent=True)
        S.barrier()
    with ExitStack() as pg:
        yT = S.sb("yT", [128, 4, L], BF16, pg)
        wg = S.sb("wglu", [128, 4, 512], BF16, pg)
        oring = Ring(S, "yao", [128, 512], BF16, 3, pg)
        sring = Ring(S, "sgl", [128, 512], F32, 3, pg)
        S.dma("pool", lambda e: e.dma_start(out=wg[:], in_=A["w_glu"].rearrange("(k p) n -> p k n", p=128)), [DB["w_glu"]], [wg], wg)
        for kt in range(4):
            S.dma("sp", lambda e, kt=kt: e.dma_start(out=yT[:, kt, :], in_=YT_d[kt * 128:(kt + 1) * 128, :]), [DB_YT], [yT], yT, concurrent=(kt > 0))
        n_ = 0
        for tb in range(8):
            for co in range(4):
                pb = PB[n_ % 4]; n_ += 1
                for kt in range(4):
                    S.op("pe", lambda e, pb=pb, kt=kt, co=co, tb=tb: e.matmul(pb[:, :], lhsT=wg[:, kt, co * 128:(co + 1) * 128], rhs=yT[:, kt, tb * 512:(tb + 1) * 512],
                                                                            start=(kt == 0), stop=(kt == 3)), [wg, yT], [pb])
                sg = sring.next(); ob = oring.next()
                S.op("act", lambda e, pb=pb, sg=sg: e.activation(out=sg[:], in_=pb[:, :], func=AF.Sigmoid), [pb], [sg])
                S.op("dve", lambda e, sg=sg, ob=ob, co=co, tb=tb: e.tensor_tensor(out=ob[:], in0=sg[:], in1=yT[:, co, tb * 512:(tb + 1) * 512], op=ALU.mult), [sg, yT], [ob])
                S.dma("sp", lambda e, ob=ob, co=co, tb=tb: e.dma_start(out=YA_d[co * 128:(co + 1) * 128, tb * 512:(tb + 1) * 512], in_=ob[:]), [ob], [DB["YA"]], ob, concurrent=True)
        S.barrier()
        if dbg and stage == 3:
            dd = dbg_tensor("YA", [512, L], BF16)
            stg = Ring(S, "dstg3", [128, L], BF16, 2, pg)
            for r0 in range(0, 512, 128):
                sg_ = stg.next()
                S.dma("sp", lambda e, sg_=sg_, r0=r0: e.dma_start(out=sg_[:], in_=YA_d[r0:r0 + 128, :]), [DB["YA"]], [sg_], sg_)
                S.dma("sp", lambda e, sg_=sg_, r0=r0: e.dma_start(out=dd[r0:r0 + 128, :], in_=sg_[:]), [sg_], [DB["dbg"]], sg_, concurrent=True)
            S.barrier()


def phase4(nc, S, A, DB, PB, K, stage, dbg, dbg_tensor):
    ident_bf = K["ident_bf"]; ones_f = K["ones_f"]; ones_bf = K["ones_bf"]
    QT_d = K["QT_d"]; KT_d = K["KT_d"]; V_d = K["V_d"]; YB_d = K["YB_d"]
    NK = NT
    with ExitStack() as ph:
        KT = S.sb("KT", [128, 4, LC + L], BF16, ph)
        Vp = S.sb("Vp", [128, NK, 4, 128], BF16, ph)
        lamr = S.sb("lamr", [1, 264], F32, ph)
        lamc = S.sb("lamc", [128, 1], F32, ph)
        subw = S.sb("subw", [128, 128], F32, ph)
        qring = Ring(S, "qtb", [128, 4, 512], BF16, 2, ph)
        pring = Ring(S, "pT", [128, 512], BF16, 6, ph)
        acc = S.sb("acc", [128, 4, 128], F32, ph)
        st4 = S.sb("st4", [128, 4, 8], F32, ph)
        ybt = Ring(S, "ybt", [128, 4, 128], BF16, 2, ph)
        ybo = Ring(S, "ybo", [128, 512], BF16, 3, ph)
        junk = S.sb("junk4", [128, 128], F32, ph)
        zeros_bf = S.sb("zeros4", [128, 128], BF16, ph)
        S.op("pool", lambda e: e.memset(zeros_bf[:], 0.0), [], [zeros_bf])
        for hd in range(4):
            S.dma("sp", lambda e, hd=hd: e.dma_start(out=KT[:, hd, :], in_=KT_d[hd * 128:(hd + 1) * 128, :]), [DB["KT"]], [KT], KT, concurrent=(hd > 0))
        for kt in range(NK):
            S.dma("sp", lambda e, kt=kt: e.dma_start(out=Vp[:, kt, :, 0:128], in_=V_d[kt * 128:(kt + 1) * 128, :].rearrange("p (h d) -> p h d", h=4)), [DB["V"]], [Vp], Vp,
                  concurrent=(kt > 0))
        S.dma("sp", lambda e: e.dma_start(out=lamr[:, 0:256], in_=A["da_lambda"]), [DB["da"]], [lamr], lamr)
        S.dma("sp", lambda e: e.dma_start(out=subw[:], in_=A["da_subln"].partition_broadcast(128)), [DB["da"]], [subw], subw)
        lv = lamr[0:1, 0:256].rearrange("p (a b d) -> p a b d", a=2, b=2)
        S.op("dve", lambda e: e.tensor_tensor(out=lv[:, :, 0, :], in0=lv[:, :, 0, :], in1=lv[:, :, 1, :], op=ALU.mult), [lamr], [lamr])
        S.op("dve", lambda e: e.reduce_sum(out=lamr[0:1, 256:258], in_=lv[:, :, 0, :], axis=AX.X), [lamr], [lamr])
        S.op("act", lambda e: e.activation(out=lamr[0:1, 256:258], in_=lamr[0:1, 256:258], func=AF.Exp), [lamr], [lamr])
        S.op("dve", lambda e: e.scalar_tensor_tensor(out=lamr[0:1, 258:259], in0=lamr[0:1, 256:257], scalar=0.2, in1=lamr[0:1, 257:258], op0=ALU.add, op1=ALU.subtract), [lamr], [lamr])
        S.op("dve", lambda e: e.tensor_scalar(out=lamr[0:1, 259:260], in0=lamr[0:1, 258:259], scalar1=-1.0, scalar2=None, op0=ALU.mult), [lamr], [lamr])
        pl = PB[3]
        S.op("pe", lambda e: e.matmul(pl[:, 0:1], lhsT=ones_f[0:1, :], rhs=lamr[0:1, 259:260], start=True, stop=True), [ones_f, lamr], [pl])
        S.op("dve", lambda e: e.tensor_copy(out=lamc[:], in_=pl[:, 0:1]), [pl], [lamc])
        S.op("dve", lambda e: e.tensor_scalar(out=subw[:], in0=subw[:], scalar1=0.8, scalar2=None, op0=ALU.mult), [subw], [subw])

        subcol = S.sb("subcol", [128, 1], F32, ph)
        S.dma("sp", lambda e: e.dma_start(out=subcol[:], in_=A["da_subln"].rearrange("o d -> d o"), allow_slow_non_contiguous=True), [DB["da"]], [subcol], subcol)
        S.op("dve", lambda e: e.tensor_scalar(out=subcol[:], in0=subcol[:], scalar1=0.8, scalar2=None, op0=ALU.mult), [subcol], [subcol])
        r1r = Ring(S, "r1_4", [128, 512], F32, 3, ph); r2r = Ring(S, "r2_4", [128, 512], F32, 3, ph)
        sqr = Ring(S, "sq_4", [128, 512], BF16, 3, ph)
        sT_i = [0]
        deferred = []

        def sbank():
            b = PB[sT_i[0] % 4]
            sT_i[0] += 1
            return b
        for qb in range(8):
            qt_b = qring.next()
            S.dma("sp", lambda e, qt_b=qt_b, qb=qb: e.dma_start(out=qt_b[:], in_=QT_d[:, qb * 512:(qb + 1) * 512].rearrange("(h p) t -> p h t", p=128)), [DB["QT"]], [qt_b], qt_b)
            for hd in range(4):
                OV = (PB[4], PB[5]); DEN = (PB[6], PB[7])
                pts = {}

                def score(kt, cp):
                    sT = sbank()
                    psl = slice(cp * 64, (cp + 1) * 64)
                    S.op("pe", lambda e, sT=sT, kt=kt, psl=psl: e.matmul(sT[:, :], lhsT=KT[psl, hd, kt * 128:(kt + 1) * 128], rhs=qt_b[psl, hd, :], start=True, stop=True), [KT, qt_b], [sT])
                    pT = pring.next()
                    S.op("act", lambda e, sT=sT, pT=pT: e.activation(out=pT[:], in_=sT[:, :], func=AF.Exp, scale=0.125), [sT], [pT])
                    pts[(kt, cp)] = pT
                score(0, 0); score(0, 1)
                for kt in range(NK):
                    if kt + 1 < NK:
                        score(kt + 1, 0); score(kt + 1, 1)
                    if kt == 6 and deferred:
                        deferred.pop(0)()
                    p0 = pts.pop((kt, 0)); p1 = pts.pop((kt, 1))
                    fl = dict(start=(kt == 0), stop=(kt == NK - 1))
                    S.op("pe", lambda e, p0=p0, kt=kt, fl=fl: e.matmul(OV[0][:, :], lhsT=Vp[:, kt, hd, 0:128], rhs=p0[:], **fl), [Vp, p0], [OV[0]])
                    S.op("pe", lambda e, p1=p1, kt=kt, fl=fl: e.matmul(OV[1][:, :], lhsT=Vp[:, kt, hd, 0:128], rhs=p1[:], **fl), [Vp, p1], [OV[1]])
                    S.op("pe", lambda e, p0=p0, fl=fl: e.matmul(DEN[0][:, :], lhsT=ones_bf[:], rhs=p0[:], **fl), [ones_bf, p0], [DEN[0]])
                    S.op("pe", lambda e, p1=p1, fl=fl: e.matmul(DEN[1][:, :], lhsT=ones_bf[:], rhs=p1[:], **fl), [ones_bf, p1], [DEN[1]])
                r1 = r1r.next(); r2 = r2r.next(); sq = sqr.next(); yo = ybo.next()
                S.op("dve", lambda e, r1=r1: e.reciprocal(out=r1[:], in_=DEN[0][:, :]), [DEN[0]], [r1])
                S.op("dve", lambda e, r2=r2: e.reciprocal(out=r2[:], in_=DEN[1][:, :]), [DEN[1]], [r2])
                S.op("dve", lambda e, r1=r1: e.tensor_tensor(out=r1[:], in0=OV[0][:, :], in1=r1[:], op=ALU.mult), [OV[0], r1], [r1])
                S.op("dve", lambda e, r2=r2: e.tensor_tensor(out=r2[:], in0=OV[1][:, :], in1=r2[:], op=ALU.mult), [OV[1], r2], [r2])
                S.op("dve", lambda e, r1=r1, r2=r2: e.scalar_tensor_tensor(out=r1[:], in0=r2[:], scalar=lamc[:, 0:1], in1=r1[:], op0=ALU.mult, op1=ALU.add), [r1, r2, lamc], [r1])
                S.op("act", lambda e, r1=r1, sq=sq: e.activation(out=sq[:], in_=r1[:], func=AF.Square), [r1], [sq])

                def tail(r1=r1, r2=r2, sq=sq, yo=yo, hd=hd, qb=qb):
                    pss = sbank()
                    S.op("pe", lambda e: e.matmul(pss[:, :], lhsT=ones_bf[:], rhs=sq[:], start=True, stop=True), [ones_bf, sq], [pss])
                    S.op("act", lambda e: e.activation(out=r2[:], in_=pss[:, :], func=AF.Sqrt, scale=1.0 / 128.0, bias=EPS), [pss], [r2])
                    S.op("dve", lambda e: e.reciprocal(out=r2[:], in_=r2[:]), [r2], [r2])
                    S.op("dve", lambda e: e.scalar_tensor_tensor(out=yo[:], in0=r1[:], scalar=subcol[:, 0:1], in1=r2[:], op0=ALU.mult, op1=ALU.mult), [r1, r2, subcol], [yo])
                    S.dma("sp", lambda e: e.dma_start(out=YB_d[hd * 128:(hd + 1) * 128, qb * 512:(qb + 1) * 512], in_=yo[:]), [yo], [DB["YB"]], yo, concurrent=True)
                deferred.append(tail)
        while deferred:
            deferred.pop(0)()
        S.barrier()
        if dbg and stage == 4:
            dd = dbg_tensor("YB", [512, L], BF16)
            stg = Ring(S, "dstg4", [128, L], BF16, 2, ph)
            for r0 in range(0, 512, 128):
                sg_ = stg.next()
                S.dma("sp", lambda e, sg_=sg_, r0=r0: e.dma_start(out=sg_[:], in_=YB_d[r0:r0 + 128, :]), [DB["YB"]], [sg_], sg_)
                S.dma("sp", lambda e, sg_=sg_, r0=r0: e.dma_start(out=dd[r0:r0 + 128, :], in_=sg_[:]), [sg_], [DB["dbg"]], sg_, concurrent=True)
            S.barrier()


def phase5(nc, S, A, DB, PB, K, stage, dbg, dbg_tensor):
    ident_f = K["ident_f"]; aff = K["aff"]
    YA_d = K["YA_d"]; YB_d = K["YB_d"]; SG_d = K["SG_d"]; X1_d = K["X1_d"]; H2_d = K["H2_d"]; ROWS_d = K["ROWS_d"]
    with ExitStack() as ph:
        wpa = S.sb("wpa", [128, 4, D], BF16, ph)
        wpb = S.sb("wpb", [128, 4, D], BF16, ph)
        wout = S.sb("wout", [128, 8, D], BF16, ph)
        wr = S.sb("wr", [128, 8, 16], F32, ph)
        rows = S.sb("rows5", [128, 4, D], F32, ph)
        S.dma("pool", lambda e: e.dma_start(out=wpa[:], in_=A["w_proj_a"].rearrange("(k p) n -> p k n", p=128)), [DB["w_proj"]], [wpa], wpa)
        S.dma("pool", lambda e: e.dma_start(out=wpb[:], in_=A["w_proj_b"].rearrange("(k p) n -> p k n", p=128)), [DB["w_proj"]], [wpb], wpb)
        S.dma("pool", lambda e: e.dma_start(out=wout[:], in_=A["w_out"].rearrange("(k p) n -> p k n", p=128)), [DB["w_out"]], [wout], wout)
        S.dma("sp", lambda e: e.dma_start(out=wr[:], in_=A["w_router"].rearrange("(k p) n -> p k n", p=128)), [DB["w_router"]], [wr], wr)
        S.dma("sp", lambda e: e.dma_start(out=rows[:].rearrange("p a b -> p (a b)"), in_=ROWS_d), [DB["ROWS"]], [rows], rows)
        yar = Ring(S, "ya5", [128, 4, 512], BF16, 2, ph)
        ybr = Ring(S, "yb5", [128, 4, 512], BF16, 2, ph)
        sgr = Ring(S, "sg5", [128, 16, 512], BF16, 2, ph)
        mT = Ring(S, "mT", [128, 8, 512], BF16, 2, ph)
        t1r = Ring(S, "t1_5", [128, 512], F32, 3, ph)
        t2r = Ring(S, "t2_5", [128, 512], F32, 3, ph)
        xr_ = Ring(S, "x5", [128, D], F32, 2, ph)
        x1r = Ring(S, "x1_5", [128, D], F32, 2, ph)
        h2r = Ring(S, "h2f", [128, D], F32, 5, ph)
        h2br = Ring(S, "h2b", [128, D], BF16, 2, ph)
        h2Tr = Ring(S, "h2T", [128, 8, 128], F32, 2, ph)
        junk = S.sb("junk5", [128, D], BF16, ph)
        st_ring = Ring(S, "st5", [128, 1, 12], F32, 8, ph)
        ex_ring = Ring(S, "ex5", [128, 16], F32, 3, ph)
        junk2 = S.sb("junk5b", [128, D], BF16, ph)
        def router_part(ti, h2, h2T, st):
            ex = ex_ring.next()
            for dt_ in range(8):
                pbk = PB[6 + dt_ // 4]
                S.op("pe", lambda e, pbk=pbk, dt_=dt_, h2=h2: e.transpose(pbk[:, (dt_ % 4) * 128:(dt_ % 4 + 1) * 128], h2[:, dt_ * 128:(dt_ + 1) * 128], ident_f[:]), [h2, ident_f], [pbk])
            S.op("act", lambda e, h2T=h2T: e.activation(out=h2T[:, 0:4, :].rearrange("p a b -> p (a b)"), in_=PB[6][:, :], func=AF.Copy), [PB[6]], [h2T])
            S.op("dve", lambda e, h2T=h2T: e.tensor_copy(out=h2T[:, 4:8, :].rearrange("p a b -> p (a b)"), in_=PB[7][:, :]), [PB[7]], [h2T])
            pl = PB[6]
            for dt_ in range(8):
                S.op("pe", lambda e, dt_=dt_, h2T=h2T: e.matmul(pl[:, 0:16], lhsT=h2T[:, dt_, :], rhs=wr[:, dt_, :], start=(dt_ == 0), stop=(dt_ == 7)), [h2T, wr], [pl])
            S.op("dve", lambda e, ti=ti: e.reduce_max(out=st[:, 0, 8:9], in_=pl[:, 0:16], axis=AX.X), [pl], [st])
            S.op("dve", lambda e, ti=ti: e.tensor_scalar(out=st[:, 0, 8:9], in0=st[:, 0, 8:9], scalar1=-1.0, scalar2=None, op0=ALU.mult), [st], [st])
            S.op("act", lambda e, ti=ti: e.activation(out=ex[:], in_=pl[:, 0:16], func=AF.Exp, bias=st[:, 0, 8:9], scale=1.0, accum_out=st[:, 0, 9:10]), [pl, st], [ex, st])
            S.op("dve", lambda e, ti=ti: e.reciprocal(out=st[:, 0, 10:11], in_=st[:, 0, 9:10]), [st], [st])
            S.op("dve", lambda e, ti=ti: e.tensor_scalar(out=aff[:, ti, :], in0=ex[:], scalar1=st[:, 0, 10:11], scalar2=None, op0=ALU.mult), [ex, st], [aff])
        pending = []
        blk_bufs = {}

        def load_block(tb):
            if tb >= 8:
                return
            ya = yar.next(); yb = ybr.next(); sg = sgr.next()
            tsl = slice(tb * 512, (tb + 1) * 512)
            S.dma("sp", lambda e: e.dma_start(out=ya[:], in_=YA_d[:, tsl].rearrange("(k p) t -> p k t", p=128)), [DB["YA"]], [ya], ya)
            S.dma("sp", lambda e: e.dma_start(out=yb[:], in_=YB_d[:, tsl].rearrange("(k p) t -> p k t", p=128)), [DB["YB"]], [yb], yb)
            S.dma("sp", lambda e: e.dma_start(out=sg[:], in_=SG_d[:, tsl].rearrange("(k p) t -> p k t", p=128)), [DB["SG"]], [sg], sg)
            blk_bufs[tb] = (ya, yb, sg)
        load_block(0)
        mo_i = [0]
        for tb in range(8):
            load_block(tb + 1)
            ya, yb, sg = blk_bufs.pop(tb)
            m_ = mT.next()
            for dm in range(8):
                pa = PB[0]; pb = PB[1]
                for kt in range(4):
                    S.op("pe", lambda e, pa=pa, kt=kt, dm=dm, ya=ya: e.matmul(pa[:, :], lhsT=wpa[:, kt, dm * 128:(dm + 1) * 128], rhs=ya[:, kt, :], start=(kt == 0), stop=(kt == 3)), [wpa, ya], [pa])
                for kt in range(4):
                    S.op("pe", lambda e, pb=pb, kt=kt, dm=dm, yb=yb: e.matmul(pb[:, :], lhsT=wpb[:, kt, dm * 128:(dm + 1) * 128], rhs=yb[:, kt, :], start=(kt == 0), stop=(kt == 3)), [wpb, yb], [pb])
                t1 = t1r.next(); t2 = t2r.next()
                S.op("dve", lambda e, pa=pa, t1=t1, sg=sg, dm=dm: e.tensor_tensor(out=t1[:], in0=pa[:, :], in1=sg[:, dm, :], op=ALU.mult), [pa, sg], [t1])
                S.op("dve", lambda e, pb=pb, t2=t2, sg=sg, dm=dm: e.tensor_tensor(out=t2[:], in0=pb[:, :], in1=sg[:, 8 + dm, :], op=ALU.mult), [pb, sg], [t2])
                S.op("pool", lambda e, t1=t1, t2=t2, m_=m_, dm=dm: e.tensor_tensor(out=m_[:, dm, :], in0=t1[:], in1=t2[:], op=ALU.add), [t1, t2], [m_])
            for tt in range(4):
                ti = tb * 4 + tt
                xt = xr_.next(); x1 = x1r.next(); h2 = h2r.next(); h2b = h2br.next(); h2T = h2Tr.next(); st = st_ring.next()
                S.dma("sp", lambda e, xt=xt, ti=ti: e.dma_start(out=xt[:], in_=A["x"][ti * 128:(ti + 1) * 128, :]), [DB["x"]], [xt], xt)
                mo = (PB[2], PB[3]) if mo_i[0] % 2 == 0 else (PB[4], PB[5])
                mo_i[0] += 1
                for half in range(2):
                    for dm in range(8):
                        S.op("pe", lambda e, half=half, dm=dm, tt=tt, m_=m_: e.matmul(mo[half][:, :], lhsT=m_[:, dm, tt * 128:(tt + 1) * 128], rhs=wout[:, dm, half * 512:(half + 1) * 512],
                                                                                start=(dm == 0), stop=(dm == 7)), [m_, wout], [mo[half]])
                if len(pending) > 2:
                    router_part(*pending.pop(0))
                for half in range(2):
                    S.op("act", lambda e, half=half, ti=ti: e.activation(out=junk[:, 0:512], in_=mo[half][:, :], func=AF.Square, accum_out=st[:, 0, half:half + 1]), [mo[half]], [junk, st])
                S.op("dve", lambda e, ti=ti: e.tensor_tensor(out=st[:, 0, 2:3], in0=st[:, 0, 0:1], in1=st[:, 0, 1:2], op=ALU.add), [st], [st])
                S.op("act", lambda e, ti=ti: e.activation(out=st[:, 0, 3:4], in_=st[:, 0, 2:3], func=AF.Sqrt, scale=1.0 / D, bias=EPS), [st], [st])
                S.op("dve", lambda e, ti=ti: e.reciprocal(out=st[:, 0, 4:5], in_=st[:, 0, 3:4]), [st], [st])
                for half in range(2):
                    hs = slice(half * 512, (half + 1) * 512)
                    S.op("dve", lambda e, half=half, hs=hs, x1=x1, ti=ti: e.scalar_tensor_tensor(out=x1[:, hs], in0=mo[half][:, :], scalar=st[:, 0, 4:5], in1=rows[:, 0, hs], op0=ALU.mult, op1=ALU.mult),
                         [mo[half], st, rows], [x1])
                S.op("pool", lambda e, x1=x1, xt=xt: e.tensor_tensor(out=x1[:], in0=x1[:], in1=xt[:], op=ALU.add), [x1, xt], [x1])
                S.dma("sp", lambda e, x1=x1, ti=ti: e.dma_start(out=X1_d[ti * 128:(ti + 1) * 128, :], in_=x1[:]), [x1], [DB["X1"]], x1, concurrent=True)
                S.op("act", lambda e, x1=x1, ti=ti: e.activation(out=junk2[:], in_=x1[:], func=AF.Square, accum_out=st[:, 0, 5:6]), [x1], [junk2, st])
                S.op("act", lambda e, ti=ti: e.activation(out=st[:, 0, 6:7], in_=st[:, 0, 5:6], func=AF.Sqrt, scale=1.0 / D, bias=EPS), [st], [st])
                S.op("dve", lambda e, ti=ti: e.reciprocal(out=st[:, 0, 7:8], in_=st[:, 0, 6:7]), [st], [st])
                S.op("dve", lambda e, x1=x1, h2=h2, ti=ti: e.scalar_tensor_tensor(out=h2[:], in0=x1[:], scalar=st[:, 0, 7:8], in1=rows[:, 1, :], op0=ALU.mult, op1=ALU.mult), [x1, st, rows], [h2])
                S.op("pool", lambda e, h2=h2: e.tensor_tensor(out=h2[:], in0=h2[:], in1=rows[:, 2, :], op=ALU.add), [h2, rows], [h2])
                S.op("act", lambda e, h2=h2, h2b=h2b: e.activation(out=h2b[:], in_=h2[:], func=AF.Copy), [h2], [h2b])
                S.dma("sp", lambda e, h2b=h2b, ti=ti: e.dma_start(out=H2_d[ti * 128:(ti + 1) * 128, :], in_=h2b[:]), [h2b], [DB["H2"]], h2b, concurrent=True)
                pending.append((ti, h2, h2T, st))
        while pending:
            router_part(*pending.pop(0))
        S.barrier()
        if dbg and stage == 5:
            dd = dbg_tensor("X1", [L, D], F32); dh = dbg_tensor("H2", [L, D], BF16); da = dbg_tensor("aff", [128, 512], F32)
            S.dma("sp", lambda e: e.dma_start(out=da, in_=aff[:].rearrange("p a b -> p (a b)")), [aff], [DB["dbg"]], aff, concurrent=True)
            for ti in range(32):
                xt = xr_.next(); hb = h2br.next()
                S.dma("sp", lambda e, xt=xt, ti=ti: e.dma_start(out=xt[:], in_=X1_d[ti * 128:(ti + 1) * 128, :]), [DB["X1"]], [xt], xt)
                S.dma("sp", lambda e, xt=xt, ti=ti: e.dma_start(out=dd[ti * 128:(ti + 1) * 128, :], in_=xt[:]), [xt], [DB["dbg"]], xt, concurrent=True)
                S.dma("sp", lambda e, hb=hb, ti=ti: e.dma_start(out=hb[:], in_=H2_d[ti * 128:(ti + 1) * 128, :]), [DB["H2"]], [hb], hb)
                S.dma("sp", lambda e, hb=hb, ti=ti: e.dma_start(out=dh[ti * 128:(ti + 1) * 128, :], in_=hb[:]), [hb], [DB["dbg"]], hb, concurrent=True)
            S.barrier()


def phase678(nc, S, A, DB, PB, K, stage, dbg, dbg_tensor):
    aff = K["aff"]; iota_f = K["iota_f"]; pidx = K["pidx"]; ones_f = K["ones_f"]; ones_bf = K["ones_bf"]; ident_bf = K["ident_bf"]
    H2_d = K["H2_d"]; X1_d = K["X1_d"]; F_d = K["F_d"]; ROWS_d = K["ROWS_d"]; out_ap = K["out"]
    CAP = 512
    with ExitStack() as ph:
        zt = S.sb("zt", [128, D], F32, ph)
        S.op("pool", lambda e: e.memset(zt[:], 0.0), [], [zt])
        for ti in range(32):
            S.dma("sp", lambda e, ti=ti: e.dma_start(out=F_d[ti * 128:(ti + 1) * 128, :], in_=zt[:]), [zt], [DB["F"]], zt, concurrent=True)
        lo = S.sb("lo", [128, 16], F32, ph); hi = S.sb("hi", [128, 16], F32, ph); mid = S.sb("mid", [128, 16], F32, ph)
        dd_ = S.sb("dd", [128, 16], F32, ph); sel = S.sb("sel", [128, 16], F32, ph); part = S.sb("part", [128, 16], F32, ph)
        cmp_ = S.sb("cmp", [128, 32, 16], F32, ph)
        maskf = S.sb("maskf", [128, 32, 16], F32, ph); maskb = S.sb("maskb", [128, 32, 16], BF16, ph)
        posm = S.sb("posm", [128, 32, 16], F32, ph); offs = S.sb("offs", [128, 32, 16], F32, ph)
        RH = S.sb("RH", [128, 32, 16, 4], BF16, ph); ahf = S.sb("ahf", [128, 32, 16], F32, ph)
        U_bf = S.sb("U_bf", [128, 128], BF16, ph)
        S.op("dve", lambda e: e.memset(lo[:], 0.0), [], [lo])
        S.op("dve", lambda e: e.memset(hi[:], 1.0), [], [hi])
        pc = PB[0]
        for it in range(27):
            S.op("dve", lambda e: e.tensor_tensor(out=mid[:], in0=lo[:], in1=hi[:], op=ALU.add), [lo, hi], [mid])
            S.op("dve", lambda e: e.tensor_scalar(out=mid[:], in0=mid[:], scalar1=0.5, scalar2=None, op0=ALU.mult), [mid], [mid])
            S.op("dve", lambda e: e.tensor_tensor(out=cmp_[:], in0=aff[:], in1=mid[:].unsqueeze(1).to_broadcast([128, 32, 16]), op=ALU.is_ge), [aff, mid], [cmp_])
            S.op("dve", lambda e: e.reduce_sum(out=part[:], in_=cmp_[:].rearrange("p t e -> p e t"), axis=AX.X), [cmp_], [part])
            S.op("pe", lambda e: e.matmul(pc[:, 0:16], lhsT=ones_f[:], rhs=part[:], start=True, stop=True), [ones_f, part], [pc])
            S.op("dve", lambda e: e.tensor_scalar(out=sel[:], in0=pc[:, 0:16], scalar1=float(CAP) - 0.5, scalar2=None, op0=ALU.is_ge), [pc], [sel])
            S.op("dve", lambda e: e.tensor_tensor(out=dd_[:], in0=mid[:], in1=lo[:], op=ALU.subtract), [mid, lo], [dd_])
            S.op("dve", lambda e: e.tensor_tensor(out=dd_[:], in0=dd_[:], in1=sel[:], op=ALU.mult), [dd_, sel], [dd_])
            S.op("dve", lambda e: e.tensor_tensor(out=lo[:], in0=lo[:], in1=dd_[:], op=ALU.add), [lo, dd_], [lo])
            S.op("dve", lambda e: e.tensor_tensor(out=dd_[:], in0=hi[:], in1=mid[:], op=ALU.subtract), [hi, mid], [dd_])
            S.op("dve", lambda e: e.tensor_tensor(out=dd_[:], in0=dd_[:], in1=sel[:], op=ALU.mult), [dd_, sel], [dd_])
            S.op("dve", lambda e: e.tensor_tensor(out=hi[:], in0=mid[:], in1=dd_[:], op=ALU.add), [mid, dd_], [hi])
        S.op("dve", lambda e: e.tensor_tensor(out=maskf[:], in0=aff[:], in1=lo[:].unsqueeze(1).to_broadcast([128, 32, 16]), op=ALU.is_ge), [aff, lo], [maskf])
        S.op("dve", lambda e: e.tensor_copy(out=maskb[:], in_=maskf[:]), [maskf], [maskb])
        S.op("dve", lambda e: e.tensor_scalar(out=U_bf[:], in0=iota_f[:, 0:128], scalar1=pidx[:, 0:1], scalar2=None, op0=ALU.is_gt), [iota_f, pidx], [U_bf])
        pcnt = PB[1]; ppos = PB[2]
        mb2 = maskb[:].rearrange("p t e -> p (t e)")
        S.op("pe", lambda e: e.matmul(pcnt[:, :], lhsT=ones_bf[:], rhs=mb2, start=True, stop=True), [ones_bf, maskb], [pcnt])
        S.op("pe", lambda e: e.matmul(ppos[:, :], lhsT=U_bf[:], rhs=mb2, start=True, stop=True), [U_bf, maskb], [ppos])
        S.op("dve", lambda e: e.memset(offs[:, 0, :], 0.0), [], [offs])
        cntv = pcnt[:, :].rearrange("p (t e) -> p t e", e=16)
        for tt in range(1, 32):
            S.op("dve", lambda e, tt=tt: e.tensor_tensor(out=offs[:, tt, :], in0=offs[:, tt - 1, :], in1=cntv[:, tt - 1, :], op=ALU.add), [offs, pcnt], [offs])
        S.op("dve", lambda e: e.tensor_tensor(out=posm[:].rearrange("p t e -> p (t e)"), in0=ppos[:, :], in1=offs[:].rearrange("p t e -> p (t e)"), op=ALU.add), [ppos, offs], [posm])
        S.op("dve", lambda e: e.scalar_tensor_tensor(out=posm[:], in0=posm[:], scalar=1.0, in1=maskf[:], op0=ALU.add, op1=ALU.mult), [posm, maskf], [posm])
        S.op("dve", lambda e: e.tensor_scalar(out=posm[:], in0=posm[:], scalar1=-1.0, scalar2=None, op0=ALU.add), [posm], [posm])
        S.op("dve", lambda e: e.tensor_copy(out=RH[:, :, :, 0], in_=iota_f[:, 0:32].unsqueeze(2).to_broadcast([128, 32, 16])), [iota_f], [RH])
        S.op("dve", lambda e: e.tensor_copy(out=RH[:, :, :, 1].rearrange("p t e -> p (t e)"), in_=pidx[:, 0:1].to_broadcast([128, 512])), [pidx], [RH])
        S.op("dve", lambda e: e.tensor_copy(out=RH[:, :, :, 2], in_=aff[:]), [aff], [RH])
        S.op("dve", lambda e: e.tensor_copy(out=ahf[:], in_=RH[:, :, :, 2]), [RH], [ahf])
        S.op("dve", lambda e: e.tensor_tensor(out=RH[:, :, :, 3], in0=aff[:], in1=ahf[:], op=ALU.subtract), [aff, ahf], [RH])
        if dbg and stage == 6:
            d1 = dbg_tensor("posm", [128, 512], F32); d2 = dbg_tensor("thr", [128, 16], F32); d3 = dbg_tensor("aff6", [128, 512], F32)
            S.dma("sp", lambda e: e.dma_start(out=d3, in_=aff[:].rearrange("p a b -> p (a b)")), [aff], [DB["dbg"]], aff, concurrent=True)
            S.dma("sp", lambda e: e.dma_start(out=d1, in_=posm[:].rearrange("p a b -> p (a b)")), [posm], [DB["dbg"]], posm, concurrent=True)
            S.dma("sp", lambda e: e.dma_start(out=d2, in_=lo[:]), [lo], [DB["dbg"]], lo, concurrent=True)
            S.barrier()
            return
        pm = ExitStack()
        mkm = S.mark()
        selr = Ring(S, "selT", [128, 512], BF16, 10, pm)
        zeros_m = S.sb("zeros_m", [128, 128], BF16, pm)
        S.op("pool", lambda e: e.memset(zeros_m[:], 0.0), [], [zeros_m])
        idxf = [S.sb("idxf%d" % i, [128, 4, 4], F32, pm) for i in range(3)]
        idxi = [S.sb("idxi%d" % i, [128, 4], I32, pm) for i in range(3)]
        gate = [S.sb("gate%d" % i, [128, 4], F32, pm) for i in range(3)]
        xs = [S.sb("xs%d" % i, [128, 4, D], BF16, pm) for i in range(3)]
        xsT = S.sb("xsT", [128, 8, 512], BF16, pm)
        wgr = Ring(S, "wg", [128, 8, 512], BF16, 3, pm)
        wur = Ring(S, "wu", [128, 8, 512], BF16, 3, pm)
        wdr = Ring(S, "wd", [128, 16, 512], BF16, 2, pm)
        hidT = S.sb("hidT", [128, 16, 512], BF16, pm)
        sgr = Ring(S, "sgm", [128, 512], F32, 2, pm)
        ysr = [S.sb("ys%d" % i, [128, D], F32, pm) for i in range(4)]

        def compaction_steps(e_):
            k = e_ % 3
            pcs = PB[0]
            steps = []
            dsteps = []

            def init():
                S.op("pe", lambda e: e.matmul(pcs[:, 0:16], lhsT=zeros_m[:], rhs=RH[:, 0, 0:4, :].rearrange("p a b -> p (a b)"), start=True, stop=False), [zeros_m, RH], [pcs])
            steps.append(init)
            sels = {}
            for tt in range(32):
                def dstep(tt=tt):
                    sl = selr.next()
                    S.op("dve", lambda e: e.tensor_scalar(out=sl[:], in0=iota_f[:, :], scalar1=posm[:, tt, e_:e_ + 1], scalar2=None, op0=ALU.is_equal), [iota_f, posm], [sl])
                    sels[tt] = sl

                def pstep(tt=tt):
                    sl = sels.pop(tt)
                    for st in range(4):
                        S.op("pe", lambda e, st=st: e.matmul(pcs[:, st * 4:(st + 1) * 4], lhsT=sl[:, st * 128:(st + 1) * 128], rhs=RH[:, tt, e_, :], start=False, stop=(tt == 31)),
                             [sl, RH], [pcs])
                dsteps.append(dstep); steps.append(pstep)

            def fin():
                S.op("dve", lambda e: e.tensor_copy(out=idxf[k][:].rearrange("p a b -> p (a b)"), in_=pcs[:, 0:16]), [pcs], [idxf[k]])
                S.op("dve", lambda e: e.scalar_tensor_tensor(out=idxf[k][:, :, 0], in0=idxf[k][:, :, 0], scalar=128.0, in1=idxf[k][:, :, 1], op0=ALU.mult, op1=ALU.add), [idxf[k]], [idxf[k]])
                S.op("dve", lambda e: e.tensor_scalar(out=idxf[k][:, :, 0], in0=idxf[k][:, :, 0], scalar1=8388608.0, scalar2=None, op0=ALU.add), [idxf[k]], [idxf[k]])
                S.op("dve", lambda e: e.tensor_single_scalar(out=idxi[k][:], in_=idxf[k][:, :, 0].bitcast(I32), scalar=0x7FFFFF, op=ALU.bitwise_and), [idxf[k]], [idxi[k]])
                S.op("dve", lambda e: e.tensor_tensor(out=gate[k][:], in0=idxf[k][:, :, 2], in1=idxf[k][:, :, 3], op=ALU.add), [idxf[k]], [gate[k]])
                for st in range(4):
                    S.dma("pool", lambda e, st=st: e.indirect_dma_start(out=xs[k][:, st, :], out_offset=None, in_=H2_d, in_offset=bass.IndirectOffsetOnAxis(ap=idxi[k][:, st:st + 1], axis=0)),
                          [DB["H2"], idxi[k]], [xs[k]], xs[k], concurrent=(st > 0))
            steps.append(fin)
            return steps, dsteps

        wtasks = []
        for e_ in range(16):
            for fb in range(4):
                wtasks.append(("gu", e_, fb))
            for half in range(2):
                wtasks.append(("d", e_, half))
        wbuf = {}
        nxt = [0]

        def prefetch(upto):
            while nxt[0] < len(wtasks) and nxt[0] <= upto:
                kind, e_, j = wtasks[nxt[0]]
                if kind == "gu":
                    wg = wgr.next(); wu = wur.next()
                    S.dma("pool", lambda e, wg=wg, e_=e_, j=j: e.dma_start(out=wg[:], in_=A["w_exp_gate"][e_, :, j * 512:(j + 1) * 512].rearrange("(k p) n -> p k n", p=128)), [DB["w_exp"]], [wg], wg)
                    S.dma("pool", lambda e, wu=wu, e_=e_, j=j: e.dma_start(out=wu[:], in_=A["w_exp_up"][e_, :, j * 512:(j + 1) * 512].rearrange("(k p) n -> p k n", p=128)), [DB["w_exp"]], [wu], wu)
                    wbuf[nxt[0]] = (wg, wu)
                else:
                    wd = wdr.next()
                    S.dma("pool", lambda e, wd=wd, e_=e_, j=j: e.dma_start(out=wd[:], in_=A["w_exp_down"][e_, :, j * 512:(j + 1) * 512].rearrange("(k p) n -> p k n", p=128)), [DB["w_exp"]], [wd], wd)
                    wbuf[nxt[0]] = (wd,)
                nxt[0] += 1

        for e0 in range(2):
            ps_, ds_ = compaction_steps(e0)
            ps_.pop(0)()
            while ds_:
                for _ in range(4):
                    if ds_:
                        ds_.pop(0)()
                for _ in range(4):
                    if len(ps_) > 1:
                        ps_.pop(0)()
            while ps_:
                ps_.pop(0)()
            if e0 == 0:
                prefetch(1)
        for e_ in range(16):
            k = e_ % 3
            csteps, dsteps_ = compaction_steps(e_ + 2) if e_ + 2 < 16 else ([], [])
            for _ in range(6):
                if dsteps_:
                    dsteps_.pop(0)()
            for dt_ in range(8):
                pt = PB[(1, 6, 7)[dt_ % 3]]
                ptb = pt.t.bitcast(BF16)
                for st in range(4):
                    S.op("pe", lambda e, ptb=ptb, st=st, dt_=dt_: e.transpose(ptb[:, st * 128:(st + 1) * 128], xs[k][:, st, dt_ * 128:(dt_ + 1) * 128], ident_bf[:]), [xs[k], ident_bf], [pt])
                if dt_ % 2 == 0:
                    S.op("act", lambda e, ptb=ptb, dt_=dt_: e.activation(out=xsT[:, dt_, :], in_=ptb[:, 0:512], func=AF.Copy), [pt], [xsT])
                else:
                    S.op("dve", lambda e, ptb=ptb, dt_=dt_: e.tensor_copy(out=xsT[:, dt_, :], in_=ptb[:, 0:512]), [pt], [xsT])
            base = e_ * 6
            for fb in range(4):
                prefetch(base + fb + 2)
                wg, wu = wbuf.pop(base + fb)
                for f4 in range(4):
                    ft = fb * 4 + f4
                    pg = PB[2 + 2 * (ft % 2)]; pu = PB[3 + 2 * (ft % 2)]
                    for kt in range(8):
                        S.op("pe", lambda e, pg=pg, wg=wg, kt=kt, f4=f4: e.matmul(pg[:, :], lhsT=wg[:, kt, f4 * 128:(f4 + 1) * 128], rhs=xsT[:, kt, :], start=(kt == 0), stop=(kt == 7)), [wg, xsT], [pg])
                    for kt in range(8):
                        S.op("pe", lambda e, pu=pu, wu=wu, kt=kt, f4=f4: e.matmul(pu[:, :], lhsT=wu[:, kt, f4 * 128:(f4 + 1) * 128], rhs=xsT[:, kt, :], start=(kt == 0), stop=(kt == 7)), [wu, xsT], [pu])
                    sg = sgr.next()
                    S.op("act", lambda e, pg=pg, sg=sg: e.activation(out=sg[:], in_=pg[:, :], func=AF.Silu), [pg], [sg])
                    S.op("dve", lambda e, pu=pu, sg=sg, ft=ft: e.tensor_tensor(out=hidT[:, ft, :], in0=sg[:], in1=pu[:, :], op=ALU.mult), [sg, pu], [hidT])
                    for _ in range(3):
                        if len(csteps) > 1:
                            csteps.pop(0)()
                        if dsteps_:
                            dsteps_.pop(0)()
            for half in range(2):
                prefetch(base + 4 + half + 2)
                (wd,) = wbuf.pop(base + 4 + half)
                for st in range(4):
                    po = PB[6 + (st % 2)]
                    for ft in range(16):
                        S.op("pe", lambda e, po=po, wd=wd, ft=ft, st=st: e.matmul(po[:, :], lhsT=hidT[:, ft, st * 128:(st + 1) * 128], rhs=wd[:, ft, :], start=(ft == 0), stop=(ft == 15)), [hidT, wd], [po])
                    S.op("act", lambda e, po=po, st=st, half=half: e.activation(out=ysr[st][:, half * 512:(half + 1) * 512], in_=po[:, :], func=AF.Copy, scale=gate[k][:, st:st + 1]), [po, gate[k]], [ysr[st]])
            while csteps:
                csteps.pop(0)()
            for st in range(4):
                S.dma("pool", lambda e, st=st: e.indirect_dma_start(out=F_d, out_offset=bass.IndirectOffsetOnAxis(ap=idxi[k][:, st:st + 1], axis=0), in_=ysr[st][:], in_offset=None, compute_op=ALU.add),
                      [ysr[st], idxi[k]], [DB["F"]], ysr[st], concurrent=(st > 0))
        S.release(mkm)
        pm.close()
        gwf = S.sb("gwf", [128, D], F32, ph)
        S.dma("sp", lambda e: e.dma_start(out=gwf[:], in_=ROWS_d[:, 3 * D:4 * D]), [DB["ROWS"]], [gwf], gwf)
        fr = Ring(S, "ft", [128, D], F32, 4, ph); x1r = Ring(S, "x1f", [128, D], F32, 4, ph); orr = Ring(S, "of", [128, D], F32, 4, ph)
        stf_ring = Ring(S, "stf", [128, 1, 4], F32, 4, ph)
        junk = S.sb("junk8", [128, D], BF16, ph)
        for ti in range(32):
            ft_ = fr.next(); x1 = x1r.next(); ot = orr.next(); stf = stf_ring.next()
            S.dma("sp", lambda e, ft_=ft_, ti=ti: e.dma_start(out=ft_[:], in_=F_d[ti * 128:(ti + 1) * 128, :]), [DB["F"]], [ft_], ft_)
            S.dma("sp", lambda e, x1=x1, ti=ti: e.dma_start(out=x1[:], in_=X1_d[ti * 128:(ti + 1) * 128, :]), [DB["X1"]], [x1], x1)
            S.op("act", lambda e, ft_=ft_, ti=ti: e.activation(out=junk[:], in_=ft_[:], func=AF.Square, accum_out=stf[:, 0, 0:1]), [ft_], [junk, stf])
            S.op("act", lambda e, ti=ti: e.activation(out=stf[:, 0, 1:2], in_=stf[:, 0, 0:1], func=AF.Sqrt, scale=1.0 / D, bias=EPS), [stf], [stf])
            S.op("dve", lambda e, ti=ti: e.reciprocal(out=stf[:, 0, 2:3], in_=stf[:, 0, 1:2]), [stf], [stf])
            S.op("dve", lambda e, ft_=ft_, ot=ot, ti=ti: e.scalar_tensor_tensor(out=ot[:], in0=ft_[:], scalar=stf[:, 0, 2:3], in1=gwf[:], op0=ALU.mult, op1=ALU.mult), [ft_, stf, gwf], [ot])
            S.op("pool", lambda e, ot=ot, x1=x1: e.tensor_tensor(out=ot[:], in0=ot[:], in1=x1[:], op=ALU.add), [ot, x1], [ot])
            S.dma("sp", lambda e, ot=ot, ti=ti: e.dma_start(out=out_ap[ti * 128:(ti + 1) * 128, :], in_=ot[:]), [ot], [DB["out"]], ot, concurrent=True)
        S.wait_all("sp", [DB["out"]])
        S.barrier()


_NC_CACHE = {}


def _core_inputs(inp, b):
    m = {}
    m["x"] = inp["x"][b]; m["ctx"] = inp["ctx"][b]
    m["c"] = inp["c"][b:b + 1]; m["c_ctx"] = np.asarray(inp["c_ctx"]).reshape(1, D)
    m["w_ada"] = inp["w_ada"][0]; m["b_ada"] = np.asarray(inp["b_ada"]).reshape(1, 6 * D)
    for n in ("norm_pre_mix", "norm_post_mix", "norm_pre_ffn", "norm_post_ffn"):
        m[n] = np.asarray(inp[n]).reshape(1, D)
    m["w_in"] = inp["w_in"][0]
    m["s5_lam_re"] = inp["s5_lam_re"][0].reshape(64, 64); m["s5_lam_im"] = inp["s5_lam_im"][0].reshape(64, 64)
    m["s5_log_dt"] = inp["s5_log_dt"][0].reshape(1, 64)
    m["s5_b_re"] = inp["s5_b_re"][0].reshape(64, 64, 16); m["s5_b_im"] = inp["s5_b_im"][0].reshape(64, 64, 16)
    m["s5_c_re"] = inp["s5_c_re"][0].reshape(2, 512, 64); m["s5_c_im"] = inp["s5_c_im"][0].reshape(2, 512, 64)
    m["s5_d"] = np.asarray(inp["s5_d"]).reshape(1, 512); m["w_glu"] = inp["w_glu"][0]
    m["da_lambda"] = inp["da_lambda"][0].reshape(1, 256); m["da_subln"] = np.asarray(inp["da_subln"]).reshape(1, 128)
    m["w_proj_a"] = inp["w_proj_a"][0]; m["w_proj_b"] = inp["w_proj_b"][0]; m["w_out"] = inp["w_out"][0]
    m["w_router"] = inp["w_router"][0]
    m["w_exp_gate"] = inp["w_exp_gate"][0]; m["w_exp_up"] = inp["w_exp_up"][0]; m["w_exp_down"] = inp["w_exp_down"][0]
    return {k: np.ascontiguousarray(np.asarray(v, dtype=np.float32)) for k, v in m.items()}


def kernel(**inputs):
    inp = {k: np.asarray(v) for k, v in inputs.items()}
    if "nc" not in _NC_CACHE:
        _NC_CACHE["nc"] = build()
    nc = _NC_CACHE["nc"]
    n = 8
    in_maps = [_core_inputs(inp, b) for b in range(n)]
    res = run_bass_kernel_spmd(nc, in_maps, core_ids=list(range(n)))
    return np.stack([np.asarray(r["out"], dtype=np.float32) for r in res.results], axis=0)
```
